# Optimizing a Trainium2 kernel written in Bass

```python
import math
import jax, jax.numpy as jnp
from jax import lax
import numpy as np

D_MODEL = 1024
BATCH = 16
SEQ = 2048
DEPTH = 4

CHUNK = 64
Q_BLOCK = 128
D_FF = 2752
DIFF_HEADS = 4
DIFF_QK_DIM = 32
DIFF_V_DIM = 2 * DIFF_QK_DIM
GLA_HEADS = 4
GLA_K_DIM = 64
GLA_V_DIM = 128
GLA_GATE_RANK = 16
GLA_TAU = 16.0
FOX_HEADS = 4
FOX_HEAD_DIM = 64
DIFF_WIDTH = DIFF_HEADS * DIFF_V_DIM
GLA_WIDTH = GLA_HEADS * GLA_V_DIM
FOX_WIDTH = FOX_HEADS * FOX_HEAD_DIM
MIX_WIDTH = DIFF_WIDTH + GLA_WIDTH + FOX_WIDTH
IN_SPLITS = (
    DIFF_HEADS * 2 * DIFF_QK_DIM,
    DIFF_HEADS * 2 * DIFF_QK_DIM,
    DIFF_WIDTH,
    GLA_HEADS * GLA_K_DIM,
    GLA_HEADS * GLA_K_DIM,
    GLA_WIDTH,
    GLA_WIDTH,
    GLA_GATE_RANK,
    FOX_WIDTH,
    FOX_WIDTH,
    FOX_WIDTH,
    FOX_HEADS,
)
IN_WIDTH = sum(IN_SPLITS)
NORM_EPS = 1e-6

kernel_name = "hybrid_diff_gla_fox_macaron"


def rms_norm(x, g):
    xf = x.astype(jnp.float32)
    y = xf * lax.rsqrt(jnp.mean(xf * xf, axis=-1, keepdims=True) + NORM_EPS)
    return (y * g.astype(jnp.float32)).astype(x.dtype)


def swiglu(h, w13, w2):
    gu = jnp.einsum('bsd,df->bsf', h, w13)
    gate, up = jnp.split(gu, 2, axis=-1)
    return jnp.einsum('bsf,fd->bsd', jax.nn.silu(gate) * up, w2)


def split_cols(p):
    out = []
    off = 0
    for w in IN_SPLITS:
        out.append(p[..., off:off + w])
        off += w
    return out


def diff_attention(q1, q2, k1, k2, v, lam):
    S = q1.shape[1]
    slopes = 2.0 ** (-8.0 * jnp.arange(1, DIFF_HEADS + 1, dtype=jnp.float32) / DIFF_HEADS)
    scale = DIFF_QK_DIM ** -0.5
    outs = []
    for blk in range(S // Q_BLOCK):
        q0 = blk * Q_BLOCK
        kend = q0 + Q_BLOCK
        qpos = jnp.arange(q0, kend)
        kpos = jnp.arange(kend)
        allowed = (kpos[None, :] // CHUNK) <= (qpos[:, None] // CHUNK)
        dist = jnp.abs(qpos[:, None] - kpos[None, :]).astype(jnp.float32)
        bias = jnp.where(allowed[None], -slopes[:, None, None] * dist[None], -jnp.inf)
        s1 = jnp.einsum('bqhd,bkhd->bhqk', q1[:, q0:kend], k1[:, :kend]) * scale + bias
        s2 = jnp.einsum('bqhd,bkhd->bhqk', q2[:, q0:kend], k2[:, :kend]) * scale + bias
        p = jax.nn.softmax(s1, axis=-1) - lam * jax.nn.softmax(s2, axis=-1)
        outs.append(jnp.einsum('bhqk,bkhd->bqhd', p, v[:, :kend]))
    return jnp.concatenate(outs, axis=1)


def fox_attention(q, k, v, log_f):
    S = q.shape[1]
    scale = FOX_HEAD_DIM ** -0.5
    F = jnp.cumsum(log_f, axis=1).transpose(0, 2, 1)
    outs = []
    for blk in range(S // Q_BLOCK):
        q0 = blk * Q_BLOCK
        kend = q0 + Q_BLOCK
        qpos = jnp.arange(q0, kend)
        kpos = jnp.arange(kend)
        causal = kpos[None, :] <= qpos[:, None]
        s = jnp.einsum('bqhd,bkhd->bhqk', q[:, q0:kend], k[:, :kend]) * scale
        s = s + F[:, :, q0:kend, None] - F[:, :, None, :kend]
        s = jnp.where(causal, s, -jnp.inf)
        p = jax.nn.softmax(s, axis=-1)
        outs.append(jnp.einsum('bhqk,bkhd->bqhd', p, v[:, :kend]))
    return jnp.concatenate(outs, axis=1)


def gla(q, k, v, log_alpha):
    B, S, H, dk = q.shape
    dv = v.shape[-1]
    n = S // CHUNK

    def to_chunks(t):
        return t.reshape(B, n, CHUNK, H, t.shape[-1]).transpose(1, 0, 3, 2, 4)

    tri = jnp.tril(jnp.ones((CHUNK, CHUNK), dtype=bool))[:, :, None]

    def step(state, inp):
        qc, kc, vc, ac = inp
        b = jnp.cumsum(ac, axis=2)
        o_inter = jnp.einsum('bhtd,bhde->bhte', qc * jnp.exp(b), state)
        diff = b[:, :, :, None, :] - b[:, :, None, :, :]
        decay = jnp.where(tri, jnp.exp(jnp.where(tri, diff, 0.0)), 0.0)
        attn = jnp.einsum('bhtd,bhsd,bhtsd->bhts', qc, kc, decay)
        o_intra = jnp.einsum('bhts,bhse->bhte', attn, vc)
        b_last = b[:, :, -1:, :]
        k_dec = kc * jnp.exp(b_last - b)
        new_state = state * jnp.exp(b_last[:, :, 0, :])[..., None] + jnp.einsum('bhsd,bhse->bhde', k_dec, vc)
        return new_state, o_inter + o_intra

    state0 = jnp.zeros((B, H, dk, dv), jnp.float32)
    _, o = lax.scan(step, state0, (to_chunks(q), to_chunks(k), to_chunks(v), to_chunks(log_alpha)))
    return o.transpose(1, 0, 3, 2, 4).reshape(B, S, H, dv)


def hybrid_mix(h, w_in, w_out, diff_q_norm, diff_k_norm, diff_lambda, diff_out_norm,
               gla_alpha_w2, gla_alpha_b, gla_out_norm, fox_q_norm, fox_k_norm,
               fox_f_bias, layer_idx):
    B, S, _ = h.shape
    p = jnp.einsum('bsd,de->bse', h, w_in).astype(jnp.float32)
    (d_q, d_k, d_v, g_q, g_k, g_v, g_r, g_a, f_q, f_k, f_v, f_f) = split_cols(p)

    d_q = rms_norm(d_q.reshape(B, S, DIFF_HEADS, 2, DIFF_QK_DIM), diff_q_norm)
    d_k = rms_norm(d_k.reshape(B, S, DIFF_HEADS, 2, DIFF_QK_DIM), diff_k_norm)
    lam_init = 0.8 - 0.6 * math.exp(-0.3 * layer_idx)
    lp = diff_lambda.astype(jnp.float32)
    lam = jnp.exp(jnp.sum(lp[0] * lp[1])) - jnp.exp(jnp.sum(lp[2] * lp[3])) + lam_init
    a = diff_attention(d_q[..., 0, :], d_q[..., 1, :], d_k[..., 0, :], d_k[..., 1, :],
                       d_v.reshape(B, S, DIFF_HEADS, DIFF_V_DIM), lam)
    a = rms_norm(a, diff_out_norm) * (1.0 - lam_init)

    z = jnp.einsum('bsr,rk->bsk', g_a, gla_alpha_w2.astype(jnp.float32)) + gla_alpha_b.astype(jnp.float32)
    log_alpha = (jax.nn.log_sigmoid(z) / GLA_TAU).reshape(B, S, GLA_HEADS, GLA_K_DIM)
    g = gla(g_q.reshape(B, S, GLA_HEADS, GLA_K_DIM) * (GLA_K_DIM ** -0.5),
            g_k.reshape(B, S, GLA_HEADS, GLA_K_DIM),
            g_v.reshape(B, S, GLA_HEADS, GLA_V_DIM), log_alpha)
    g = rms_norm(g, gla_out_norm).reshape(B, S, GLA_WIDTH) * jax.nn.silu(g_r)

    f_q = rms_norm(f_q.reshape(B, S, FOX_HEADS, FOX_HEAD_DIM), fox_q_norm)
    f_k = rms_norm(f_k.reshape(B, S, FOX_HEADS, FOX_HEAD_DIM), fox_k_norm)
    log_f = jax.nn.log_sigmoid(f_f + fox_f_bias.astype(jnp.float32))
    c = fox_attention(f_q, f_k, f_v.reshape(B, S, FOX_HEADS, FOX_HEAD_DIM), log_f)

    o = jnp.concatenate([a.reshape(B, S, DIFF_WIDTH), g, c.reshape(B, S, FOX_WIDTH)], axis=-1)
    return jnp.einsum('bse,ed->bsd', o.astype(h.dtype), w_out)


def setup_inputs(seed: int = 0) -> dict:
    key = jax.random.key(seed)
    ks = jax.random.split(key, 24)
    L = DEPTH

    def nrm(k, shape, scale):
        return jax.random.normal(k, shape, jnp.float32) * scale

    def gain(k, shape):
        return 1.0 + 0.1 * jax.random.normal(k, shape, jnp.float32)

    return {
        "x": jax.random.normal(ks[0], (BATCH, SEQ, D_MODEL), jnp.float32),
        "ffn1_norm": gain(ks[1], (L, D_MODEL)),
        "ffn1_w13": nrm(ks[2], (L, D_MODEL, 2 * D_FF), D_MODEL ** -0.5),
        "ffn1_w2": nrm(ks[3], (L, D_FF, D_MODEL), D_FF ** -0.5),
        "mix_norm": gain(ks[4], (L, D_MODEL)),
        "w_in": nrm(ks[5], (L, D_MODEL, IN_WIDTH), D_MODEL ** -0.5),
        "w_out": nrm(ks[6], (L, MIX_WIDTH, D_MODEL), MIX_WIDTH ** -0.5),
        "diff_q_norm": gain(ks[7], (L, DIFF_QK_DIM)),
        "diff_k_norm": gain(ks[8], (L, DIFF_QK_DIM)),
        "diff_lambda": nrm(ks[9], (L, 4, DIFF_QK_DIM), 0.1),
        "diff_out_norm": gain(ks[10], (L, DIFF_V_DIM)),
        "gla_alpha_w2": nrm(ks[11], (L, GLA_GATE_RANK, GLA_HEADS * GLA_K_DIM), GLA_GATE_RANK ** -0.5),
        "gla_alpha_b": nrm(ks[12], (L, GLA_HEADS * GLA_K_DIM), 0.1),
        "gla_out_norm": gain(ks[13], (L, GLA_V_DIM)),
        "fox_q_norm": gain(ks[14], (L, FOX_HEAD_DIM)),
        "fox_k_norm": gain(ks[15], (L, FOX_HEAD_DIM)),
        "fox_f_bias": 1.0 + nrm(ks[16], (L, FOX_HEADS), 0.5),
        "ffn2_norm": gain(ks[17], (L, D_MODEL)),
        "ffn2_w13": nrm(ks[18], (L, D_MODEL, 2 * D_FF), D_MODEL ** -0.5),
        "ffn2_w2": nrm(ks[19], (L, D_FF, D_MODEL), D_FF ** -0.5),
    }


def reference(x, ffn1_norm, ffn1_w13, ffn1_w2, mix_norm, w_in, w_out, diff_q_norm,
              diff_k_norm, diff_lambda, diff_out_norm, gla_alpha_w2, gla_alpha_b,
              gla_out_norm, fox_q_norm, fox_k_norm, fox_f_bias, ffn2_norm, ffn2_w13,
              ffn2_w2):
    for i in range(DEPTH):
        x = x + 0.5 * swiglu(rms_norm(x, ffn1_norm[i]), ffn1_w13[i], ffn1_w2[i])
        x = x + hybrid_mix(rms_norm(x, mix_norm[i]), w_in[i], w_out[i], diff_q_norm[i],
                           diff_k_norm[i], diff_lambda[i], diff_out_norm[i],
                           gla_alpha_w2[i], gla_alpha_b[i], gla_out_norm[i],
                           fox_q_norm[i], fox_k_norm[i], fox_f_bias[i], i)
        x = x + 0.5 * swiglu(rms_norm(x, ffn2_norm[i]), ffn2_w13[i], ffn2_w2[i])
    return x
```

```python
import math
from contextlib import ExitStack

import numpy as np
import concourse.bass as bass
import concourse.mybir as mybir
from concourse.bass_utils import run_bass_kernel_spmd

F32 = mybir.dt.float32
BF16 = mybir.dt.bfloat16
AF = mybir.ActivationFunctionType
ALU = mybir.AluOpType
AX = mybir.AxisListType

D = 1024
S = 2048
L = 4
DFF = 2752
NT = 4
TT = 512
NK = 8
EPS = 1e-6
N_CORES = 8
SEQ_PER_CORE = 2
NEG = -30000.0

GS = 384
FFN_GROUPS = [[(384 * g + 128 * c, 128) for c in range(3)] for g in range(7)] + [[(2688, 64)]]

G_D1, G_D2, G_F1, G_F2, G_G1, G_G2, G_G3 = 0, 768, 1280, 1792, 2060, 2588, 3100
NCOL = 3612
GW = {"d1": (G_D1, 768), "d2": (G_D2, 512), "f1": (G_F1, 512), "f2": (G_F2, 268),
      "g1": (G_G1, 528), "g2": (G_G2, 512), "g3": (G_G3, 512)}

SP_GQD, SP_GKD, SP_GDO, SP_GFQ, SP_GFK, SP_GGO, SP_FB, SP_GAB, SP_LAM, NSP = 0, 1, 2, 3, 4, 5, 6, 7, 11, 144
DE_GQD, DE_GDO, DE_GFQ, DE_NFB, DE_NGAB, DE_NLAM, NDE = 0, 1, 2, 3, 4, 8, 16
CF_EPS, CF_ONE, CF_HM, CF_MJ, CF_MASK, CF_TRI, CF_DBIAS, NCF = 0, 1, 2, 6, 16, 80, 144, 400
CB_SEL, CB_ID, CB_BD, CB_DIAG, NCB = 0, 16, 144, 272, 912


class Buf:
    __slots__ = ("name", "w", "r")

    def __init__(self, name):
        self.name = name
        self.w = None
        self.r = {}


class Eng:
    def __init__(self, name, handle, sem):
        self.name = name
        self.h = handle
        self.sem = sem
        self.cnt = 0
        self.waited = {}


class Chan:
    def __init__(self, name, sem):
        self.name = name
        self.sem = sem
        self.cnt = 0


class KB:
    def __init__(self, nc, es):
        self.nc = nc
        self.es = es
        self.engs = {}
        for name, h in (("pe", nc.tensor), ("act", nc.scalar), ("dve", nc.vector),
                        ("pool", nc.gpsimd), ("sp", nc.sync)):
            sem = es.enter_context(nc.semaphore("s_" + name))
            self.engs[name] = Eng(name, h, sem)
        self.semobj = {e.name: e.sem for e in self.engs.values()}
        self.chans = []

    def chan(self, name):
        sem = self.es.enter_context(self.nc.semaphore("c_" + name))
        c = Chan("c_" + name, sem)
        self.semobj[c.name] = sem
        self.chans.append(c)
        return c

    def sb(self, name, shape, dt):
        return self.es.enter_context(self.nc.sbuf_tensor(name, shape, dt))

    def _deps(self, eng, reads, writes):
        deps = {}

        def add(tok, same_ok):
            if tok is None:
                return
            k, v = tok
            if k == eng.name and not same_ok:
                return
            if deps.get(k, 0) < v:
                deps[k] = v

        for b in reads:
            add(b.w, eng.name != "pe")
        for b in writes:
            add(b.w, False)
            for k, v in b.r.items():
                add((k, v), False)
        return deps

    def _wait(self, eng, deps):
        for k, v in deps.items():
            if eng.waited.get(k, 0) < v:
                eng.h.wait_ge(self.semobj[k], v)
                eng.waited[k] = v

    def _record(self, tok, reads, writes):
        k, v = tok
        for b in writes:
            b.w = tok
            b.r = {}
        for b in reads:
            if b.r.get(k, 0) < v:
                b.r[k] = v

    def op(self, ename, fn, reads=(), writes=(), inc=True):
        eng = self.engs[ename]
        self._wait(eng, self._deps(eng, reads, writes))
        ins = fn(eng.h)
        if inc:
            ins.then_inc(eng.sem, 1)
            eng.cnt += 1
            tok = (eng.name, eng.cnt)
        else:
            tok = (eng.name, eng.cnt + 1)
        self._record(tok, reads, writes)
        return tok

    def dma(self, qname, chan, out, in_, reads=(), writes=()):
        eng = self.engs[qname]
        self._wait(eng, self._deps(eng, reads, writes))
        eng.h.dma_start(out=out, in_=in_).then_inc(chan.sem, 16)
        chan.cnt += 16
        tok = (chan.name, chan.cnt)
        self._record(tok, reads, writes)
        return tok

    def mm(self, out, lhsT, rhs, start, stop, reads=(), writes=(), inc=True, skip=False):
        if skip:
            fn = lambda h: h.matmul(out, lhsT=lhsT, rhs=rhs, start=start, stop=stop, skip_group_check=True)
        else:
            fn = lambda h: h.matmul(out, lhsT=lhsT, rhs=rhs, start=start, stop=stop)
        return self.op("pe", fn, reads, writes, inc)

    def mm_group(self, out, pairs, reads, writes):
        n = len(pairs)
        tok = None
        for i, (lt, rh) in enumerate(pairs):
            tok = self.mm(out, lt, rh, i == 0, i == n - 1,
                          reads if i == 0 else (), writes if i == 0 else (), inc=(i == n - 1))
        return tok

    def wait_all(self, ename, toks):
        eng = self.engs[ename]
        deps = {}
        for k, v in toks:
            if deps.get(k, 0) < v:
                deps[k] = v
        self._wait(eng, deps)

    def barrier(self):
        toks = [(e.name, e.cnt) for e in self.engs.values() if e.cnt > 0]
        toks += [(c.name, c.cnt) for c in self.chans if c.cnt > 0]
        for e in self.engs.values():
            self.wait_all(e.name, [t for t in toks if t[0] != e.name])


def mkap(view, off, dims):
    base = view.ap
    return bass.AP(view.tensor, view.offset + off, [list(base[0])] + [list(d) for d in dims])


class Prog:
    def __init__(self, n_layers=L, n_seq=SEQ_PER_CORE, phases=("ffn1", "diff", "fox", "gla", "ffn2")):
        self.n_layers = n_layers
        self.n_seq = n_seq
        self.phases = phases

    def build(self):
        nc = bass.Bass("TRN2", target_bir_lowering=False)
        self.nc = nc
        nl, ns = self.n_layers, self.n_seq
        dr = {}

        def din(name, shape):
            dr[name] = nc.dram_tensor(name, list(shape), F32, kind="ExternalInput").ap()

        din("xT", (ns, D, S))
        din("ffn1_w13", (L, D, 2 * DFF))
        din("ffn1_w2", (L, DFF, D))
        din("ffn2_w13", (L, D, 2 * DFF))
        din("ffn2_w2", (L, DFF, D))
        din("w_in_r", (L, D, NCOL))
        din("w_out", (L, D, D))
        din("aw2", (L, 16, 256))
        din("gn", (128, 3 * L * NK))
        din("spl", (L, 128, NSP))
        din("cstf", (128, NCF))
        din("cstb", (128, NCB))
        din("qapad", (128, 4 * TT))
        self.dr = dr
        self.outT = nc.dram_tensor("outT", [ns, D, S], F32, kind="ExternalOutput").ap()

        with ExitStack() as es:
            kb = KB(nc, es)
            self.kb = kb
            self.alloc(kb)
            self.setup()
            for s in range(ns):
                self.load_x(s)
                for l in range(nl):
                    self.layer_setup(l)
                    if "ffn1" in self.phases:
                        self.ffn(l, 0, dr["ffn1_w13"], dr["ffn1_w2"])
                    mix = [p for p in ("diff", "fox", "gla") if p in self.phases]
                    if mix:
                        kb.barrier()
                        self.norm_to_HT(l, 1)
                        if "diff" in mix:
                            self.diff_phase(l)
                            kb.barrier()
                        if "fox" in mix:
                            self.fox_phase(l)
                            kb.barrier()
                        if "gla" in mix:
                            self.gla_phase(l)
                            kb.barrier()
                    if "ffn2" in self.phases:
                        self.ffn(l, 2, dr["ffn2_w13"], dr["ffn2_w2"])
                self.store_x(s)
            kb.wait_all("sp", [(self.ch_out.name, self.ch_out.cnt)])
        return nc

    def alloc(self, kb):
        nc = self.nc
        self.XT = kb.sb("XT", [128, NK, S], F32)
        self.XTb = [[Buf(f"xt{k}_{t}") for t in range(NT)] for k in range(NK)]
        self.HT = kb.sb("HT", [128, NK, S], BF16)
        self.HTb = [[Buf(f"ht{k}_{t}") for t in range(NT)] for k in range(NK)]
        WP = kb.sb("WP", [128, 18432], BF16)
        MPB = kb.sb("MPB", [128, 23040], BF16)
        MPF = kb.sb("MPF", [128, 3616], F32)
        self.WP, self.MPB, self.MPF = WP, MPB, MPF

        def v3(pool, off, a, b):
            return pool[:, off:off + a * b].rearrange("p (a b) -> p a b", b=b)

        def v2(pool, off, n):
            return pool[:, off:off + n]

        self.WA = [v3(WP, i * 6144, NK, 2 * GS) for i in range(2)]
        self.WAb = [Buf(f"wa{i}") for i in range(2)]
        self.WB = [v3(WP, 12288 + i * 3072, 3, D) for i in range(2)]
        self.WBb = [Buf(f"wb{i}") for i in range(2)]
        self.ch_wa = [kb.chan(f"wa{i}") for i in range(2)]
        self.ch_wb = [kb.chan(f"wb{i}") for i in range(2)]
        self.SACT = [v2(MPB, i * 512, 512) for i in range(2)]
        self.SACTb = [Buf(f"sact{i}") for i in range(2)]
        self.ACTT = [v3(MPB, 1024 + i * 1536, 3, TT) for i in range(2)]
        self.ACTTb = [[Buf(f"actT{i}_{c}") for c in range(3)] for i in range(2)]
        self.Wd1 = v3(WP, 0, NK, 768)
        self.Wd2 = v3(WP, 6144, NK, 512)
        self.WOd = v3(WP, 10240, 2, D)
        self.Wf1 = v3(WP, 12288, NK, 512)
        self.WOf = v3(WP, 16384, 2, D)
        self.Wf2 = v3(WP, 0, NK, 268)
        self.Wg1 = v3(WP, 0, NK, 528)
        self.Wg2 = v3(WP, 4224, NK, 512)
        self.Wg3 = v3(WP, 8320, NK, 512)
        self.WOg = v3(WP, 12416, 4, D)
        self.Wb = {n: Buf("w_" + n) for n in ("d1", "d2", "od", "f1", "of", "f2", "g1", "g2", "g3", "og")}
        self.ch_wm = {n: kb.chan("wm_" + n) for n in self.Wb}
        self.ch_qp = kb.chan("qapad")
        self.KA = v3(MPB, 0, 4, S)
        self.KAb = [[Buf(f"ka{h}_{t}") for t in range(NT)] for h in range(4)]
        self.KApad = Buf("kapad")
        self.V1 = MPB[:, 8192:16384].rearrange("p (a b c) -> p a b c", a=16, b=4)
        self.V1b = [Buf(f"v1_{t}") for t in range(NT)]
        self.V1ones = Buf("v1ones")
        self.QA = v3(MPB, 16384, 4, TT)
        self.QAb = [Buf(f"qa{h}") for h in range(4)]
        self.QApad = Buf("qapad")
        self.PT = [v2(MPB, 18432 + i * 512, 512) for i in range(3)]
        self.PTb = [Buf(f"pt{i}") for i in range(3)]
        self.OTT = v3(MPB, 19968, 2, TT)
        self.OTTb = [Buf(f"ott{h}") for h in range(4)]
        self.FH, self.FM, self.FL, self.FS = [v2(MPB, 20992 + i * 512, 512) for i in range(4)]
        self.FHb, self.FMb, self.FLb, self.FSb = [Buf(n) for n in ("fh", "fm", "fl", "fs")]
        self.R1, self.T1, self.T2, self.AA = [v2(MPF, i * 512, 512) for i in range(4)]
        self.R1b, self.T1b, self.T2b, self.AAb = [Buf(n) for n in ("r1", "t1", "t2", "aa")]
        self.FE, self.FCS, self.FR1 = [v2(MPF, 512 + i * 512, 512) for i in range(3)]
        self.FLN, self.FR2 = self.FE, self.FR1
        self.FEb, self.FCSb, self.FR1b = [Buf(n) for n in ("fe", "fcs", "fr1")]
        self.FLNb, self.FR2b = self.FEb, self.FR1b
        self.FBIAS = v3(MPF, 2048, 16, 4)
        self.FBIASb = [Buf(f"fbias{t}") for t in range(NT)]
        self.FCAR = v2(MPF, 2112, 1)
        self.FCARb = Buf("fcar")
        self.QT = v3(MPB, 0, 4, TT)
        self.KT = v3(MPB, 2048, 4, TT)
        self.KDT = v3(MPB, 4096, 4, TT)
        self.QTb, self.KTb, self.KDTb = [[Buf(f"{n}{h}") for h in range(4)] for n in ("qt", "kt", "kdt")]
        self.KDEC = MPB[:, 6144:7168].rearrange("p (a b c) -> p a b c", a=4, b=4)
        self.KDECb = Buf("kdec")
        self.VG = v3(MPB, 7168, 4, TT)
        self.VGb = [Buf(f"vg{i}") for i in range(4)]
        self.SR = v3(MPB, 9216, 4, TT)
        self.SRb = [Buf(f"sr{h}") for h in range(4)]
        self.AT = v3(MPB, 11264, 8, 64)
        self.ATb = [Buf(f"at{i}") for i in range(8)]
        self.STB = v3(MPB, 11776, 4, 128)
        self.STBb = [Buf(f"stb{h}") for h in range(4)]
        self.GAT = v2(MPB, 12288, 512)
        self.GATb = Buf("gat")
        self.OTG = v3(MPB, 12800, 4, TT)
        self.OTGb = [Buf(f"otg{h}") for h in range(4)]
        self.LSP, self.BL, self.EB, self.ENB, self.DD, self.UU = [v2(MPF, i * 512, 512) for i in range(6)]
        self.LSPb, self.BLb, self.EBb, self.ENBb, self.DDb, self.UUb = [Buf(n) for n in ("lsp", "bl", "eb", "enb", "dd", "uu")]
        self.ST = v3(MPF, 3072, 4, 128)
        self.STb = [Buf(f"st{h}") for h in range(4)]
        self.EBL = v3(MPF, 3584, 4, 8)
        self.EBLb = [Buf(f"ebl{h}") for h in range(4)]
        self.SQ = [kb.sb(f"sq{i}", [128, TT], BF16) for i in range(2)]
        self.SQb = [Buf(f"sq{i}") for i in range(2)]
        self.LNT = kb.sb("lnt", [128, TT], F32)
        self.LNTb = Buf("lnt")
        self.RSTD = kb.sb("rstd", [128, TT], F32)
        self.RSTDb = Buf("rstd")
        self.GN = kb.sb("GN", [128, 3 * L * NK], F32)
        self.SPL = kb.sb("SPL", [128, NSP], F32)
        self.SPLb = Buf("spl")
        self.DER = kb.sb("DER", [128, NDE], F32)
        self.DERb = Buf("der")
        self.LTMP = kb.sb("LTMP", [128, 40], F32)
        self.LTMPb = Buf("ltmp")
        self.AW2 = kb.sb("AW2", [16, 256], BF16)
        self.AW2b = Buf("aw2")
        self.CF = kb.sb("CF", [128, NCF], F32)
        self.CB = kb.sb("CB", [128, NCB], BF16)
        self.ONES = kb.sb("ONES", [128, 128], BF16)
        self.MASK = kb.sb("MASK", [128, TT], BF16)
        self.cstb = Buf("cst")
        self.IDENT = self.CB[:, CB_ID:CB_ID + 128]
        self.BD32 = self.CB[:, CB_BD:CB_BD + 128]
        self.DIAGB = self.CB[:, CB_DIAG:CB_DIAG + 640].rearrange("p (a b) -> p a b", b=128)
        self.SEL = self.CB[:, CB_SEL:CB_SEL + 4]
        self.DBIAS = self.CF[:, CF_DBIAS:CF_DBIAS + 256].rearrange("p (h t k) -> p h t k", h=4, t=4)
        self.TRI = self.CF[:, CF_TRI:CF_TRI + 64]
        self.PS = [kb.es.enter_context(nc.psum_tensor(f"ps{i}", [128, TT], F32)) for i in range(8)]
        self.PSb = [Buf(f"ps{i}") for i in range(8)]
        self.PSsub = {1: [Buf(f"ps1_{i}") for i in range(4)],
                      2: [Buf(f"ps2_{i}") for i in range(8)],
                      3: [Buf(f"ps3_{i}") for i in range(8)]}
        self.ch_x = kb.chan("x")
        self.ch_out = kb.chan("out")
        self.ch_c = kb.chan("cst")
        self.ch_l = kb.chan("lay")
        self.ch_l2 = kb.chan("lay2")
        self.sq_i = 0
        self.gu_i = 0
        self.y_i = 0
        self.w_i = 0
        self.m_i = 0
        self.s_i = 0
        self.pt_i = 0
        self.cur_layer = -1

    def setup(self):
        kb = self.kb
        kb.dma("sp", self.ch_c, self.GN[:], self.dr["gn"][:, :], writes=[self.cstb])
        kb.dma("sp", self.ch_c, self.CF[:], self.dr["cstf"][:, :], writes=[self.cstb])
        kb.dma("pool", self.ch_c, self.CB[:], self.dr["cstb"][:, :], writes=[self.cstb])
        kb.op("pool", lambda h: h.memset(self.ONES[:], 1.0), writes=[self.cstb])
        kb.op("pool", lambda h: h.memset(self.MASK[:], 1.0), writes=[self.cstb])
        kb.op("pool", lambda h: h.memset(self.MASK[:].rearrange("p (c t) -> p c t", t=64)[:, :, 0:1], 0.0), writes=[self.cstb])

    def layer_setup(self, l):
        if self.cur_layer == l and self.n_layers == 1:
            return
        self.cur_layer = l
        kb = self.kb
        lam_init = 0.8 - 0.6 * math.exp(-0.3 * l)
        kb.dma("sp", self.ch_l, self.SPL[:], self.dr["spl"][l], writes=[self.SPLb])
        kb.dma("pool", self.ch_l2, self.AW2[:], self.dr["aw2"][l], writes=[self.AW2b])
        sp, de = self.SPL, self.DER
        rd, wr = [self.SPLb], [self.DERb]

        def ts(col_out, col_in, n, mul):
            kb.op("dve", lambda h: h.tensor_scalar(out=de[:, col_out:col_out + n], in0=sp[:, col_in:col_in + n],
                                                   scalar1=mul, scalar2=None, op0=ALU.mult), rd, wr)

        ts(DE_GQD, SP_GQD, 1, 32 ** -0.5)
        ts(DE_GDO, SP_GDO, 1, 1.0 - lam_init)
        ts(DE_GFQ, SP_GFQ, 1, 64 ** -0.5)
        ts(DE_NFB, SP_FB, 1, -1.0)
        ts(DE_NGAB, SP_GAB, 4, -1.0)
        lt = self.LTMP
        kb.op("dve", lambda h: h.tensor_tensor(out=lt[:, 0:32], in0=sp[:, SP_LAM:SP_LAM + 32],
                                               in1=sp[:, SP_LAM + 32:SP_LAM + 64], op=ALU.mult), rd, [self.LTMPb])
        kb.op("dve", lambda h: h.reduce_sum(out=lt[:, 32:33], in_=lt[:, 0:32], axis=AX.X), [self.LTMPb], [self.LTMPb])
        kb.op("dve", lambda h: h.tensor_tensor(out=lt[:, 0:32], in0=sp[:, SP_LAM + 64:SP_LAM + 96],
                                               in1=sp[:, SP_LAM + 96:SP_LAM + 128], op=ALU.mult), rd, [self.LTMPb])
        kb.op("dve", lambda h: h.reduce_sum(out=lt[:, 33:34], in_=lt[:, 0:32], axis=AX.X), [self.LTMPb], [self.LTMPb])
        kb.op("act", lambda h: h.activation(out=lt[:, 34:36], in_=lt[:, 32:34], func=AF.Exp), [self.LTMPb], [self.LTMPb])
        kb.op("dve", lambda h: h.scalar_tensor_tensor(out=lt[:, 36:37], in0=lt[:, 34:35], scalar=-1.0, in1=lt[:, 35:36],
                                                      op0=ALU.mult, op1=ALU.add), [self.LTMPb], [self.LTMPb])
        kb.op("dve", lambda h: h.tensor_scalar(out=de[:, DE_NLAM:DE_NLAM + 1], in0=lt[:, 36:37], scalar1=-lam_init,
                                               scalar2=None, op0=ALU.add), [self.LTMPb], wr)

    def load_x(self, s):
        kb = self.kb
        for k in range(NK):
            kb.dma("sp", self.ch_x, self.XT[:, k, :], self.dr["xT"][s, k * 128:(k + 1) * 128, :],
                   writes=[b for b in self.XTb[k]])
        tok = (self.ch_x.name, self.ch_x.cnt)
        for row in self.XTb:
            for b in row:
                b.w = tok

    def store_x(self, s):
        kb = self.kb
        for k in range(NK):
            kb.dma("sp", self.ch_out, self.outT[s, k * 128:(k + 1) * 128, :], self.XT[:, k, :],
                   reads=[b for b in self.XTb[k]])
        tok = (self.ch_out.name, self.ch_out.cnt)
        for row in self.XTb:
            for b in row:
                b.r[tok[0]] = tok[1]

    def misc_bank(self, banks=(0, 1)):
        j = banks[self.m_i % len(banks)]
        self.m_i += 1
        bufs = [self.PSb[j]] + self.PSsub.get(j, [])
        return self.PS[j], bufs

    def rstd_from(self, ps_ap, n, scale, psbufs):
        kb = self.kb
        kb.op("act", lambda h: h.activation(out=self.LNT[0:n, :], in_=ps_ap, func=AF.Ln,
                                            bias=self.CF[0:n, CF_EPS:CF_EPS + 1], scale=scale),
              reads=list(psbufs) + [self.cstb], writes=[self.LNTb])
        kb.op("act", lambda h: h.activation(out=self.RSTD[0:n, :], in_=self.LNT[0:n, :], func=AF.Exp, scale=-0.5),
              reads=[self.LNTb], writes=[self.RSTDb])

    def norm_to_HT(self, l, which):
        kb = self.kb
        gbase = (which * L + l) * NK
        psn, psnb = self.PS[6], self.PSb[6]
        for t in range(NT):
            ts = slice(t * TT, (t + 1) * TT)
            for k in range(NK):
                i = self.sq_i % 2
                self.sq_i += 1
                kb.op("act", lambda h, k=k, i=i: h.activation(out=self.SQ[i][:], in_=self.XT[:, k, ts], func=AF.Square),
                      reads=[self.XTb[k][t]], writes=[self.SQb[i]])
                kb.mm(psn[:], self.ONES[:], self.SQ[i][:], k == 0, k == NK - 1,
                      reads=[self.SQb[i], self.cstb], writes=[psnb] if k == 0 else [])
            psnb.w = ("pe", kb.engs["pe"].cnt)
            self.rstd_from(psn[:], 128, 1.0 / D, [psnb])
            for k in range(NK):
                kb.op("dve", lambda h, k=k: h.scalar_tensor_tensor(
                    out=self.HT[:, k, ts], in0=self.XT[:, k, ts], scalar=self.GN[:, gbase + k:gbase + k + 1],
                    in1=self.RSTD[:], op0=ALU.mult, op1=ALU.mult),
                    reads=[self.XTb[k][t], self.RSTDb, self.cstb], writes=[self.HTb[k][t]])

    def ffn_load(self, l, gi, w13, w2):
        kb = self.kb
        slot = self.w_i % 2
        self.w_i += 1
        chunks = FFN_GROUPS[gi]
        fo = chunks[0][0]
        width = sum(c[1] for c in chunks)
        wa, wb = self.WA[slot], self.WB[slot]
        src = w13[l].rearrange("(k p) n -> p k n", p=128)
        kb.dma("pool", self.ch_wa[slot], wa[:, :, 0:width], src[:, :, fo:fo + width], writes=[self.WAb[slot]])
        kb.dma("pool", self.ch_wa[slot], wa[:, :, GS:GS + width], src[:, :, DFF + fo:DFF + fo + width],
               writes=[self.WAb[slot]])
        if width >= 128:
            nch = width // 128
            src2 = w2[l, fo:fo + width, :].rearrange("(c p) n -> p c n", p=128)
            kb.dma("pool", self.ch_wb[slot], wb[:, 0:nch, :], src2, writes=[self.WBb[slot]])
        else:
            kb.dma("pool", self.ch_wb[slot], wb[0:width, 0, :], w2[l, fo:fo + width, :], writes=[self.WBb[slot]])
        return slot

    def ffn_p1(self, slot, gi, t, aslot):
        kb = self.kb
        ts = slice(t * TT, (t + 1) * TT)
        wa = self.WA[slot]
        for ci, (fo, fs) in enumerate(FFN_GROUPS[gi]):
            j = self.gu_i % 2
            self.gu_i += 1
            pg, pgb = self.PS[2 * j], self.PSb[2 * j]
            pu, pub = self.PS[2 * j + 1], self.PSb[2 * j + 1]
            hreads = [self.HTb[k][t] for k in range(NK)] + [self.WAb[slot]]
            kb.mm_group(pg[0:fs, :], [(wa[:, k, ci * 128:ci * 128 + fs], self.HT[:, k, ts]) for k in range(NK)],
                        hreads, [pgb])
            kb.mm_group(pu[0:fs, :], [(wa[:, k, GS + ci * 128:GS + ci * 128 + fs], self.HT[:, k, ts]) for k in range(NK)],
                        hreads, [pub])
            kb.op("act", lambda h, j=j, fs=fs, pg=pg: h.activation(out=self.SACT[j][0:fs, :], in_=pg[0:fs, :], func=AF.Silu),
                  reads=[pgb], writes=[self.SACTb[j]])
            kb.op("dve", lambda h, j=j, fs=fs, pu=pu, ci=ci: h.tensor_tensor(
                out=self.ACTT[aslot][0:fs, ci, :], in0=self.SACT[j][0:fs, :], in1=pu[0:fs, :], op=ALU.mult),
                reads=[self.SACTb[j], pub], writes=[self.ACTTb[aslot][ci]])

    def ffn_p2(self, slot, gi, t, aslot):
        kb = self.kb
        ts = slice(t * TT, (t + 1) * TT)
        wb = self.WB[slot]
        chunks = FFN_GROUPS[gi]
        for dc in range(NK):
            j = 4 + (self.y_i % 2)
            self.y_i += 1
            py, pyb = self.PS[j], self.PSb[j]
            kb.mm_group(py[:], [(wb[0:fs, ci, dc * 128:(dc + 1) * 128], self.ACTT[aslot][0:fs, ci, :])
                                for ci, (fo, fs) in enumerate(chunks)],
                        [self.WBb[slot]] + [self.ACTTb[aslot][ci] for ci in range(len(chunks))], [pyb])
            kb.op("dve", lambda h, dc=dc, py=py: h.scalar_tensor_tensor(
                out=self.XT[:, dc, ts], in0=py[:], scalar=0.5, in1=self.XT[:, dc, ts], op0=ALU.mult, op1=ALU.add),
                reads=[pyb, self.XTb[dc][t]], writes=[self.XTb[dc][t]])

    def ffn(self, l, which, w13, w2):
        self.norm_to_HT(l, which)
        ng = len(FFN_GROUPS)
        items = [(gi, t) for gi in range(ng) for t in range(NT)]
        slots = {}
        slots[0] = self.ffn_load(l, 0, w13, w2)
        slots[1] = self.ffn_load(l, 1, w13, w2)
        prev = None
        for idx, (gi, t) in enumerate(items):
            aslot = idx % 2
            self.ffn_p1(slots[gi], gi, t, aslot)
            if prev is not None:
                pgi, pt, pas = prev
                self.ffn_p2(slots[pgi], pgi, pt, pas)
                if pt == NT - 1 and pgi + 2 < ng:
                    slots[pgi + 2] = self.ffn_load(l, pgi + 2, w13, w2)
            prev = (gi, t, aslot)
        pgi, pt, pas = prev
        self.ffn_p2(slots[pgi], pgi, pt, pas)

    def wload(self, l, name, view):
        off, n = GW[name]
        src = self.dr["w_in_r"][l].rearrange("(k p) n -> p k n", p=128)[:, :, off:off + n]
        self.kb.dma("pool", self.ch_wm[name], view[:, :, 0:n], src, writes=[self.Wb[name]])

    def woload(self, l, name, view, r0, nch):
        src = self.dr["w_out"][l, r0:r0 + nch * 128, :].rearrange("(c p) n -> p c n", p=128)
        self.kb.dma("pool", self.ch_wm[name], view[:, 0:nch, :], src, writes=[self.Wb[name]])

    def inproj_fm(self, wview, wbuf, c0, m, t, banks=(0, 1)):
        ts = slice(t * TT, (t + 1) * TT)
        ps, pb = self.misc_bank(banks)
        self.kb.mm_group(ps[0:m, :], [(wview[:, k, c0:c0 + m], self.HT[:, k, ts]) for k in range(NK)],
                         [self.HTb[k][t] for k in range(NK)] + [wbuf], pb)
        return ps, pb

    def v_tokmajor(self, wview, wbuf, c0, n, t, dst_fn, dst_bufs, banks=(0, 1)):
        for tc in range(4):
            tok = slice(t * TT + tc * 128, t * TT + (tc + 1) * 128)
            ps, pb = self.misc_bank(banks)
            self.kb.mm_group(ps[:, 0:n], [(self.HT[:, k, tok], wview[:, k, c0:c0 + n]) for k in range(NK)],
                             [self.HTb[k][t] for k in range(NK)] + [wbuf], pb)
            dst_fn(tc, ps, pb)

    def wout_partial(self, wo, wob, ot, otbufs, nch, t, banks=(0, 1)):
        kb = self.kb
        ts = slice(t * TT, (t + 1) * TT)
        for dc in range(NK):
            ps, pb = self.misc_bank(banks)
            kb.mm_group(ps[:], [(wo[:, j, dc * 128:(dc + 1) * 128], ot[:, j, :]) for j in range(nch)],
                        [wob] + list(otbufs), pb)
            kb.op("dve", lambda h, dc=dc, ps=ps: h.tensor_tensor(out=self.XT[:, dc, ts], in0=ps[:], in1=self.XT[:, dc, ts],
                                                                 op=ALU.add),
                  reads=pb + [self.XTb[dc][t]], writes=[self.XTb[dc][t]])

    def v1_ap(self, kbk, h):
        return self.V1[:, kbk, h, :]

    def attn_core(self, h, t, maps, krows, bias_fn, diag_idx, obanks):
        kb = self.kb
        nkb = 4 * t + 4
        tiles = [(mi, kbk) for mi in range(len(maps)) for kbk in range(nkb)]
        pend = None
        for item in tiles + [None]:
            if item is not None:
                mi, kbk = item
                r0, nr = maps[mi]
                qlo = max(t * TT, kbk * 128)
                c0 = qlo - t * TT
                n = TT - c0
                diag = kbk * 128 >= t * TT
                sj = 2 + (self.s_i % 2)
                self.s_i += 1
                pss, pssb = self.PS[sj], [self.PSb[sj]] + self.PSsub[sj]
                kb.mm(pss[:, 0:n], self.KA[r0:r0 + nr, h, kbk * 128:(kbk + 1) * 128], self.QA[r0:r0 + nr, h, c0:TT],
                      True, not diag, reads=[self.KAb[h][kbk // 4], self.KApad, self.QAb[h], self.QApad],
                      writes=pssb, inc=not diag, skip=True)
                if diag:
                    kb.mm(pss[:, 0:128], self.IDENT, self.DIAGB[:, diag_idx, :], False, True,
                          reads=[self.cstb], writes=[], inc=True, skip=True)
                    for b in pssb:
                        b.w = ("pe", kb.engs["pe"].cnt)
                pi = self.pt_i % 3
                self.pt_i += 1
                bias_ap, bias_bufs = bias_fn(kbk)
                kb.op("act", lambda hh, pi=pi, n=n, pss=pss, bias_ap=bias_ap: hh.activation(
                    out=self.PT[pi][:, 0:n], in_=pss[:, 0:n], func=AF.Exp, bias=bias_ap, scale=1.0),
                    reads=pssb + bias_bufs, writes=[self.PTb[pi]])
                cur = (mi, kbk, pi, c0, n)
            if pend is not None:
                pmi, pkb, ppi, pc0, pn = pend
                ob = obanks[pmi]
                kb.mm(self.PS[ob][:, pc0:TT], self.v1_ap(pkb, h), self.PT[ppi][:, 0:pn], pkb == 0, pkb == nkb - 1,
                      reads=[self.V1b[pkb // 4], self.V1ones, self.PTb[ppi]],
                      writes=[self.PSb[ob]] if pkb == 0 else [], inc=True, skip=True)
                if pkb == nkb - 1:
                    self.PSb[ob].w = ("pe", kb.engs["pe"].cnt)
            pend = cur if item is not None else None

    def diff_phase(self, l):
        kb = self.kb
        self.wload(l, "d1", self.Wd1)
        self.wload(l, "d2", self.Wd2)
        self.woload(l, "od", self.WOd, 0, 2)
        kb.op("pool", lambda h: h.memset(self.KA[32:64, :, :], 0.0), writes=[self.KApad])
        kb.op("pool", lambda h: h.memset(self.KA[96:128, :, :], 0.0), writes=[self.KApad])
        kb.op("pool", lambda h: h.memset(self.KA[32:34, :, :], 1.0), writes=[self.KApad])
        kb.op("pool", lambda h: h.memset(self.KA[96:98, :, :], 1.0), writes=[self.KApad])
        kb.op("pool", lambda h: h.memset(self.V1[:, :, :, 64:128], 1.0), writes=[self.V1ones])
        kb.dma("pool", self.ch_qp, self.QA[:, :, :], self.dr["qapad"].rearrange("p (a b) -> p a b", b=TT),
               writes=[self.QApad] + self.QAb)
        for t in range(NT):
            ts = slice(t * TT, (t + 1) * TT)
            for h in range(4):
                for side in (0, 1):
                    wv, wb = (self.Wd1, self.Wb["d1"]) if side == 0 else (self.Wd2, self.Wb["d2"])
                    ps, pb = self.inproj_fm(wv, wb, h * 128, 128, t)
                    i = self.sq_i % 2
                    self.sq_i += 1
                    kb.op("act", lambda hh, i=i, ps=ps: hh.activation(out=self.SQ[i][:], in_=ps[:], func=AF.Square),
                          reads=pb, writes=[self.SQb[i]])
                    ps2, pb2 = self.misc_bank()
                    kb.mm(ps2[:], self.BD32, self.SQ[i][:], True, True, reads=[self.SQb[i], self.cstb], writes=pb2)
                    self.rstd_from(ps2[:], 128, 1.0 / 32, pb2)
                    for r0 in (0, 64):
                        if side == 0:
                            dst, dbufs = self.QA[r0:r0 + 32, h, :], [self.QAb[h]]
                            gcol, gb = self.DER[r0:r0 + 32, DE_GQD:DE_GQD + 1], self.DERb
                        else:
                            dst, dbufs = self.KA[r0:r0 + 32, h, ts], [self.KAb[h][t]]
                            gcol, gb = self.SPL[r0:r0 + 32, SP_GKD:SP_GKD + 1], self.SPLb
                        kb.op("dve", lambda hh, r0=r0, ps=ps, dst=dst, gcol=gcol: hh.scalar_tensor_tensor(
                            out=dst, in0=ps[r0:r0 + 32, :], scalar=gcol, in1=self.RSTD[r0:r0 + 32, :],
                            op0=ALU.mult, op1=ALU.mult), reads=pb + [self.RSTDb, gb], writes=dbufs)

            def vcopy(tc, ps, pb, t=t):
                kb.op("act", lambda hh: hh.activation(out=self.V1[:, 4 * t + tc, :, 0:64], in_=ps[:, 0:256].rearrange("p (a b) -> p a b", b=64), func=AF.Copy),
                      reads=pb, writes=[self.V1b[t]])
            self.v_tokmajor(self.Wd1, self.Wb["d1"], 512, 256, t, vcopy, None)
            for h in range(4):
                obanks = (4, 5) if h % 2 == 0 else (6, 7)
                self.attn_core(h, t, [(0, 64), (64, 64)], None,
                               lambda kbk, h=h, t=t: (self.DBIAS[:, h, t, kbk:kbk + 1], [self.cstb]), h, obanks)
                o0, o1 = self.PS[obanks[0]], self.PS[obanks[1]]
                ob0, ob1 = self.PSb[obanks[0]], self.PSb[obanks[1]]
                kb.op("dve", lambda hh: hh.reciprocal(out=self.R1[0:64, :], in_=o0[64:128, :]), [ob0], [self.R1b])
                kb.op("dve", lambda hh: hh.tensor_tensor(out=self.T1[0:64, :], in0=o0[0:64, :], in1=self.R1[0:64, :], op=ALU.mult),
                      [ob0, self.R1b], [self.T1b])
                kb.op("dve", lambda hh: hh.reciprocal(out=self.R1[0:64, :], in_=o1[64:128, :]), [ob1, self.T1b], [self.R1b])
                kb.op("dve", lambda hh: hh.tensor_tensor(out=self.T2[0:64, :], in0=o1[0:64, :], in1=self.R1[0:64, :], op=ALU.mult),
                      [ob1, self.R1b], [self.T2b])
                kb.op("dve", lambda hh: hh.scalar_tensor_tensor(out=self.AA[0:64, :], in0=self.T2[0:64, :],
                                                                scalar=self.DER[0:64, DE_NLAM:DE_NLAM + 1],
                                                                in1=self.T1[0:64, :], op0=ALU.mult, op1=ALU.add),
                      [self.T1b, self.T2b, self.DERb], [self.AAb])
                i = self.sq_i % 2
                self.sq_i += 1
                kb.op("act", lambda hh, i=i: hh.activation(out=self.SQ[i][0:64, :], in_=self.AA[0:64, :], func=AF.Square),
                      [self.AAb], [self.SQb[i]])
                ps2, pb2 = self.misc_bank()
                kb.mm(ps2[0:64, :], self.ONES[0:64, 0:64], self.SQ[i][0:64, :], True, True,
                      reads=[self.SQb[i], self.cstb], writes=pb2)
                self.rstd_from(ps2[0:64, :], 64, 1.0 / 64, pb2)
                hb = (h % 2) * 64
                kb.op("dve", lambda hh, hb=hb, h=h: hh.scalar_tensor_tensor(
                    out=self.OTT[hb:hb + 64, h // 2, :], in0=self.AA[0:64, :], scalar=self.DER[0:64, DE_GDO:DE_GDO + 1],
                    in1=self.RSTD[0:64, :], op0=ALU.mult, op1=ALU.mult),
                    [self.AAb, self.RSTDb, self.DERb], [self.OTTb[h]])
            self.wout_partial(self.WOd, self.Wb["od"], self.OTT, self.OTTb, 2, t)

    def fox_phase(self, l):
        kb = self.kb
        self.wload(l, "f1", self.Wf1)
        self.wload(l, "f2", self.Wf2)
        self.woload(l, "of", self.WOf, 768, 2)
        kb.op("pool", lambda h: h.memset(self.KA[64:128, :, :], 0.0), writes=[self.KApad])
        kb.op("pool", lambda h: h.memset(self.KA[64:76, :, :], 1.0), writes=[self.KApad])
        kb.op("pool", lambda h: h.memset(self.QA[64:128, :, :], 0.0), writes=[self.QApad] + self.QAb)
        kb.op("pool", lambda h: h.memset(self.V1[:, :, :, 64:128], 1.0), writes=[self.V1ones])
        one_col = self.CF[0:12, CF_ONE:CF_ONE + 1]
        for t in range(NT):
            ts = slice(t * TT, (t + 1) * TT)
            for h in range(4):
                for side in (0, 1):
                    if side == 0:
                        ps, pb = self.inproj_fm(self.Wf1, self.Wb["f1"], h * 64, 64, t)
                    else:
                        ps, pb = self.inproj_fm(self.Wf2, self.Wb["f2"], h * 64, 64, t)
                    i = self.sq_i % 2
                    self.sq_i += 1
                    kb.op("act", lambda hh, i=i, ps=ps: hh.activation(out=self.SQ[i][0:64, :], in_=ps[0:64, :], func=AF.Square),
                          reads=pb, writes=[self.SQb[i]])
                    ps2, pb2 = self.misc_bank()
                    kb.mm(ps2[0:64, :], self.ONES[0:64, 0:64], self.SQ[i][0:64, :], True, True,
                          reads=[self.SQb[i], self.cstb], writes=pb2)
                    self.rstd_from(ps2[0:64, :], 64, 1.0 / 64, pb2)
                    if side == 0:
                        dst, dbufs = self.QA[0:64, h, :], [self.QAb[h]]
                        gcol, gb = self.DER[0:64, DE_GFQ:DE_GFQ + 1], self.DERb
                    else:
                        dst, dbufs = self.KA[0:64, h, ts], [self.KAb[h][t]]
                        gcol, gb = self.SPL[0:64, SP_GFK:SP_GFK + 1], self.SPLb
                    kb.op("dve", lambda hh, ps=ps, dst=dst, gcol=gcol: hh.scalar_tensor_tensor(
                        out=dst, in0=ps[0:64, :], scalar=gcol, in1=self.RSTD[0:64, :], op0=ALU.mult, op1=ALU.mult),
                        reads=pb + [self.RSTDb, gb], writes=dbufs)

            def vcopy(tc, ps, pb, t=t):
                kb.op("act", lambda hh: hh.activation(out=self.V1[:, 4 * t + tc, :, 0:64], in_=ps[:, 0:256].rearrange("p (a b) -> p a b", b=64), func=AF.Copy),
                      reads=pb, writes=[self.V1b[t]])
            self.v_tokmajor(self.Wf1, self.Wb["f1"], 256, 256, t, vcopy, None)
            ps, pb = self.inproj_fm(self.Wf2, self.Wb["f2"], 256, 12, t)
            kb.op("act", lambda hh, ps=ps: hh.activation(out=self.FE[0:12, :], in_=ps[0:12, :], func=AF.Exp,
                                                         bias=self.DER[0:12, DE_NFB:DE_NFB + 1], scale=-1.0),
                  reads=pb + [self.DERb], writes=[self.FEb])
            kb.op("act", lambda hh: hh.activation(out=self.FLN[0:12, :], in_=self.FE[0:12, :], func=AF.Ln, bias=one_col, scale=1.0),
                  reads=[self.FEb, self.cstb], writes=[self.FLNb])
            init = 0.0 if t == 0 else self.FCAR[0:12, 0:1]
            kb.op("dve", lambda hh, init=init: hh.tensor_tensor_scan(
                out=self.FCS[0:12, :], data0=one_col.to_broadcast([12, TT]), data1=self.FLN[0:12, :], initial=init,
                op0=ALU.mult, op1=ALU.add), reads=[self.FLNb, self.FCARb, self.cstb], writes=[self.FCSb])
            kb.op("dve", lambda hh: hh.tensor_copy(out=self.FCAR[0:12, 0:1], in_=self.FCS[0:12, TT - 1:TT]),
                  reads=[self.FCSb], writes=[self.FCARb])
            kb.op("dve", lambda hh: hh.tensor_scalar(out=self.FH[0:12, :], in0=self.FCS[0:12, :], scalar1=-1.0, scalar2=None, op0=ALU.mult),
                  [self.FCSb], [self.FHb])
            kb.op("dve", lambda hh: hh.scalar_tensor_tensor(out=self.FR1[0:12, :], in0=self.FCS[0:12, :], scalar=-1.0,
                                                            in1=self.FH[0:12, :], op0=ALU.mult, op1=ALU.subtract),
                  [self.FCSb, self.FHb], [self.FR1b])
            kb.op("dve", lambda hh: hh.tensor_copy(out=self.FM[0:12, :], in_=self.FR1[0:12, :]), [self.FR1b], [self.FMb])
            kb.op("dve", lambda hh: hh.tensor_tensor(out=self.FR2[0:12, :], in0=self.FR1[0:12, :], in1=self.FM[0:12, :], op=ALU.subtract),
                  [self.FR1b, self.FMb], [self.FR2b])
            kb.op("dve", lambda hh: hh.tensor_copy(out=self.FL[0:12, :], in_=self.FR2[0:12, :]), [self.FR2b], [self.FLb])
            mj = lambda j: self.CF[0:12, CF_MJ + j:CF_MJ + j + 1]
            kb.op("dve", lambda hh: hh.tensor_scalar(out=self.FS[0:12, :], in0=self.FH[0:12, :], scalar1=mj(0), scalar2=None, op0=ALU.mult),
                  [self.FHb, self.cstb], [self.FSb])
            kb.op("dve", lambda hh: hh.scalar_tensor_tensor(out=self.FS[0:12, :], in0=self.FM[0:12, :], scalar=mj(1),
                                                            in1=self.FS[0:12, :], op0=ALU.mult, op1=ALU.add),
                  [self.FMb, self.FSb], [self.FSb])
            kb.op("dve", lambda hh: hh.scalar_tensor_tensor(out=self.FS[0:12, :], in0=self.FL[0:12, :], scalar=mj(2),
                                                            in1=self.FS[0:12, :], op0=ALU.mult, op1=ALU.add),
                  [self.FLb, self.FSb], [self.FSb])
            for h in range(4):
                kb.op("dve", lambda hh, h=h: hh.tensor_scalar(out=self.QA[64:76, h, :], in0=self.FS[0:12, :],
                                                              scalar1=self.CF[0:12, CF_HM + h:CF_HM + h + 1], scalar2=None, op0=ALU.mult),
                      [self.FSb, self.cstb], [self.QAb[h]])
            ps2, pb2 = self.misc_bank()
            for tc in range(4):
                kb.mm(ps2[:, tc * 4:(tc + 1) * 4], self.FS[0:12, tc * 128:(tc + 1) * 128], self.SEL[0:12, 0:4], True, True,
                      reads=[self.FSb, self.cstb], writes=pb2 if tc == 0 else [], inc=(tc == 3), skip=True)
            for b in pb2:
                b.w = ("pe", kb.engs["pe"].cnt)
            kb.op("dve", lambda hh, ps2=ps2, t=t: hh.tensor_copy(
                out=self.FBIAS[:, 4 * t:4 * t + 4, :], in_=ps2[:, 0:16].rearrange("p (a b) -> p a b", b=4)),
                reads=pb2, writes=[self.FBIASb[t]])
            for h in range(4):
                ob = 4 + h
                self.attn_core(h, t, [(0, 76)], None,
                               lambda kbk, h=h: (self.FBIAS[:, kbk, h:h + 1], [self.FBIASb[kbk // 4]]), 4, (ob,))
                o0, ob0 = self.PS[ob], self.PSb[ob]
                kb.op("dve", lambda hh, o0=o0: hh.reciprocal(out=self.R1[0:64, :], in_=o0[64:128, :]), [ob0], [self.R1b])
                hb = (h % 2) * 64
                kb.op("dve", lambda hh, o0=o0, hb=hb, h=h: hh.tensor_tensor(
                    out=self.OTT[hb:hb + 64, h // 2, :], in0=o0[0:64, :], in1=self.R1[0:64, :], op=ALU.mult),
                    [ob0, self.R1b], [self.OTTb[h]])
            self.wout_partial(self.WOf, self.Wb["of"], self.OTT, self.OTTb, 2, t)

    def gla_phase(self, l):
        kb = self.kb
        self.wload(l, "g1", self.Wg1)
        self.wload(l, "g2", self.Wg2)
        self.wload(l, "g3", self.Wg3)
        self.woload(l, "og", self.WOg, 256, 4)
        kb.op("pool", lambda h: h.memset(self.ST[0:64, :, :], 0.0), writes=self.STb)
        kb.op("pool", lambda h: h.memset(self.STB[0:64, :, :], 0.0), writes=self.STBb)
        mb = (0, 1, 2, 3)
        for t in range(NT):
            ts = slice(t * TT, (t + 1) * TT)
            ps, pb = self.inproj_fm(self.Wg1, self.Wb["g1"], 512, 16, t, mb)
            kb.op("act", lambda hh, ps=ps: hh.activation(out=self.GAT[0:16, :], in_=ps[0:16, :], func=AF.Copy),
                  reads=pb, writes=[self.GATb])
            for h in range(4):
                psz, pbz = self.misc_bank(mb)
                kb.mm(psz[0:64, :], self.AW2[0:16, h * 64:(h + 1) * 64], self.GAT[0:16, :], True, True,
                      reads=[self.AW2b, self.GATb], writes=pbz)
                kb.op("act", lambda hh, psz=psz, h=h: hh.activation(out=self.LSP[0:64, :], in_=psz[0:64, :], func=AF.Exp,
                                                                    bias=self.DER[0:64, DE_NGAB + h:DE_NGAB + h + 1], scale=-1.0),
                      reads=pbz + [self.DERb], writes=[self.LSPb])
                kb.op("act", lambda hh: hh.activation(out=self.LSP[0:64, :], in_=self.LSP[0:64, :], func=AF.Ln,
                                                      bias=self.CF[0:64, CF_ONE:CF_ONE + 1], scale=1.0),
                      reads=[self.LSPb, self.cstb], writes=[self.LSPb])
                kb.op("dve", lambda hh: hh.tensor_tensor_scan(
                    out=self.BL[0:64, :], data0=self.MASK[0:64, :],
                    data1=self.LSP[0:64, :], initial=0.0, op0=ALU.mult, op1=ALU.add),
                    reads=[self.LSPb, self.cstb], writes=[self.BLb])
                kb.op("act", lambda hh: hh.activation(out=self.EB[0:64, :], in_=self.BL[0:64, :], func=AF.Exp, scale=-1.0 / 16),
                      [self.BLb], [self.EBb])
                kb.op("act", lambda hh: hh.activation(out=self.ENB[0:64, :], in_=self.BL[0:64, :], func=AF.Exp, scale=1.0 / 16),
                      [self.BLb], [self.ENBb])
                bl3 = self.BL[0:64, :].rearrange("p (c t) -> p c t", t=64)
                kb.op("dve", lambda hh, bl3=bl3: hh.tensor_tensor(
                    out=self.DD[0:64, :].rearrange("p (c t) -> p c t", t=64), in0=bl3,
                    in1=bl3[:, :, 63:64].to_broadcast([64, 8, 64]), op=ALU.subtract), [self.BLb], [self.DDb])
                kb.op("act", lambda hh: hh.activation(out=self.DD[0:64, :], in_=self.DD[0:64, :], func=AF.Exp, scale=1.0 / 16),
                      [self.DDb], [self.DDb])
                kb.op("dve", lambda hh, h=h: hh.tensor_copy(
                    out=self.EBL[0:64, h, :], in_=self.EB[0:64, :].rearrange("p (c t) -> p c t", t=64)[:, :, 63]),
                    [self.EBb], [self.EBLb[h]])
                psq, pbq = self.inproj_fm(self.Wg1, self.Wb["g1"], h * 64, 64, t, mb)
                kb.op("dve", lambda hh, psq=psq, h=h: hh.scalar_tensor_tensor(
                    out=self.QT[0:64, h, :], in0=psq[0:64, :], scalar=0.125, in1=self.EB[0:64, :], op0=ALU.mult, op1=ALU.mult),
                    pbq + [self.EBb], [self.QTb[h]])
                psk, pbk = self.inproj_fm(self.Wg1, self.Wb["g1"], 256 + h * 64, 64, t, mb)
                kb.op("dve", lambda hh, psk=psk, h=h: hh.tensor_tensor(
                    out=self.KT[0:64, h, :], in0=psk[0:64, :], in1=self.ENB[0:64, :], op=ALU.mult),
                    pbk + [self.ENBb], [self.KTb[h]])
                kb.op("dve", lambda hh, psk=psk, h=h: hh.tensor_tensor(
                    out=self.KDT[0:64, h, :], in0=psk[0:64, :], in1=self.DD[0:64, :], op=ALU.mult),
                    pbk + [self.DDb], [self.KDTb[h]])
            pst, pbt = self.misc_bank(mb)
            pst_b = pst[:].bitcast(BF16).rearrange("p (a b c) -> p a b c", a=4, b=4)
            first = True
            for tc in range(4):
                for h in range(4):
                    kb.op("pe", lambda hh, tc=tc, h=h: hh.transpose(
                        out=pst_b[:, tc, h, :], in_=self.KDT[0:64, h, tc * 128:(tc + 1) * 128], identity=self.IDENT[0:64, 0:64]),
                        reads=[self.KDTb[h], self.cstb], writes=pbt if first else [], inc=(tc == 3 and h == 3))
                    first = False
            for b in pbt:
                b.w = ("pe", kb.engs["pe"].cnt)
            kb.op("act", lambda hh: hh.activation(out=self.MPB[:, 6144:7168], in_=pst[:].bitcast(BF16), func=AF.Copy), reads=pbt, writes=[self.KDECb])

            def vcopy(tc, ps, pb):
                kb.op("act", lambda hh: hh.activation(out=self.VG[:, tc, :], in_=ps[:, :], func=AF.Copy),
                      reads=pb, writes=[self.VGb[tc]])
            self.v_tokmajor(self.Wg2, self.Wb["g2"], 0, 512, t, vcopy, None, mb)
            for h in range(4):
                psr, pbr = self.inproj_fm(self.Wg3, self.Wb["g3"], h * 128, 128, t, mb)
                kb.op("act", lambda hh, psr=psr, h=h: hh.activation(out=self.SR[:, h, :], in_=psr[:], func=AF.Silu),
                      reads=pbr, writes=[self.SRb[h]])
            a_i = 0
            for c in range(8):
                base = (c % 2) * 64
                tc = c // 2
                cs = slice(c * 64, (c + 1) * 64)
                for h in range(4):
                    sl = a_i % 16
                    a_i += 1
                    bj = 2 + sl // 8
                    psa = self.PS[bj][0:64, (sl % 8) * 64:(sl % 8 + 1) * 64]
                    psab = self.PSsub[bj][sl % 8]
                    kb.mm(psa, self.KT[0:64, h, cs], self.QT[0:64, h, cs], True, True,
                          reads=[self.KTb[h], self.QTb[h]], writes=[psab, self.PSb[bj]])
                    ai = sl % 8
                    kb.op("dve", lambda hh, psa=psa, ai=ai, base=base: hh.tensor_tensor(
                        out=self.AT[base:base + 64, ai, :], in0=psa, in1=self.TRI[0:64, :], op=ALU.mult),
                        reads=[psab, self.cstb], writes=[self.ATb[ai]])
                    po, pob = self.PS[4 + h], self.PSb[4 + h]
                    kb.mm(po[:, cs], self.VG[base:base + 64, tc, h * 128:(h + 1) * 128], self.AT[base:base + 64, ai, :], True, False,
                          reads=[self.VGb[tc], self.ATb[ai]], writes=[pob] if c == 0 else [], inc=False, skip=True)
                    kb.mm(po[:, cs], self.STB[0:64, h, :], self.QT[0:64, h, cs], False, True,
                          reads=[self.STBb[h], self.QTb[h]], writes=[], inc=True, skip=True)
                    if c == 7:
                        pob.w = ("pe", kb.engs["pe"].cnt)
                    ksl = (c * 4 + h) % 4
                    pkv = self.PS[1][0:64, ksl * 128:(ksl + 1) * 128]
                    pkvb = self.PSsub[1][ksl]
                    kb.mm(pkv, self.KDEC[base:base + 64, tc, h, :], self.VG[base:base + 64, tc, h * 128:(h + 1) * 128], True, True,
                          reads=[self.KDECb, self.VGb[tc]], writes=[pkvb, self.PSb[1]])
                    kb.op("dve", lambda hh, h=h, c=c, pkv=pkv: hh.scalar_tensor_tensor(
                        out=self.ST[0:64, h, :], in0=self.ST[0:64, h, :], scalar=self.EBL[0:64, h, c:c + 1], in1=pkv,
                        op0=ALU.mult, op1=ALU.add), reads=[self.STb[h], self.EBLb[h], pkvb], writes=[self.STb[h]])
                    kb.op("act", lambda hh, h=h: hh.activation(out=self.STB[0:64, h, :], in_=self.ST[0:64, h, :], func=AF.Copy),
                          reads=[self.STb[h]], writes=[self.STBb[h]])
            for h in range(4):
                po, pob = self.PS[4 + h], self.PSb[4 + h]
                i = self.sq_i % 2
                self.sq_i += 1
                kb.op("act", lambda hh, i=i, po=po: hh.activation(out=self.SQ[i][:], in_=po[:], func=AF.Square),
                      reads=[pob], writes=[self.SQb[i]])
                ps2, pb2 = self.misc_bank((0,))
                kb.mm(ps2[:], self.ONES[:], self.SQ[i][:], True, True, reads=[self.SQb[i], self.cstb], writes=pb2)
                self.rstd_from(ps2[:], 128, 1.0 / 128, pb2)
                kb.op("dve", lambda hh, po=po: hh.scalar_tensor_tensor(
                    out=self.UU[:], in0=po[:], scalar=self.SPL[:, SP_GGO:SP_GGO + 1], in1=self.RSTD[:], op0=ALU.mult, op1=ALU.mult),
                    reads=[pob, self.RSTDb, self.SPLb], writes=[self.UUb])
                kb.op("dve", lambda hh, h=h: hh.tensor_tensor(out=self.OTG[:, h, :], in0=self.UU[:], in1=self.SR[:, h, :], op=ALU.mult),
                      reads=[self.UUb, self.SRb[h]], writes=[self.OTGb[h]])
            self.wout_partial(self.WOg, self.Wb["og"], self.OTG, self.OTGb, 4, t, (0,))


IN_OFF = {"d_q": 0, "d_k": 256, "d_v": 512, "g_q": 768, "g_k": 1024, "g_v": 1280, "g_r": 1792, "g_a": 2304,
          "f_q": 2320, "f_k": 2576, "f_v": 2832, "f_f": 3088}


def host_w_in(w_in):
    w_in = np.asarray(w_in, dtype=np.float32)
    out = np.zeros((L, D, NCOL), np.float32)

    def put(dst, src, n):
        out[:, :, dst:dst + n] = w_in[:, :, src:src + n]

    for h in range(4):
        for m in range(2):
            put(G_D1 + h * 128 + m * 64, IN_OFF["d_q"] + h * 64 + m * 32, 32)
            put(G_D2 + h * 128 + m * 64, IN_OFF["d_k"] + h * 64 + m * 32, 32)
    put(G_D1 + 512, IN_OFF["d_v"], 256)
    put(G_F1, IN_OFF["f_q"], 256)
    put(G_F1 + 256, IN_OFF["f_v"], 256)
    put(G_F2, IN_OFF["f_k"], 256)
    for j in range(3):
        put(G_F2 + 256 + 4 * j, IN_OFF["f_f"], 4)
    put(G_G1, IN_OFF["g_q"], 256)
    put(G_G1 + 256, IN_OFF["g_k"], 256)
    put(G_G1 + 512, IN_OFF["g_a"], 16)
    put(G_G2, IN_OFF["g_v"], 512)
    put(G_G3, IN_OFF["g_r"], 512)
    return out


def host_gn(ffn1_norm, mix_norm, ffn2_norm):
    arr = np.stack([ffn1_norm, mix_norm, ffn2_norm], axis=0)
    arr = arr.reshape(3, L, NK, 128).transpose(3, 0, 1, 2).reshape(128, 3 * L * NK)
    return np.ascontiguousarray(arr, dtype=np.float32)


def host_spl(diff_q_norm, diff_k_norm, diff_out_norm, fox_q_norm, fox_k_norm, gla_out_norm, fox_f_bias,
             gla_alpha_b, diff_lambda):
    spl = np.zeros((L, 128, NSP), np.float32)
    p = np.arange(128)
    for l in range(L):
        spl[l, :, SP_GQD] = diff_q_norm[l][p % 32]
        spl[l, :, SP_GKD] = diff_k_norm[l][p % 32]
        spl[l, :, SP_GDO] = diff_out_norm[l][p % 64]
        spl[l, :, SP_GFQ] = fox_q_norm[l][p % 64]
        spl[l, :, SP_GFK] = fox_k_norm[l][p % 64]
        spl[l, :, SP_GGO] = gla_out_norm[l][p]
        spl[l, :, SP_FB] = fox_f_bias[l][p % 4]
        for h in range(4):
            spl[l, :, SP_GAB + h] = gla_alpha_b[l][h * 64 + p % 64]
        spl[l, :, SP_LAM:SP_LAM + 128] = np.asarray(diff_lambda[l]).reshape(1, 128)
    return spl


def host_consts():
    slopes = [2.0 ** (-8.0 * (h + 1) / 4) for h in range(4)]
    p = np.arange(128)
    cf = np.zeros((128, NCF), np.float32)
    cf[:, CF_EPS] = EPS
    cf[:, CF_ONE] = 1.0
    for h in range(4):
        cf[:, CF_HM + h] = ((p % 4) == h) & (p < 12)
    for j in range(3):
        cf[:, CF_MJ + j] = ((p // 4) == j) & (p < 12)
    cf[:, CF_MASK:CF_MASK + 64] = 1.0
    cf[:, CF_MASK] = 0.0
    s_ = np.arange(64)
    tri = (s_[:, None] <= s_[None, :]).astype(np.float32)
    cf[0:64, CF_TRI:CF_TRI + 64] = tri
    cf[64:128, CF_TRI:CF_TRI + 64] = tri
    db = np.zeros((128, 4, 4, 16), np.float32)
    for h in range(4):
        for t in range(4):
            for kbk in range(16):
                db[:, h, t, kbk] = slopes[h] * (128 * kbk + p - 512 * t)
    cf[:, CF_DBIAS:CF_DBIAS + 256] = db.reshape(128, 256)
    cb = np.zeros((128, NCB), np.float32)
    for h in range(4):
        cb[:, CB_SEL + h] = -1.0 * (((p % 4) == h) & (p < 12))
    cb[:, CB_ID:CB_ID + 128] = np.eye(128, dtype=np.float32)
    bd = np.zeros((128, 128), np.float32)
    bd[0:32, 0:32] = 1.0
    bd[64:96, 64:96] = 1.0
    cb[:, CB_BD:CB_BD + 128] = bd
    j = np.arange(128)
    diag = np.zeros((128, 5, 128), np.float32)
    allowed = (p[:, None] // 64) <= (j[None, :] // 64)
    fut = p[:, None] > j[None, :]
    for h in range(4):
        m = np.where(fut, -2.0 * slopes[h] * (p[:, None] - j[None, :]), 0.0)
        diag[:, h, :] = np.where(allowed, m, NEG)
    diag[:, 4, :] = np.where(p[:, None] <= j[None, :], 0.0, NEG)
    cb[:, CB_DIAG:CB_DIAG + 640] = diag.reshape(128, 640)
    qp = np.zeros((128, 4, TT), np.float32)
    jj = np.arange(TT)
    for h in range(4):
        for r in (32, 96):
            qp[r, h, :] = -slopes[h] * 128.0 * (jj // 128)
            qp[r + 1, h, :] = -slopes[h] * (jj % 128)
    return cf, cb, qp.reshape(128, 4 * TT)


def kernel(x, ffn1_norm, ffn1_w13, ffn1_w2, mix_norm, w_in, w_out, diff_q_norm,
           diff_k_norm, diff_lambda, diff_out_norm, gla_alpha_w2, gla_alpha_b,
           gla_out_norm, fox_q_norm, fox_k_norm, fox_f_bias, ffn2_norm, ffn2_w13,
           ffn2_w2, _prog=None, _n_cores=N_CORES):
    x = np.asarray(x, dtype=np.float32)
    prog = _prog or Prog()
    nc = prog.build()
    ns = prog.n_seq
    f = lambda a: np.ascontiguousarray(np.asarray(a), dtype=np.float32)
    xT = np.ascontiguousarray(x.transpose(0, 2, 1))
    cf, cb, qp = host_consts()
    shared = {
        "ffn1_w13": f(ffn1_w13), "ffn1_w2": f(ffn1_w2), "ffn2_w13": f(ffn2_w13), "ffn2_w2": f(ffn2_w2),
        "w_in_r": host_w_in(w_in), "w_out": f(w_out), "aw2": f(gla_alpha_w2),
        "gn": host_gn(f(ffn1_norm), f(mix_norm), f(ffn2_norm)),
        "spl": host_spl(f(diff_q_norm), f(diff_k_norm), f(diff_out_norm), f(fox_q_norm), f(fox_k_norm),
                        f(gla_out_norm), f(fox_f_bias), f(gla_alpha_b), f(diff_lambda)),
        "cstf": cf, "cstb": cb, "qapad": qp,
    }
    in_maps = []
    for c in range(_n_cores):
        m = dict(shared)
        m["xT"] = xT[c * ns:(c + 1) * ns]
        in_maps.append(m)
    res = run_bass_kernel_spmd(nc, in_maps, core_ids=list(range(_n_cores)))
    outT = np.concatenate([r["outT"] for r in res.results], axis=0)
    return np.ascontiguousarray(outT.transpose(0, 2, 1))
```

```python
import math
from contextlib import ExitStack

import numpy as np
import concourse.bass as bass
import concourse.mybir as mybir
from concourse.bass_utils import run_bass_kernel_spmd

F32 = mybir.dt.float32
BF16 = mybir.dt.bfloat16
AF = mybir.ActivationFunctionType
ALU = mybir.AluOpType
AX = mybir.AxisListType

D = 1024
S = 2048
L = 4
DFF = 2752
NT = 4
TT = 512
NK = 8
EPS = 1e-6
N_CORES = 8
SEQ_PER_CORE = 2
NEG = -30000.0

GS = 384
FFN_GROUPS = [[(384 * g + 128 * c, 128) for c in range(3)] for g in range(7)] + [[(2688, 64)]]

G_D1, G_F1, G_F2, G_G1, G_G2, G_G3 = 0, 768, 1536, 1548, 2076, 2588
NCOL = 3100
GW = {"d1": (G_D1, 768), "f1": (G_F1, 768), "f2": (G_F2, 12),
      "g1": (G_G1, 528), "g2": (G_G2, 512), "g3": (G_G3, 512)}

SP_GQD, SP_GKD, SP_GDO, SP_GFQ, SP_GFK, SP_GGO, SP_FB, SP_GAB, SP_LAM, NSP = 0, 1, 2, 3, 4, 5, 6, 7, 11, 144
DE_GQD, DE_GDO, DE_GFQ, DE_NFB, DE_NGAB, DE_NLAM, DE_GD, DE_GF, NDE = 0, 1, 2, 3, 4, 8, 9, 10, 16
CF_EPS, CF_ONE, CF_HM, CF_MJ, CF_MASK, CF_TRI, CF_DBIAS, NCF = 0, 1, 2, 6, 16, 80, 144, 400
CB_SEL, CB_ID, CB_BD, CB_DIAG, CB_BD64, NCB = 0, 16, 144, 272, 912, 1040


class Buf:
    __slots__ = ("name", "w", "r")

    def __init__(self, name):
        self.name = name
        self.w = None
        self.r = {}


class Eng:
    def __init__(self, name, handle, sem):
        self.name = name
        self.h = handle
        self.sem = sem
        self.cnt = 0
        self.waited = {}


class Chan:
    def __init__(self, name, sem):
        self.name = name
        self.sem = sem
        self.cnt = 0


class KB:
    def __init__(self, nc, es):
        self.nc = nc
        self.es = es
        self.engs = {}
        for name, h in (("pe", nc.tensor), ("act", nc.scalar), ("dve", nc.vector),
                        ("pool", nc.gpsimd), ("sp", nc.sync)):
            sem = es.enter_context(nc.semaphore("s_" + name))
            self.engs[name] = Eng(name, h, sem)
        self.semobj = {e.name: e.sem for e in self.engs.values()}
        self.chans = []

    def chan(self, name):
        sem = self.es.enter_context(self.nc.semaphore("c_" + name))
        c = Chan("c_" + name, sem)
        self.semobj[c.name] = sem
        self.chans.append(c)
        return c

    def sb(self, name, shape, dt):
        return self.es.enter_context(self.nc.sbuf_tensor(name, shape, dt))

    def _deps(self, eng, reads, writes):
        deps = {}

        def add(tok, same_ok):
            if tok is None:
                return
            k, v = tok
            if k == eng.name and not same_ok:
                return
            if deps.get(k, 0) < v:
                deps[k] = v

        for b in reads:
            add(b.w, eng.name != "pe")
        for b in writes:
            add(b.w, False)
            for k, v in b.r.items():
                add((k, v), False)
        return deps

    def _wait(self, eng, deps):
        for k, v in deps.items():
            if eng.waited.get(k, 0) < v:
                eng.h.wait_ge(self.semobj[k], v)
                eng.waited[k] = v

    def _record(self, tok, reads, writes):
        k, v = tok
        for b in writes:
            b.w = tok
            b.r = {}
        for b in reads:
            if b.r.get(k, 0) < v:
                b.r[k] = v

    def op(self, ename, fn, reads=(), writes=(), inc=True):
        eng = self.engs[ename]
        self._wait(eng, self._deps(eng, reads, writes))
        ins = fn(eng.h)
        if inc:
            ins.then_inc(eng.sem, 1)
            eng.cnt += 1
            tok = (eng.name, eng.cnt)
        else:
            tok = (eng.name, eng.cnt + 1)
        self._record(tok, reads, writes)
        return tok

    def dma(self, qname, chan, out, in_, reads=(), writes=()):
        eng = self.engs[qname]
        self._wait(eng, self._deps(eng, reads, writes))
        eng.h.dma_start(out=out, in_=in_).then_inc(chan.sem, 16)
        chan.cnt += 16
        tok = (chan.name, chan.cnt)
        self._record(tok, reads, writes)
        return tok

    def mm(self, out, lhsT, rhs, start, stop, reads=(), writes=(), inc=True, skip=False):
        if skip:
            fn = lambda h: h.matmul(out, lhsT=lhsT, rhs=rhs, start=start, stop=stop, skip_group_check=True)
        else:
            fn = lambda h: h.matmul(out, lhsT=lhsT, rhs=rhs, start=start, stop=stop)
        return self.op("pe", fn, reads, writes, inc)

    def mm_group(self, out, pairs, reads, writes):
        n = len(pairs)
        tok = None
        for i, (lt, rh) in enumerate(pairs):
            tok = self.mm(out, lt, rh, i == 0, i == n - 1,
                          reads if i == 0 else (), writes if i == 0 else (), inc=(i == n - 1))
        return tok

    def wait_all(self, ename, toks):
        eng = self.engs[ename]
        deps = {}
        for k, v in toks:
            if deps.get(k, 0) < v:
                deps[k] = v
        self._wait(eng, deps)

    def barrier(self):
        toks = [(e.name, e.cnt) for e in self.engs.values() if e.cnt > 0]
        toks += [(c.name, c.cnt) for c in self.chans if c.cnt > 0]
        for e in self.engs.values():
            self.wait_all(e.name, [t for t in toks if t[0] != e.name])


def mkap(view, off, dims):
    base = view.ap
    return bass.AP(view.tensor, view.offset + off, [list(base[0])] + [list(d) for d in dims])


class Prog:
    def __init__(self, n_layers=L, n_seq=SEQ_PER_CORE, phases=("ffn1", "diff", "fox", "gla", "ffn2")):
        self.n_layers = n_layers
        self.n_seq = n_seq
        self.phases = phases

    def build(self):
        nc = bass.Bass("TRN2", target_bir_lowering=False)
        self.nc = nc
        nl, ns = self.n_layers, self.n_seq
        dr = {}

        def din(name, shape):
            dr[name] = nc.dram_tensor(name, list(shape), F32, kind="ExternalInput").ap()

        din("xT", (ns, D, S))
        din("ffn1_w13", (L, D, 2 * DFF))
        din("ffn1_w2", (L, DFF, D))
        din("ffn2_w13", (L, D, 2 * DFF))
        din("ffn2_w2", (L, DFF, D))
        din("w_in_r", (L, D, NCOL))
        din("w_out", (L, D, D))
        din("aw2", (L, 16, 256))
        din("gn", (128, 3 * L * NK))
        din("spl", (L, 128, NSP))
        din("cstf", (128, NCF))
        din("cstb", (128, NCB))
        din("qapad", (2, 128, 4 * TT))
        self.dr = dr
        self.outT = nc.dram_tensor("outT", [ns, D, S], F32, kind="ExternalOutput").ap()

        with ExitStack() as es:
            kb = KB(nc, es)
            self.kb = kb
            self.alloc(kb)
            self.setup()
            for s in range(ns):
                self.load_x(s)
                for l in range(nl):
                    self.layer_setup(l)
                    if "ffn1" in self.phases:
                        self.ffn(l, 0, dr["ffn1_w13"], dr["ffn1_w2"])
                    mix = [p for p in ("diff", "fox", "gla") if p in self.phases]
                    if mix:
                        kb.barrier()
                        self.norm_to_HT(l, 1)
                        if "diff" in mix:
                            self.diff_phase(l)
                            kb.barrier()
                        if "fox" in mix:
                            self.fox_phase(l)
                            kb.barrier()
                        if "gla" in mix:
                            self.gla_phase(l)
                            kb.barrier()
                    if "ffn2" in self.phases:
                        self.ffn(l, 2, dr["ffn2_w13"], dr["ffn2_w2"])
                self.store_x(s)
            kb.wait_all("sp", [(self.ch_out.name, self.ch_out.cnt)])
        return nc

    def alloc(self, kb):
        nc = self.nc
        self.XT = kb.sb("XT", [128, NK, S], F32)
        self.XTb = [[Buf(f"xt{k}_{t}") for t in range(NT)] for k in range(NK)]
        self.HT = kb.sb("HT", [128, NK, S], BF16)
        self.HTb = [[Buf(f"ht{k}_{t}") for t in range(NT)] for k in range(NK)]
        WP = kb.sb("WP", [128, 18432], BF16)
        MPB = kb.sb("MPB", [128, 23040], BF16)
        MPF = kb.sb("MPF", [128, 3648], F32)
        self.WP, self.MPB, self.MPF = WP, MPB, MPF

        def v3(pool, off, a, b):
            return pool[:, off:off + a * b].rearrange("p (a b) -> p a b", b=b)

        def v2(pool, off, n):
            return pool[:, off:off + n]

        self.WA = [v3(WP, i * 6144, NK, 2 * GS) for i in range(2)]
        self.WAb = [Buf(f"wa{i}") for i in range(2)]
        self.WB = [v3(WP, 12288 + i * 3072, 3, D) for i in range(2)]
        self.WBb = [Buf(f"wb{i}") for i in range(2)]
        self.ch_wa = [kb.chan(f"wa{i}") for i in range(2)]
        self.ch_wb = [kb.chan(f"wb{i}") for i in range(2)]
        self.SACT = [v2(MPB, i * 512, 512) for i in range(2)]
        self.SACTb = [Buf(f"sact{i}") for i in range(2)]
        self.ACTT = [v3(MPB, 1024 + i * 1536, 3, TT) for i in range(2)]
        self.ACTTb = [[Buf(f"actT{i}_{c}") for c in range(3)] for i in range(2)]
        self.Wd1 = v3(WP, 0, NK, 768)
        self.WOd = v3(WP, 6144, 2, D)
        self.Wf1 = v3(WP, 8192, NK, 768)
        self.Wf2 = v3(WP, 14336, NK, 12)
        self.WOf = v3(WP, 14432, 2, D)
        self.Wg1 = v3(WP, 0, NK, 528)
        self.Wg2 = v3(WP, 4224, NK, 512)
        self.Wg3 = v3(WP, 8320, NK, 512)
        self.WOg = v3(WP, 12416, 4, D)
        self.Wb = {n: Buf("w_" + n) for n in ("d1", "od", "f1", "of", "f2", "g1", "g2", "g3", "og")}
        self.ch_wm = {n: kb.chan("wm_" + n) for n in self.Wb}
        self.ch_qp = kb.chan("qapad")
        self.KA = v3(MPB, 0, 4, S)
        self.KAb = [[Buf(f"ka{h}_{t}") for t in range(NT)] for h in range(4)]
        self.KApad = Buf("kapad")
        self.V1 = MPB[:, 8192:16384].rearrange("p (a b c) -> p a b c", a=16, b=4)
        self.V1b = [Buf(f"v1_{t}") for t in range(NT)]
        self.V1ones = Buf("v1ones")
        self.QA = v3(MPB, 16384, 4, TT)
        self.QAb = [Buf(f"qa{h}") for h in range(4)]
        self.QA1 = v3(MPB, 20992, 4, TT)
        self.QApad = Buf("qapad")
        self.PT = [v2(MPB, 18432 + i * 512, 512) for i in range(3)]
        self.PTb = [Buf(f"pt{i}") for i in range(3)]
        self.OTT = v3(MPB, 19968, 2, TT)
        self.OTTb = [Buf(f"ott{h}") for h in range(4)]
        self.FH, self.FM, self.FL, self.FS = [v2(MPB, 20992 + i * 512, 512) for i in range(4)]
        self.FHb, self.FMb, self.FLb, self.FSb = [Buf(n) for n in ("fh", "fm", "fl", "fs")]
        self.R1, self.T1, self.T2, self.AA = [v2(MPF, i * 512, 512) for i in range(4)]
        self.R1b, self.T1b, self.T2b, self.AAb = [Buf(n) for n in ("r1", "t1", "t2", "aa")]
        self.FE, self.FCS, self.FR1 = [v2(MPF, 512 + i * 512, 512) for i in range(3)]
        self.FLN, self.FR2 = self.FE, self.FR1
        self.FEb, self.FCSb, self.FR1b = [Buf(n) for n in ("fe", "fcs", "fr1")]
        self.FLNb, self.FR2b = self.FEb, self.FR1b
        self.FBIAS = v3(MPF, 2048, 16, 4)
        self.FBIASb = [Buf(f"fbias{t}") for t in range(NT)]
        self.FCAR = v2(MPF, 2112, 1)
        self.FCARb = Buf("fcar")
        self.gQT = [v3(MPB, 0, 4, TT), v3(MPB, 13312, 4, TT)]
        self.gKT = [v3(MPB, 2048, 4, TT), v3(MPB, 15360, 4, TT)]
        self.KDT = v2(MPB, 4096, 512)
        self.KDTb = Buf("kdt")
        self.gKDEC = [MPB[:, o:o + 1024].rearrange("p (a b c) -> p a b c", a=4, b=4) for o in (4608, 17408)]
        self.gVG = [v3(MPB, 5632, 4, TT), v3(MPB, 18432, 4, TT)]
        self.gSR = [v3(MPB, 7680, 4, TT), v3(MPB, 20480, 4, TT)]
        self.gQTb = [[Buf(f"qt{p}{h}") for h in range(4)] for p in range(2)]
        self.gKTb = [[Buf(f"kt{p}{h}") for h in range(4)] for p in range(2)]
        self.gKDECb = [Buf(f"kdec{p}") for p in range(2)]
        self.gVGb = [[Buf(f"vg{p}{i}") for i in range(4)] for p in range(2)]
        self.gSRb = [[Buf(f"sr{p}{h}") for h in range(4)] for p in range(2)]
        self.AT = v3(MPB, 9728, 8, 64)
        self.ATb = [Buf(f"at{i}") for i in range(8)]
        self.STB = v3(MPB, 10240, 4, 128)
        self.STBb = [Buf(f"stb{h}") for h in range(4)]
        self.GAT = v2(MPB, 10752, 512)
        self.GATb = Buf("gat")
        self.OTG = v3(MPB, 11264, 4, TT)
        self.OTGb = [Buf(f"otg{h}") for h in range(4)]
        self.LSP, self.BL, self.EB, self.ENB, self.DD, self.UU = [v2(MPF, i * 512, 512) for i in range(6)]
        self.LSPb, self.BLb, self.EBb, self.ENBb, self.DDb, self.UUb = [Buf(n) for n in ("lsp", "bl", "eb", "enb", "dd", "uu")]
        self.ST = v3(MPF, 3072, 4, 128)
        self.STb = [Buf(f"st{h}") for h in range(4)]
        self.gEBL = [v3(MPF, 3584, 4, 8), v3(MPF, 3616, 4, 8)]
        self.gEBLb = [[Buf(f"ebl{p}{h}") for h in range(4)] for p in range(2)]
        self.SQ = [kb.sb(f"sq{i}", [128, TT], BF16) for i in range(2)]
        self.SQb = [Buf(f"sq{i}") for i in range(2)]
        self.LNT = kb.sb("lnt", [128, TT], F32)
        self.LNTb = Buf("lnt")
        self.RSTD = kb.sb("rstd", [128, TT], F32)
        self.RSTDb = Buf("rstd")
        self.GN = kb.sb("GN", [128, 3 * L * NK], F32)
        self.SPL = kb.sb("SPL", [128, NSP], F32)
        self.SPLb = Buf("spl")
        self.DER = kb.sb("DER", [128, NDE], F32)
        self.DERb = Buf("der")
        self.LTMP = kb.sb("LTMP", [128, 40], F32)
        self.LTMPb = Buf("ltmp")
        self.AW2 = kb.sb("AW2", [16, 256], BF16)
        self.AW2b = Buf("aw2")
        self.CF = kb.sb("CF", [128, NCF], F32)
        self.CB = kb.sb("CB", [128, NCB], BF16)
        self.ONES = kb.sb("ONES", [128, 128], BF16)
        self.MASK = kb.sb("MASK", [128, TT], BF16)
        self.cstb = Buf("cst")
        self.IDENT = self.CB[:, CB_ID:CB_ID + 128]
        self.BD32 = self.CB[:, CB_BD:CB_BD + 128]
        self.BD64 = self.CB[:, CB_BD64:CB_BD64 + 128]
        self.DIAGB = self.CB[:, CB_DIAG:CB_DIAG + 640].rearrange("p (a b) -> p a b", b=128)
        self.SEL = self.CB[:, CB_SEL:CB_SEL + 4]
        self.DBIAS = self.CF[:, CF_DBIAS:CF_DBIAS + 256].rearrange("p (h t k) -> p h t k", h=4, t=4)
        self.TRI = self.CF[:, CF_TRI:CF_TRI + 64]
        self.PS = [kb.es.enter_context(nc.psum_tensor(f"ps{i}", [128, TT], F32)) for i in range(8)]
        self.PSb = [Buf(f"ps{i}") for i in range(8)]
        self.PSsub = {1: [Buf(f"ps1_{i}") for i in range(4)],
                      2: [Buf(f"ps2_{i}") for i in range(8)],
                      3: [Buf(f"ps3_{i}") for i in range(8)]}
        self.ch_x = kb.chan("x")
        self.ch_out = kb.chan("out")
        self.ch_c = kb.chan("cst")
        self.ch_l = kb.chan("lay")
        self.ch_l2 = kb.chan("lay2")
        self.sq_i = 0
        self.gu_i = 0
        self.y_i = 0
        self.w_i = 0
        self.m_i = 0
        self.s_i = 0
        self.pt_i = 0
        self.cur_layer = -1

    def setup(self):
        kb = self.kb
        kb.dma("sp", self.ch_c, self.GN[:], self.dr["gn"][:, :], writes=[self.cstb])
        kb.dma("sp", self.ch_c, self.CF[:], self.dr["cstf"][:, :], writes=[self.cstb])
        kb.dma("pool", self.ch_c, self.CB[:], self.dr["cstb"][:, :], writes=[self.cstb])
        kb.op("pool", lambda h: h.memset(self.ONES[:], 1.0), writes=[self.cstb])
        kb.op("pool", lambda h: h.memset(self.MASK[:], 1.0), writes=[self.cstb])
        kb.op("pool", lambda h: h.memset(self.MASK[:].rearrange("p (c t) -> p c t", t=64)[:, :, 0:1], 0.0), writes=[self.cstb])

    def layer_setup(self, l):
        if self.cur_layer == l and self.n_layers == 1:
            return
        self.cur_layer = l
        kb = self.kb
        lam_init = 0.8 - 0.6 * math.exp(-0.3 * l)
        kb.dma("sp", self.ch_l, self.SPL[:], self.dr["spl"][l], writes=[self.SPLb])
        kb.dma("pool", self.ch_l2, self.AW2[:], self.dr["aw2"][l], writes=[self.AW2b])
        sp, de = self.SPL, self.DER
        rd, wr = [self.SPLb], [self.DERb]

        def ts(col_out, col_in, n, mul):
            kb.op("dve", lambda h: h.tensor_scalar(out=de[:, col_out:col_out + n], in0=sp[:, col_in:col_in + n],
                                                   scalar1=mul, scalar2=None, op0=ALU.mult), rd, wr)

        ts(DE_GQD, SP_GQD, 1, 32 ** -0.5)
        ts(DE_GDO, SP_GDO, 1, 1.0 - lam_init)
        ts(DE_GFQ, SP_GFQ, 1, 64 ** -0.5)
        ts(DE_NFB, SP_FB, 1, -1.0)
        ts(DE_NGAB, SP_GAB, 4, -1.0)
        for (r0, col_out, col_in, mul) in ((0, DE_GD, SP_GQD, 32 ** -0.5), (64, DE_GD, SP_GKD, 1.0),
                                           (0, DE_GF, SP_GFQ, 64 ** -0.5), (64, DE_GF, SP_GFK, 1.0)):
            kb.op("dve", lambda h, r0=r0, col_out=col_out, col_in=col_in, mul=mul: h.tensor_scalar(
                out=de[r0:r0 + 64, col_out:col_out + 1], in0=sp[r0:r0 + 64, col_in:col_in + 1],
                scalar1=mul, scalar2=None, op0=ALU.mult), rd, wr)
        lt = self.LTMP
        kb.op("dve", lambda h: h.tensor_tensor(out=lt[:, 0:32], in0=sp[:, SP_LAM:SP_LAM + 32],
                                               in1=sp[:, SP_LAM + 32:SP_LAM + 64], op=ALU.mult), rd, [self.LTMPb])
        kb.op("dve", lambda h: h.reduce_sum(out=lt[:, 32:33], in_=lt[:, 0:32], axis=AX.X), [self.LTMPb], [self.LTMPb])
        kb.op("dve", lambda h: h.tensor_tensor(out=lt[:, 0:32], in0=sp[:, SP_LAM + 64:SP_LAM + 96],
                                               in1=sp[:, SP_LAM + 96:SP_LAM + 128], op=ALU.mult), rd, [self.LTMPb])
        kb.op("dve", lambda h: h.reduce_sum(out=lt[:, 33:34], in_=lt[:, 0:32], axis=AX.X), [self.LTMPb], [self.LTMPb])
        kb.op("act", lambda h: h.activation(out=lt[:, 34:36], in_=lt[:, 32:34], func=AF.Exp), [self.LTMPb], [self.LTMPb])
        kb.op("dve", lambda h: h.scalar_tensor_tensor(out=lt[:, 36:37], in0=lt[:, 34:35], scalar=-1.0, in1=lt[:, 35:36],
                                                      op0=ALU.mult, op1=ALU.add), [self.LTMPb], [self.LTMPb])
        kb.op("dve", lambda h: h.tensor_scalar(out=de[:, DE_NLAM:DE_NLAM + 1], in0=lt[:, 36:37], scalar1=-lam_init,
                                               scalar2=None, op0=ALU.add), [self.LTMPb], wr)

    def load_x(self, s):
        kb = self.kb
        for k in range(NK):
            kb.dma("sp", self.ch_x, self.XT[:, k, :], self.dr["xT"][s, k * 128:(k + 1) * 128, :],
                   writes=[b for b in self.XTb[k]])
        tok = (self.ch_x.name, self.ch_x.cnt)
        for row in self.XTb:
            for b in row:
                b.w = tok

    def store_x(self, s):
        kb = self.kb
        for k in range(NK):
            kb.dma("sp", self.ch_out, self.outT[s, k * 128:(k + 1) * 128, :], self.XT[:, k, :],
                   reads=[b for b in self.XTb[k]])
        tok = (self.ch_out.name, self.ch_out.cnt)
        for row in self.XTb:
            for b in row:
                b.r[tok[0]] = tok[1]

    def misc_bank(self, banks=(0, 1)):
        j = banks[self.m_i % len(banks)]
        self.m_i += 1
        bufs = [self.PSb[j]] + self.PSsub.get(j, [])
        return self.PS[j], bufs

    def rstd_from(self, ps_ap, n, scale, psbufs, which=0):
        kb = self.kb
        R, Rb = (self.RSTD, self.RSTDb) if which == 0 else (self.LNT, self.LNTb)
        kb.op("act", lambda h: h.activation(out=R[0:n, :], in_=ps_ap, func=AF.Ln,
                                            bias=self.CF[0:n, CF_EPS:CF_EPS + 1], scale=scale),
              reads=list(psbufs) + [self.cstb], writes=[Rb])
        kb.op("act", lambda h: h.activation(out=R[0:n, :], in_=R[0:n, :], func=AF.Exp, scale=-0.5),
              reads=[Rb], writes=[Rb])
        return R, Rb

    def norm_to_HT(self, l, which):
        kb = self.kb
        gbase = (which * L + l) * NK
        psn, psnb = self.PS[6], self.PSb[6]
        for t in range(NT):
            ts = slice(t * TT, (t + 1) * TT)
            for k in range(NK):
                i = self.sq_i % 2
                self.sq_i += 1
                kb.op("act", lambda h, k=k, i=i: h.activation(out=self.SQ[i][:], in_=self.XT[:, k, ts], func=AF.Square),
                      reads=[self.XTb[k][t]], writes=[self.SQb[i]])
                kb.mm(psn[:], self.ONES[:], self.SQ[i][:], k == 0, k == NK - 1,
                      reads=[self.SQb[i], self.cstb], writes=[psnb] if k == 0 else [])
            psnb.w = ("pe", kb.engs["pe"].cnt)
            self.rstd_from(psn[:], 128, 1.0 / D, [psnb])
            for k in range(NK):
                kb.op("dve", lambda h, k=k: h.scalar_tensor_tensor(
                    out=self.HT[:, k, ts], in0=self.XT[:, k, ts], scalar=self.GN[:, gbase + k:gbase + k + 1],
                    in1=self.RSTD[:], op0=ALU.mult, op1=ALU.mult),
                    reads=[self.XTb[k][t], self.RSTDb, self.cstb], writes=[self.HTb[k][t]])

    def ffn_load(self, l, gi, w13, w2):
        kb = self.kb
        slot = self.w_i % 2
        self.w_i += 1
        chunks = FFN_GROUPS[gi]
        fo = chunks[0][0]
        width = sum(c[1] for c in chunks)
        wa, wb = self.WA[slot], self.WB[slot]
        src = w13[l].rearrange("(k p) n -> p k n", p=128)
        kb.dma("pool", self.ch_wa[slot], wa[:, :, 0:width], src[:, :, fo:fo + width], writes=[self.WAb[slot]])
        kb.dma("pool", self.ch_wa[slot], wa[:, :, GS:GS + width], src[:, :, DFF + fo:DFF + fo + width],
               writes=[self.WAb[slot]])
        if width >= 128:
            nch = width // 128
            src2 = w2[l, fo:fo + width, :].rearrange("(c p) n -> p c n", p=128)
            kb.dma("pool", self.ch_wb[slot], wb[:, 0:nch, :], src2, writes=[self.WBb[slot]])
        else:
            kb.dma("pool", self.ch_wb[slot], wb[0:width, 0, :], w2[l, fo:fo + width, :], writes=[self.WBb[slot]])
        return slot

    def ffn_p1(self, slot, gi, t, aslot):
        kb = self.kb
        ts = slice(t * TT, (t + 1) * TT)
        wa = self.WA[slot]
        for ci, (fo, fs) in enumerate(FFN_GROUPS[gi]):
            j = self.gu_i % 2
            self.gu_i += 1
            pg, pgb = self.PS[2 * j], self.PSb[2 * j]
            pu, pub = self.PS[2 * j + 1], self.PSb[2 * j + 1]
            hreads = [self.HTb[k][t] for k in range(NK)] + [self.WAb[slot]]
            kb.mm_group(pg[0:fs, :], [(wa[:, k, ci * 128:ci * 128 + fs], self.HT[:, k, ts]) for k in range(NK)],
                        hreads, [pgb])
            kb.mm_group(pu[0:fs, :], [(wa[:, k, GS + ci * 128:GS + ci * 128 + fs], self.HT[:, k, ts]) for k in range(NK)],
                        hreads, [pub])
            kb.op("act", lambda h, j=j, fs=fs, pg=pg: h.activation(out=self.SACT[j][0:fs, :], in_=pg[0:fs, :], func=AF.Silu),
                  reads=[pgb], writes=[self.SACTb[j]])
            kb.op("dve", lambda h, j=j, fs=fs, pu=pu, ci=ci: h.tensor_tensor(
                out=self.ACTT[aslot][0:fs, ci, :], in0=self.SACT[j][0:fs, :], in1=pu[0:fs, :], op=ALU.mult),
                reads=[self.SACTb[j], pub], writes=[self.ACTTb[aslot][ci]])

    def ffn_p2(self, slot, gi, t, aslot):
        kb = self.kb
        ts = slice(t * TT, (t + 1) * TT)
        wb = self.WB[slot]
        chunks = FFN_GROUPS[gi]
        for dc in range(NK):
            j = 4 + (self.y_i % 2)
            self.y_i += 1
            py, pyb = self.PS[j], self.PSb[j]
            kb.mm_group(py[:], [(wb[0:fs, ci, dc * 128:(dc + 1) * 128], self.ACTT[aslot][0:fs, ci, :])
                                for ci, (fo, fs) in enumerate(chunks)],
                        [self.WBb[slot]] + [self.ACTTb[aslot][ci] for ci in range(len(chunks))], [pyb])
            kb.op("dve", lambda h, dc=dc, py=py: h.scalar_tensor_tensor(
                out=self.XT[:, dc, ts], in0=py[:], scalar=0.5, in1=self.XT[:, dc, ts], op0=ALU.mult, op1=ALU.add),
                reads=[pyb, self.XTb[dc][t]], writes=[self.XTb[dc][t]])

    def ffn(self, l, which, w13, w2):
        self.norm_to_HT(l, which)
        ng = len(FFN_GROUPS)
        items = [(gi, t) for gi in range(ng) for t in range(NT)]
        slots = {}
        slots[0] = self.ffn_load(l, 0, w13, w2)
        slots[1] = self.ffn_load(l, 1, w13, w2)
        prev = None
        for idx, (gi, t) in enumerate(items):
            aslot = idx % 2
            self.ffn_p1(slots[gi], gi, t, aslot)
            if prev is not None:
                pgi, pt, pas = prev
                self.ffn_p2(slots[pgi], pgi, pt, pas)
                if pt == NT - 1 and pgi + 2 < ng:
                    slots[pgi + 2] = self.ffn_load(l, pgi + 2, w13, w2)
            prev = (gi, t, aslot)
        pgi, pt, pas = prev
        self.ffn_p2(slots[pgi], pgi, pt, pas)

    def wload(self, l, name, view):
        off, n = GW[name]
        src = self.dr["w_in_r"][l].rearrange("(k p) n -> p k n", p=128)[:, :, off:off + n]
        self.kb.dma("pool", self.ch_wm[name], view[:, :, 0:n], src, writes=[self.Wb[name]])

    def woload(self, l, name, view, r0, nch):
        src = self.dr["w_out"][l, r0:r0 + nch * 128, :].rearrange("(c p) n -> p c n", p=128)
        self.kb.dma("pool", self.ch_wm[name], view[:, 0:nch, :], src, writes=[self.Wb[name]])

    def inproj_fm(self, wview, wbuf, c0, m, t, banks=(0, 1)):
        ts = slice(t * TT, (t + 1) * TT)
        ps, pb = self.misc_bank(banks)
        self.kb.mm_group(ps[0:m, :], [(wview[:, k, c0:c0 + m], self.HT[:, k, ts]) for k in range(NK)],
                         [self.HTb[k][t] for k in range(NK)] + [wbuf], pb)
        return ps, pb

    def v_tokmajor(self, wview, wbuf, c0, n, t, dst_fn, dst_bufs, banks=(0, 1)):
        for tc in range(4):
            tok = slice(t * TT + tc * 128, t * TT + (tc + 1) * 128)
            ps, pb = self.misc_bank(banks)
            self.kb.mm_group(ps[:, 0:n], [(self.HT[:, k, tok], wview[:, k, c0:c0 + n]) for k in range(NK)],
                             [self.HTb[k][t] for k in range(NK)] + [wbuf], pb)
            dst_fn(tc, ps, pb)

    def wout_partial(self, wo, wob, ot, otbufs, nch, t, banks=(0, 1)):
        kb = self.kb
        ts = slice(t * TT, (t + 1) * TT)
        for dc in range(NK):
            ps, pb = self.misc_bank(banks)
            kb.mm_group(ps[:], [(wo[:, j, dc * 128:(dc + 1) * 128], ot[:, j, :]) for j in range(nch)],
                        [wob] + list(otbufs), pb)
            kb.op("dve", lambda h, dc=dc, ps=ps: h.tensor_tensor(out=self.XT[:, dc, ts], in0=ps[:], in1=self.XT[:, dc, ts],
                                                                 op=ALU.add),
                  reads=pb + [self.XTb[dc][t]], writes=[self.XTb[dc][t]])

    def v1_ap(self, kbk, h):
        return self.V1[:, kbk, h, :]

    class _BG:
        def __init__(self, gen):
            self.gen = gen
            self.cond = None
            self.done = gen is None

        def step(self):
            if self.done:
                return
            if self.cond is not None:
                if not self.cond():
                    return
                self.cond = None
            try:
                item = next(self.gen)
                if callable(item):
                    self.cond = item
            except StopIteration:
                self.done = True

        def drain(self):
            while not self.done:
                if self.cond is not None:
                    assert self.cond(), "bg stream blocked at drain"
                    self.cond = None
                self.step()

    def run_streams(self, main, bg, ratio=1):
        b = Prog._BG(bg)
        for _ in main:
            for _i in range(ratio):
                b.step()
        b.drain()

    def attn_core(self, h, t, maps, bias_fn, diag_idx, obanks):
        kb = self.kb
        nkb = 4 * t + 4
        tiles = [(mi, kbk) for kbk in range(nkb) for mi in range(len(maps))]
        pend = []
        LAG = 1
        for item in tiles + [None] * LAG:
            if item is not None:
                mi, kbk = item
                qa = maps[mi]
                qlo = max(t * TT, kbk * 128)
                c0 = qlo - t * TT
                n = TT - c0
                diag = kbk * 128 >= t * TT
                sj = 2 + (self.s_i % 2)
                self.s_i += 1
                pss, pssb = self.PS[sj], [self.PSb[sj]] + self.PSsub[sj]
                kb.mm(pss[:, 0:n], self.KA[:, h, kbk * 128:(kbk + 1) * 128], qa[:, h, c0:TT],
                      True, not diag, reads=[self.KAb[h][kbk // 4], self.KApad, self.QAb[h], self.QApad],
                      writes=pssb, inc=not diag, skip=True)
                if diag:
                    kb.mm(pss[:, 0:128], self.IDENT, self.DIAGB[:, diag_idx, :], False, True,
                          reads=[self.cstb], writes=[], inc=True, skip=True)
                    for b in pssb:
                        b.w = ("pe", kb.engs["pe"].cnt)
                pi = self.pt_i % 3
                self.pt_i += 1
                bias_ap, bias_bufs = bias_fn(kbk)
                kb.op("act", lambda hh, pi=pi, n=n, pss=pss, bias_ap=bias_ap: hh.activation(
                    out=self.PT[pi][:, 0:n], in_=pss[:, 0:n], func=AF.Exp, bias=bias_ap, scale=1.0),
                    reads=pssb + bias_bufs, writes=[self.PTb[pi]])
                pend.append((mi, kbk, pi, c0, n))
            if len(pend) > LAG or (item is None and pend):
                pmi, pkb, ppi, pc0, pn = pend.pop(0)
                ob = obanks[pmi]
                kb.mm(self.PS[ob][:, pc0:TT], self.v1_ap(pkb, h), self.PT[ppi][:, 0:pn], pkb == 0, pkb == nkb - 1,
                      reads=[self.V1b[pkb // 4], self.V1ones, self.PTb[ppi]],
                      writes=[self.PSb[ob]] if pkb == 0 else [], inc=True, skip=True)
                if pkb == nkb - 1:
                    self.PSb[ob].w = ("pe", kb.engs["pe"].cnt)
            yield

    def chain_qk(self, wv, wbuf, c0, m, t, ss_lhsT, nscale, outs):
        kb = self.kb
        ts = slice(t * TT, (t + 1) * TT)
        ps, pb = self.PS[0], [self.PSb[0]]
        ps2, pb2 = self.PS[1], [self.PSb[1]] + self.PSsub[1]
        kb.mm_group(ps[0:m, :], [(wv[:, k, c0:c0 + m], self.HT[:, k, ts]) for k in range(NK)],
                    [self.HTb[k][t] for k in range(NK)] + [wbuf], pb)
        yield
        kb.op("act", lambda hh: hh.activation(out=self.SQ[0][0:m, :], in_=ps[0:m, :], func=AF.Square),
              reads=pb, writes=[self.SQb[0]])
        yield
        kb.mm(ps2[0:m, :], ss_lhsT, self.SQ[0][0:m, :], True, True, reads=[self.SQb[0], self.cstb], writes=pb2)
        yield
        R, Rb = self.rstd_from(ps2[0:m, :], m, nscale, pb2, 1)
        yield
        for (dst, dbufs, r0, nr, gcol, gb) in outs:
            kb.op("dve", lambda hh, dst=dst, r0=r0, nr=nr, gcol=gcol: hh.scalar_tensor_tensor(
                out=dst, in0=ps[r0:r0 + nr, :], scalar=gcol, in1=R[r0:r0 + nr, :], op0=ALU.mult, op1=ALU.mult),
                reads=pb + [Rb, gb], writes=dbufs)
        yield

    def chain_v(self, wv, wbuf, c0, t):
        kb = self.kb
        for tc in range(4):
            tok = slice(t * TT + tc * 128, t * TT + (tc + 1) * 128)
            j = tc % 2
            ps, pb = self.PS[j], [self.PSb[j]] + self.PSsub.get(j, [])
            kb.mm_group(ps[:, 0:256], [(self.HT[:, k, tok], wv[:, k, c0:c0 + 256]) for k in range(NK)],
                        [self.HTb[k][t] for k in range(NK)] + [wbuf], pb)
            yield
            kb.op("dve", lambda hh, ps=ps, tc=tc: hh.tensor_copy(
                out=self.V1[:, 4 * t + tc, :, 0:64], in_=ps[:, 0:256].rearrange("p (a b) -> p a b", b=64)),
                reads=pb, writes=[self.V1b[t]])
            yield

    def wout_gen(self, wo, wob, ot, otbufs, nch, t, banks):
        kb = self.kb
        ts = slice(t * TT, (t + 1) * TT)
        for dc in range(NK):
            j = banks[dc % len(banks)]
            ps, pb = self.PS[j], [self.PSb[j]] + self.PSsub.get(j, [])
            kb.mm_group(ps[:], [(wo[:, jj, dc * 128:(dc + 1) * 128], ot[:, jj, :]) for jj in range(nch)],
                        [wob] + list(otbufs), pb)
            kb.op("dve", lambda hh, dc=dc, ps=ps: hh.tensor_tensor(out=self.XT[:, dc, ts], in0=ps[:], in1=self.XT[:, dc, ts],
                                                                   op=ALU.add),
                  reads=pb + [self.XTb[dc][t]], writes=[self.XTb[dc][t]])
            yield

    def diff_bg(self, t):
        ts = slice(t * TT, (t + 1) * TT)
        yield from self.chain_v(self.Wd1, self.Wb["d1"], 512, t)
        for h in range(4):
            if t > 0:
                yield (lambda h=h, t=t: (t - 1, h) in self.attn_done)
            g = lambda r0: self.DER[r0:r0 + 32, DE_GD:DE_GD + 1]
            outs = [(self.QA[0:32, h, :], [self.QAb[h]], 0, 32, g(0), self.DERb),
                    (self.QA1[64:96, h, :], [self.QAb[h]], 32, 32, g(32), self.DERb),
                    (self.KA[0:32, h, ts], [self.KAb[h][t]], 64, 32, g(64), self.DERb),
                    (self.KA[64:96, h, ts], [self.KAb[h][t]], 96, 32, g(96), self.DERb)]
            yield from self.chain_qk(self.Wd1, self.Wb["d1"], h * 128, 128, t, self.BD32, 1.0 / 32, outs)

    def diff_post(self, h, t):
        kb = self.kb
        obanks = (4, 5) if h % 2 == 0 else (6, 7)
        o0, o1 = self.PS[obanks[0]], self.PS[obanks[1]]
        ob0, ob1 = self.PSb[obanks[0]], self.PSb[obanks[1]]
        kb.op("dve", lambda hh: hh.reciprocal(out=self.R1[0:64, :], in_=o0[64:128, :]), [ob0], [self.R1b])
        kb.op("dve", lambda hh: hh.tensor_tensor(out=self.T1[0:64, :], in0=o0[0:64, :], in1=self.R1[0:64, :], op=ALU.mult),
              [ob0, self.R1b], [self.T1b])
        yield
        kb.op("dve", lambda hh: hh.reciprocal(out=self.R1[0:64, :], in_=o1[64:128, :]), [ob1, self.T1b], [self.R1b])
        kb.op("dve", lambda hh: hh.tensor_tensor(out=self.T2[0:64, :], in0=o1[0:64, :], in1=self.R1[0:64, :], op=ALU.mult),
              [ob1, self.R1b], [self.T2b])
        yield
        kb.op("dve", lambda hh: hh.scalar_tensor_tensor(out=self.AA[0:64, :], in0=self.T2[0:64, :],
                                                        scalar=self.DER[0:64, DE_NLAM:DE_NLAM + 1],
                                                        in1=self.T1[0:64, :], op0=ALU.mult, op1=ALU.add),
              [self.T1b, self.T2b, self.DERb], [self.AAb])
        yield
        kb.op("act", lambda hh: hh.activation(out=self.SQ[1][0:64, :], in_=self.AA[0:64, :], func=AF.Square),
              [self.AAb], [self.SQb[1]])
        yield
        kb.mm(o0[0:64, :], self.ONES[0:64, 0:64], self.SQ[1][0:64, :], True, True,
              reads=[self.SQb[1], self.cstb], writes=[ob0])
        yield
        R, Rb = self.rstd_from(o0[0:64, :], 64, 1.0 / 64, [ob0], 0)
        yield
        hb = (h % 2) * 64
        kb.op("dve", lambda hh: hh.scalar_tensor_tensor(
            out=self.OTT[hb:hb + 64, h // 2, :], in0=self.AA[0:64, :], scalar=self.DER[0:64, DE_GDO:DE_GDO + 1],
            in1=R[0:64, :], op0=ALU.mult, op1=ALU.mult),
            [self.AAb, Rb, self.DERb], [self.OTTb[h]])
        yield

    def diff_main(self, t):
        for h in range(4):
            obanks = (4, 5) if h % 2 == 0 else (6, 7)
            yield from self.attn_core(h, t, [self.QA, self.QA1],
                                      lambda kbk, h=h, t=t: (self.DBIAS[:, h, t, kbk:kbk + 1], [self.cstb]), h, obanks)
            self.attn_done.add((t, h))
            if h >= 1:
                yield from self.diff_post(h - 1, t)
        yield from self.diff_post(3, t)
        yield from self.wout_gen(self.WOd, self.Wb["od"], self.OTT, self.OTTb, 2, t, (4, 5))

    def diff_phase(self, l):
        kb = self.kb
        self.wload(l, "d1", self.Wd1)
        self.woload(l, "od", self.WOd, 0, 2)
        if "fox" in self.phases:
            self.fox_wload(l)
            self.fox_prefetched = True
        kb.op("pool", lambda h: h.memset(self.KA[32:64, :, :], 0.0), writes=[self.KApad])
        kb.op("pool", lambda h: h.memset(self.KA[96:128, :, :], 0.0), writes=[self.KApad])
        kb.op("pool", lambda h: h.memset(self.KA[32:34, :, :], 1.0), writes=[self.KApad])
        kb.op("pool", lambda h: h.memset(self.KA[96:98, :, :], 1.0), writes=[self.KApad])
        kb.op("pool", lambda h: h.memset(self.V1[:, :, :, 64:128], 1.0), writes=[self.V1ones])
        kb.dma("pool", self.ch_qp, self.QA[:, :, :], self.dr["qapad"][0].rearrange("p (a b) -> p a b", b=TT),
               writes=[self.QApad] + self.QAb)
        kb.dma("pool", self.ch_qp, self.QA1[:, :, :], self.dr["qapad"][1].rearrange("p (a b) -> p a b", b=TT),
               writes=[self.QApad] + self.QAb)
        self.attn_done = set()
        self.run_streams(iter(()), self.diff_bg(0))
        for t in range(NT):
            self.run_streams(self.diff_main(t), self.diff_bg(t + 1) if t + 1 < NT else None)

    def fox_fgate(self, t):
        kb = self.kb
        one_col = self.CF[0:12, CF_ONE:CF_ONE + 1]
        ps, pb = self.PS[0], [self.PSb[0]]
        ts = slice(t * TT, (t + 1) * TT)
        kb.mm_group(ps[0:12, :], [(self.Wf2[:, k, 0:12], self.HT[:, k, ts]) for k in range(NK)],
                    [self.HTb[k][t] for k in range(NK)] + [self.Wb["f2"]], pb)
        yield
        kb.op("act", lambda hh: hh.activation(out=self.FE[0:12, :], in_=ps[0:12, :], func=AF.Exp,
                                              bias=self.DER[0:12, DE_NFB:DE_NFB + 1], scale=-1.0),
              reads=pb + [self.DERb], writes=[self.FEb])
        yield
        kb.op("act", lambda hh: hh.activation(out=self.FLN[0:12, :], in_=self.FE[0:12, :], func=AF.Ln, bias=one_col, scale=1.0),
              reads=[self.FEb, self.cstb], writes=[self.FLNb])
        yield
        init = 0.0 if t == 0 else self.FCAR[0:12, 0:1]
        kb.op("dve", lambda hh: hh.tensor_tensor_scan(
            out=self.FCS[0:12, :], data0=one_col.to_broadcast([12, TT]), data1=self.FLN[0:12, :], initial=init,
            op0=ALU.mult, op1=ALU.add), reads=[self.FLNb, self.FCARb, self.cstb], writes=[self.FCSb])
        kb.op("dve", lambda hh: hh.tensor_copy(out=self.FCAR[0:12, 0:1], in_=self.FCS[0:12, TT - 1:TT]),
              reads=[self.FCSb], writes=[self.FCARb])
        yield
        kb.op("dve", lambda hh: hh.tensor_scalar(out=self.FH[0:12, :], in0=self.FCS[0:12, :], scalar1=-1.0, scalar2=None, op0=ALU.mult),
              [self.FCSb], [self.FHb])
        kb.op("dve", lambda hh: hh.scalar_tensor_tensor(out=self.FR1[0:12, :], in0=self.FCS[0:12, :], scalar=-1.0,
                                                        in1=self.FH[0:12, :], op0=ALU.mult, op1=ALU.subtract),
              [self.FCSb, self.FHb], [self.FR1b])
        yield
        kb.op("dve", lambda hh: hh.tensor_copy(out=self.FM[0:12, :], in_=self.FR1[0:12, :]), [self.FR1b], [self.FMb])
        kb.op("dve", lambda hh: hh.tensor_tensor(out=self.FR2[0:12, :], in0=self.FR1[0:12, :], in1=self.FM[0:12, :], op=ALU.subtract),
              [self.FR1b, self.FMb], [self.FR2b])
        yield
        kb.op("dve", lambda hh: hh.tensor_copy(out=self.FL[0:12, :], in_=self.FR2[0:12, :]), [self.FR2b], [self.FLb])
        mj = lambda j: self.CF[0:12, CF_MJ + j:CF_MJ + j + 1]
        kb.op("dve", lambda hh: hh.tensor_scalar(out=self.FS[0:12, :], in0=self.FH[0:12, :], scalar1=mj(0), scalar2=None, op0=ALU.mult),
              [self.FHb, self.cstb], [self.FSb])
        yield
        kb.op("dve", lambda hh: hh.scalar_tensor_tensor(out=self.FS[0:12, :], in0=self.FM[0:12, :], scalar=mj(1),
                                                        in1=self.FS[0:12, :], op0=ALU.mult, op1=ALU.add),
              [self.FMb, self.FSb], [self.FSb])
        kb.op("dve", lambda hh: hh.scalar_tensor_tensor(out=self.FS[0:12, :], in0=self.FL[0:12, :], scalar=mj(2),
                                                        in1=self.FS[0:12, :], op0=ALU.mult, op1=ALU.add),
              [self.FLb, self.FSb], [self.FSb])
        yield
        ps2, pb2 = self.PS[1], [self.PSb[1]] + self.PSsub[1]
        for tc in range(4):
            kb.mm(ps2[:, tc * 4:(tc + 1) * 4], self.FS[0:12, tc * 128:(tc + 1) * 128], self.SEL[0:12, 0:4], True, True,
                  reads=[self.FSb, self.cstb], writes=pb2 if tc == 0 else [], inc=(tc == 3), skip=True)
        for b in pb2:
            b.w = ("pe", kb.engs["pe"].cnt)
        yield
        kb.op("dve", lambda hh: hh.tensor_copy(
            out=self.FBIAS[:, 4 * t:4 * t + 4, :], in_=ps2[:, 0:16].rearrange("p (a b) -> p a b", b=4)),
            reads=pb2, writes=[self.FBIASb[t]])
        yield
        if t > 0:
            yield (lambda t=t: (t - 1, 3) in self.attn_done)
        for h in range(4):
            kb.op("dve", lambda hh, h=h: hh.tensor_scalar(out=self.QA[64:76, h, :], in0=self.FS[0:12, :],
                                                          scalar1=self.CF[0:12, CF_HM + h:CF_HM + h + 1], scalar2=None, op0=ALU.mult),
                  [self.FSb, self.cstb], [self.QAb[h]])
        yield

    def fox_wload(self, l):
        self.wload(l, "f1", self.Wf1)
        self.wload(l, "f2", self.Wf2)
        self.woload(l, "of", self.WOf, 768, 2)

    def fox_bg(self, t):
        ts = slice(t * TT, (t + 1) * TT)
        yield from self.chain_v(self.Wf1, self.Wb["f1"], 512, t)
        for h in range(4):
            if t > 0:
                yield (lambda h=h, t=t: (t - 1, h) in self.attn_done)
            outs = [(self.QA[0:64, h, :], [self.QAb[h]], 0, 64, self.DER[0:64, DE_GF:DE_GF + 1], self.DERb),
                    (self.KA[0:64, h, ts], [self.KAb[h][t]], 64, 64, self.DER[64:128, DE_GF:DE_GF + 1], self.DERb)]
            yield from self.chain_qk(self.Wf1, self.Wb["f1"], h * 128, 128, t, self.BD64, 1.0 / 64, outs)
        yield from self.fox_fgate(t)

    def fox_post(self, h, t):
        kb = self.kb
        ob = 4 + h
        o0, ob0 = self.PS[ob], self.PSb[ob]
        kb.op("dve", lambda hh: hh.reciprocal(out=self.R1[0:64, :], in_=o0[64:128, :]), [ob0], [self.R1b])
        yield
        hb = (h % 2) * 64
        kb.op("dve", lambda hh: hh.tensor_tensor(
            out=self.OTT[hb:hb + 64, h // 2, :], in0=o0[0:64, :], in1=self.R1[0:64, :], op=ALU.mult),
            [ob0, self.R1b], [self.OTTb[h]])
        yield

    def fox_main(self, t):
        for h in range(4):
            yield from self.attn_core(h, t, [self.QA],
                                      lambda kbk, h=h: (self.FBIAS[:, kbk, h:h + 1], [self.FBIASb[kbk // 4]]), 4, (4 + h,))
            self.attn_done.add((t, h))
            if h >= 1:
                yield from self.fox_post(h - 1, t)
        yield from self.fox_post(3, t)
        yield from self.wout_gen(self.WOf, self.Wb["of"], self.OTT, self.OTTb, 2, t, (4, 5))

    def fox_phase(self, l):
        kb = self.kb
        if not getattr(self, "fox_prefetched", False):
            self.fox_wload(l)
        self.fox_prefetched = False
        kb.op("pool", lambda h: h.memset(self.KA[64:128, :, :], 0.0), writes=[self.KApad])
        kb.op("pool", lambda h: h.memset(self.KA[64:76, :, :], 1.0), writes=[self.KApad])
        kb.op("pool", lambda h: h.memset(self.QA[64:128, :, :], 0.0), writes=[self.QApad] + self.QAb)
        kb.op("pool", lambda h: h.memset(self.V1[:, :, :, 64:128], 1.0), writes=[self.V1ones])
        self.attn_done = set()
        self.run_streams(iter(()), self.fox_bg(0))
        for t in range(NT):
            self.run_streams(self.fox_main(t), self.fox_bg(t + 1) if t + 1 < NT else None)

    def gla_bg(self, t, par):
        kb = self.kb
        ts = slice(t * TT, (t + 1) * TT)
        QT, KT, KDEC, VG, SR, EBL = self.gQT[par], self.gKT[par], self.gKDEC[par], self.gVG[par], self.gSR[par], self.gEBL[par]
        QTb, KTb, KDECb, VGb, SRb, EBLb = (self.gQTb[par], self.gKTb[par], self.gKDECb[par], self.gVGb[par],
                                           self.gSRb[par], self.gEBLb[par])
        ps0, pb0 = self.PS[0], [self.PSb[0]]
        hreads = [self.HTb[k][t] for k in range(NK)]
        g1, g1b = self.Wg1, self.Wb["g1"]
        kb.mm_group(ps0[0:16, :], [(g1[:, k, 512:528], self.HT[:, k, ts]) for k in range(NK)], hreads + [g1b], pb0)
        yield
        kb.op("act", lambda hh: hh.activation(out=self.GAT[0:16, :], in_=ps0[0:16, :], func=AF.Copy), reads=pb0, writes=[self.GATb])
        yield
        for h in range(4):
            kb.mm(ps0[0:64, :], self.AW2[0:16, h * 64:(h + 1) * 64], self.GAT[0:16, :], True, True,
                  reads=[self.AW2b, self.GATb], writes=pb0)
            yield
            kb.op("act", lambda hh, h=h: hh.activation(out=self.LSP[0:64, :], in_=ps0[0:64, :], func=AF.Exp,
                                                       bias=self.DER[0:64, DE_NGAB + h:DE_NGAB + h + 1], scale=-1.0),
                  reads=pb0 + [self.DERb], writes=[self.LSPb])
            kb.op("act", lambda hh: hh.activation(out=self.LSP[0:64, :], in_=self.LSP[0:64, :], func=AF.Ln,
                                                  bias=self.CF[0:64, CF_ONE:CF_ONE + 1], scale=1.0),
                  reads=[self.LSPb, self.cstb], writes=[self.LSPb])
            yield
            kb.op("dve", lambda hh: hh.tensor_tensor_scan(out=self.BL[0:64, :], data0=self.MASK[0:64, :], data1=self.LSP[0:64, :],
                                                          initial=0.0, op0=ALU.mult, op1=ALU.add),
                  reads=[self.LSPb, self.cstb], writes=[self.BLb])
            yield
            kb.op("act", lambda hh: hh.activation(out=self.EB[0:64, :], in_=self.BL[0:64, :], func=AF.Exp, scale=-1.0 / 16),
                  [self.BLb], [self.EBb])
            kb.op("act", lambda hh: hh.activation(out=self.ENB[0:64, :], in_=self.BL[0:64, :], func=AF.Exp, scale=1.0 / 16),
                  [self.BLb], [self.ENBb])
            yield
            bl3 = self.BL[0:64, :].rearrange("p (c t) -> p c t", t=64)
            kb.op("dve", lambda hh, bl3=bl3: hh.tensor_tensor(
                out=self.DD[0:64, :].rearrange("p (c t) -> p c t", t=64), in0=bl3,
                in1=bl3[:, :, 63:64].to_broadcast([64, 8, 64]), op=ALU.subtract), [self.BLb], [self.DDb])
            yield
            kb.op("act", lambda hh: hh.activation(out=self.DD[0:64, :], in_=self.DD[0:64, :], func=AF.Exp, scale=1.0 / 16),
                  [self.DDb], [self.DDb])
            kb.op("dve", lambda hh, h=h: hh.tensor_copy(
                out=EBL[0:64, h, :], in_=self.EB[0:64, :].rearrange("p (c t) -> p c t", t=64)[:, :, 63]),
                [self.EBb], [EBLb[h]])
            yield
            kb.mm_group(ps0[0:64, :], [(g1[:, k, h * 64:(h + 1) * 64], self.HT[:, k, ts]) for k in range(NK)], hreads + [g1b], pb0)
            yield
            kb.op("dve", lambda hh, h=h: hh.scalar_tensor_tensor(
                out=QT[0:64, h, :], in0=ps0[0:64, :], scalar=0.125, in1=self.EB[0:64, :], op0=ALU.mult, op1=ALU.mult),
                pb0 + [self.EBb], [QTb[h]])
            yield
            kb.mm_group(ps0[0:64, :], [(g1[:, k, 256 + h * 64:256 + (h + 1) * 64], self.HT[:, k, ts]) for k in range(NK)],
                        hreads + [g1b], pb0)
            yield
            kb.op("dve", lambda hh, h=h: hh.tensor_tensor(out=KT[0:64, h, :], in0=ps0[0:64, :], in1=self.ENB[0:64, :], op=ALU.mult),
                  pb0 + [self.ENBb], [KTb[h]])
            kb.op("dve", lambda hh: hh.tensor_tensor(out=self.KDT[0:64, :], in0=ps0[0:64, :], in1=self.DD[0:64, :], op=ALU.mult),
                  pb0 + [self.DDb], [self.KDTb])
            yield
            pst_b = ps0[:].bitcast(BF16)[:, 0:256].rearrange("p (a c) -> p a c", a=4)
            for tc in range(4):
                kb.op("pe", lambda hh, tc=tc: hh.transpose(
                    out=pst_b[:, tc, :], in_=self.KDT[0:64, tc * 128:(tc + 1) * 128], identity=self.IDENT[0:64, 0:64]),
                    reads=[self.KDTb, self.cstb], writes=pb0 if tc == 0 else [], inc=(tc == 3))
            for b in pb0:
                b.w = ("pe", kb.engs["pe"].cnt)
            yield
            kb.op("act", lambda hh, h=h, pst_b=pst_b: hh.activation(out=KDEC[:, :, h, :], in_=pst_b, func=AF.Copy),
                  reads=pb0, writes=[KDECb])
            yield
        for tc in range(4):
            tok = slice(t * TT + tc * 128, t * TT + (tc + 1) * 128)
            kb.mm_group(ps0[:, :], [(self.HT[:, k, tok], self.Wg2[:, k, 0:512]) for k in range(NK)], hreads + [self.Wb["g2"]], pb0)
            yield
            kb.op("dve", lambda hh, tc=tc: hh.tensor_copy(out=VG[:, tc, :], in_=ps0[:, :]), reads=pb0, writes=[VGb[tc]])
            yield
        for h in range(4):
            kb.mm_group(ps0[:, :], [(self.Wg3[:, k, h * 128:(h + 1) * 128], self.HT[:, k, ts]) for k in range(NK)],
                        hreads + [self.Wb["g3"]], pb0)
            yield
            kb.op("act", lambda hh, h=h: hh.activation(out=SR[:, h, :], in_=ps0[:, :], func=AF.Silu), reads=pb0, writes=[SRb[h]])
            yield

    def gla_main(self, t, par):
        kb = self.kb
        QT, KT, KDEC, VG, SR, EBL = self.gQT[par], self.gKT[par], self.gKDEC[par], self.gVG[par], self.gSR[par], self.gEBL[par]
        QTb, KTb, KDECb, VGb, SRb, EBLb = (self.gQTb[par], self.gKTb[par], self.gKDECb[par], self.gVGb[par],
                                           self.gSRb[par], self.gEBLb[par])
        units = [(c, h) for c in range(8) for h in range(4)]

        def stage1(u, c, h):
            base = (c % 2) * 64
            tc = c // 2
            cs = slice(c * 64, (c + 1) * 64)
            bj = 2 + (u % 2)
            pbk = [self.PSb[bj]]
            pbv = [self.PSb[1]]
            psa = self.PS[bj][0:64, 0:64]
            pkv = self.PS[1][0:64, (u % 4) * 128:(u % 4 + 1) * 128]
            kb.mm(psa, KT[0:64, h, cs], QT[0:64, h, cs], True, True, reads=[KTb[h], QTb[h]], writes=pbk)
            kb.mm(pkv, KDEC[base:base + 64, tc, h, :], VG[base:base + 64, tc, h * 128:(h + 1) * 128], True, True,
                  reads=[KDECb, VGb[tc]], writes=pbv)
            ai = u % 8
            kb.op("dve", lambda hh: hh.tensor_tensor(out=self.AT[base:base + 64, ai, :], in0=psa, in1=self.TRI[0:64, :], op=ALU.mult),
                  reads=pbk + [self.cstb], writes=[self.ATb[ai]])
            kb.op("dve", lambda hh: hh.scalar_tensor_tensor(
                out=self.ST[0:64, h, :], in0=self.ST[0:64, h, :], scalar=EBL[0:64, h, c:c + 1], in1=pkv,
                op0=ALU.mult, op1=ALU.add), reads=[self.STb[h], EBLb[h]] + pbv, writes=[self.STb[h]])
            return ai

        def stage2(u, c, h, ai):
            base = (c % 2) * 64
            tc = c // 2
            cs = slice(c * 64, (c + 1) * 64)
            po, pob = self.PS[4 + h], self.PSb[4 + h]
            kb.mm(po[:, cs], VG[base:base + 64, tc, h * 128:(h + 1) * 128], self.AT[base:base + 64, ai, :], True, False,
                  reads=[VGb[tc], self.ATb[ai]], writes=[pob] if c == 0 else [], inc=True, skip=True)
            if base != 0:
                pe = kb.engs["pe"]
                pe.h.wait_ge(pe.sem, pe.cnt)
            kb.mm(po[:, cs], self.STB[0:64, h, :], QT[0:64, h, cs], False, True,
                  reads=[self.STBb[h], QTb[h]], writes=[], inc=True, skip=True)
            if c == 7:
                pob.w = ("pe", kb.engs["pe"].cnt)
            kb.op("act", lambda hh: hh.activation(out=self.STB[0:64, h, :], in_=self.ST[0:64, h, :], func=AF.Copy),
                  reads=[self.STb[h]], writes=[self.STBb[h]])

        prev = None
        for u, (c, h) in enumerate(units):
            st = stage1(u, c, h)
            if prev is not None:
                stage2(*prev)
            prev = (u, c, h, st)
            yield
        stage2(*prev)
        yield
        b1 = [self.PSb[2]]
        for h in range(4):
            po, pob = self.PS[4 + h], self.PSb[4 + h]
            i = self.sq_i % 2
            self.sq_i += 1
            kb.op("act", lambda hh, i=i, po=po: hh.activation(out=self.SQ[i][:], in_=po[:], func=AF.Square),
                  reads=[pob], writes=[self.SQb[i]])
            kb.mm(self.PS[2][:], self.ONES[:], self.SQ[i][:], True, True, reads=[self.SQb[i], self.cstb], writes=b1)
            yield
            R, Rb = self.rstd_from(self.PS[2][:], 128, 1.0 / 128, b1, 0)
            kb.op("dve", lambda hh, po=po: hh.scalar_tensor_tensor(
                out=self.UU[:], in0=po[:], scalar=self.SPL[:, SP_GGO:SP_GGO + 1], in1=R[:], op0=ALU.mult, op1=ALU.mult),
                reads=[pob, Rb, self.SPLb], writes=[self.UUb])
            kb.op("dve", lambda hh, h=h: hh.tensor_tensor(out=self.OTG[:, h, :], in0=self.UU[:], in1=SR[:, h, :], op=ALU.mult),
                  reads=[self.UUb, SRb[h]], writes=[self.OTGb[h]])
            yield
        yield from self.wout_gen(self.WOg, self.Wb["og"], self.OTG, self.OTGb, 4, t, (3, 2))

    def gla_phase(self, l):
        kb = self.kb
        self.wload(l, "g1", self.Wg1)
        self.wload(l, "g2", self.Wg2)
        self.wload(l, "g3", self.Wg3)
        self.woload(l, "og", self.WOg, 256, 4)
        kb.op("pool", lambda h: h.memset(self.ST[0:64, :, :], 0.0), writes=self.STb)
        kb.op("pool", lambda h: h.memset(self.STB[0:64, :, :], 0.0), writes=self.STBb)
        self.run_streams(iter(()), self.gla_bg(0, 0))
        for t in range(NT):
            self.run_streams(self.gla_main(t, t % 2), self.gla_bg(t + 1, (t + 1) % 2) if t + 1 < NT else None, ratio=3)


IN_OFF = {"d_q": 0, "d_k": 256, "d_v": 512, "g_q": 768, "g_k": 1024, "g_v": 1280, "g_r": 1792, "g_a": 2304,
          "f_q": 2320, "f_k": 2576, "f_v": 2832, "f_f": 3088}


def host_w_in(w_in):
    w_in = np.asarray(w_in, dtype=np.float32)
    out = np.zeros((L, D, NCOL), np.float32)

    def put(dst, src, n):
        out[:, :, dst:dst + n] = w_in[:, :, src:src + n]

    for h in range(4):
        for m in range(2):
            put(G_D1 + h * 128 + m * 32, IN_OFF["d_q"] + h * 64 + m * 32, 32)
            put(G_D1 + h * 128 + 64 + m * 32, IN_OFF["d_k"] + h * 64 + m * 32, 32)
        put(G_F1 + h * 128, IN_OFF["f_q"] + h * 64, 64)
        put(G_F1 + h * 128 + 64, IN_OFF["f_k"] + h * 64, 64)
    put(G_D1 + 512, IN_OFF["d_v"], 256)
    put(G_F1 + 512, IN_OFF["f_v"], 256)
    for j in range(3):
        put(G_F2 + 4 * j, IN_OFF["f_f"], 4)
    put(G_G1, IN_OFF["g_q"], 256)
    put(G_G1 + 256, IN_OFF["g_k"], 256)
    put(G_G1 + 512, IN_OFF["g_a"], 16)
    put(G_G2, IN_OFF["g_v"], 512)
    put(G_G3, IN_OFF["g_r"], 512)
    return out


def host_gn(ffn1_norm, mix_norm, ffn2_norm):
    arr = np.stack([ffn1_norm, mix_norm, ffn2_norm], axis=0)
    arr = arr.reshape(3, L, NK, 128).transpose(3, 0, 1, 2).reshape(128, 3 * L * NK)
    return np.ascontiguousarray(arr, dtype=np.float32)


def host_spl(diff_q_norm, diff_k_norm, diff_out_norm, fox_q_norm, fox_k_norm, gla_out_norm, fox_f_bias,
             gla_alpha_b, diff_lambda):
    spl = np.zeros((L, 128, NSP), np.float32)
    p = np.arange(128)
    for l in range(L):
        spl[l, :, SP_GQD] = diff_q_norm[l][p % 32]
        spl[l, :, SP_GKD] = diff_k_norm[l][p % 32]
        spl[l, :, SP_GDO] = diff_out_norm[l][p % 64]
        spl[l, :, SP_GFQ] = fox_q_norm[l][p % 64]
        spl[l, :, SP_GFK] = fox_k_norm[l][p % 64]
        spl[l, :, SP_GGO] = gla_out_norm[l][p]
        spl[l, :, SP_FB] = fox_f_bias[l][p % 4]
        for h in range(4):
            spl[l, :, SP_GAB + h] = gla_alpha_b[l][h * 64 + p % 64]
        spl[l, :, SP_LAM:SP_LAM + 128] = np.asarray(diff_lambda[l]).reshape(1, 128)
    return spl


def host_consts():
    slopes = [2.0 ** (-8.0 * (h + 1) / 4) for h in range(4)]
    p = np.arange(128)
    cf = np.zeros((128, NCF), np.float32)
    cf[:, CF_EPS] = EPS
    cf[:, CF_ONE] = 1.0
    for h in range(4):
        cf[:, CF_HM + h] = ((p % 4) == h) & (p < 12)
    for j in range(3):
        cf[:, CF_MJ + j] = ((p // 4) == j) & (p < 12)
    cf[:, CF_MASK:CF_MASK + 64] = 1.0
    cf[:, CF_MASK] = 0.0
    s_ = np.arange(64)
    tri = (s_[:, None] <= s_[None, :]).astype(np.float32)
    cf[0:64, CF_TRI:CF_TRI + 64] = tri
    cf[64:128, CF_TRI:CF_TRI + 64] = tri
    db = np.zeros((128, 4, 4, 16), np.float32)
    for h in range(4):
        for t in range(4):
            for kbk in range(16):
                db[:, h, t, kbk] = slopes[h] * (128 * kbk + p - 512 * t)
    cf[:, CF_DBIAS:CF_DBIAS + 256] = db.reshape(128, 256)
    cb = np.zeros((128, NCB), np.float32)
    for h in range(4):
        cb[:, CB_SEL + h] = -1.0 * (((p % 4) == h) & (p < 12))
    cb[:, CB_ID:CB_ID + 128] = np.eye(128, dtype=np.float32)
    cb[:, CB_BD:CB_BD + 128] = ((p[:, None] // 32) == (p[None, :] // 32)).astype(np.float32)
    cb[:, CB_BD64:CB_BD64 + 128] = ((p[:, None] // 64) == (p[None, :] // 64)).astype(np.float32)
    j = np.arange(128)
    diag = np.zeros((128, 5, 128), np.float32)
    allowed = (p[:, None] // 64) <= (j[None, :] // 64)
    fut = p[:, None] > j[None, :]
    for h in range(4):
        m = np.where(fut, -2.0 * slopes[h] * (p[:, None] - j[None, :]), 0.0)
        diag[:, h, :] = np.where(allowed, m, NEG)
    diag[:, 4, :] = np.where(p[:, None] <= j[None, :], 0.0, NEG)
    cb[:, CB_DIAG:CB_DIAG + 640] = diag.reshape(128, 640)
    qp = np.zeros((2, 128, 4, TT), np.float32)
    jj = np.arange(TT)
    for h in range(4):
        for mi, r in enumerate((32, 96)):
            qp[mi, r, h, :] = -slopes[h] * 128.0 * (jj // 128)
            qp[mi, r + 1, h, :] = -slopes[h] * (jj % 128)
    return cf, cb, qp.reshape(2, 128, 4 * TT)


def kernel(x, ffn1_norm, ffn1_w13, ffn1_w2, mix_norm, w_in, w_out, diff_q_norm,
           diff_k_norm, diff_lambda, diff_out_norm, gla_alpha_w2, gla_alpha_b,
           gla_out_norm, fox_q_norm, fox_k_norm, fox_f_bias, ffn2_norm, ffn2_w13,
           ffn2_w2, _prog=None, _n_cores=N_CORES):
    x = np.asarray(x, dtype=np.float32)
    prog = _prog or Prog()
    nc = prog.build()
    ns = prog.n_seq
    f = lambda a: np.ascontiguousarray(np.asarray(a), dtype=np.float32)
    xT = np.ascontiguousarray(x.transpose(0, 2, 1))
    cf, cb, qp = host_consts()
    shared = {
        "ffn1_w13": f(ffn1_w13), "ffn1_w2": f(ffn1_w2), "ffn2_w13": f(ffn2_w13), "ffn2_w2": f(ffn2_w2),
        "w_in_r": host_w_in(w_in), "w_out": f(w_out), "aw2": f(gla_alpha_w2),
        "gn": host_gn(f(ffn1_norm), f(mix_norm), f(ffn2_norm)),
        "spl": host_spl(f(diff_q_norm), f(diff_k_norm), f(diff_out_norm), f(fox_q_norm), f(fox_k_norm),
                        f(gla_out_norm), f(fox_f_bias), f(gla_alpha_b), f(diff_lambda)),
        "cstf": cf, "cstb": cb, "qapad": qp,
    }
    in_maps = []
    for c in range(_n_cores):
        m = dict(shared)
        m["xT"] = xT[c * ns:(c + 1) * ns]
        in_maps.append(m)
    res = run_bass_kernel_spmd(nc, in_maps, core_ids=list(range(_n_cores)))
    outT = np.concatenate([r["outT"] for r in res.results], axis=0)
    return np.ascontiguousarray(outT.transpose(0, 2, 1))
```

```python
import math
from contextlib import ExitStack

import numpy as np
import concourse.bass as bass
import concourse.mybir as mybir
from concourse.bass_utils import run_bass_kernel_spmd

F32 = mybir.dt.float32
BF16 = mybir.dt.bfloat16
AF = mybir.ActivationFunctionType
ALU = mybir.AluOpType
AX = mybir.AxisListType

D = 1024
S = 2048
L = 4
DFF = 2752
NT = 4
TT = 512
NK = 8
EPS = 1e-6
N_CORES = 8
SEQ_PER_CORE = 2
NEG = -30000.0

GS = 384
FFN_GROUPS = [[(384 * g + 128 * c, 128) for c in range(3)] for g in range(7)] + [[(2688, 64)]]

G_D1, G_F1, G_F2, G_G1, G_G2, G_G3 = 0, 768, 1536, 1548, 2076, 2588
NCOL = 3100
GW = {"d1": (G_D1, 768), "f1": (G_F1, 768), "f2": (G_F2, 12),
      "g1": (G_G1, 528), "g2": (G_G2, 512), "g3": (G_G3, 512)}

SP_GQD, SP_GKD, SP_GDO, SP_GFQ, SP_GFK, SP_GGO, SP_FB, SP_GAB, SP_LAM, NSP = 0, 1, 2, 3, 4, 5, 6, 7, 11, 144
DE_GQD, DE_GDO, DE_GFQ, DE_NFB, DE_NGAB, DE_NLAM, DE_GD, DE_GF, NDE = 0, 1, 2, 3, 4, 8, 9, 10, 16
CF_EPS, CF_ONE, CF_HM, CF_MJ, CF_MASK, CF_TRI, CF_DBIAS, NCF = 0, 1, 2, 6, 16, 80, 144, 400
CB_SEL, CB_ID, CB_BD, CB_DIAG, CB_BD64, NCB = 0, 16, 144, 272, 912, 1040


class Buf:
    __slots__ = ("name", "w", "r")

    def __init__(self, name):
        self.name = name
        self.w = None
        self.r = {}


class Eng:
    def __init__(self, name, handle, sem):
        self.name = name
        self.h = handle
        self.sem = sem
        self.cnt = 0
        self.waited = {}


class Chan:
    def __init__(self, name, sem):
        self.name = name
        self.sem = sem
        self.cnt = 0


class KB:
    def __init__(self, nc, es):
        self.nc = nc
        self.es = es
        self.engs = {}
        for name, h in (("pe", nc.tensor), ("act", nc.scalar), ("dve", nc.vector),
                        ("pool", nc.gpsimd), ("sp", nc.sync)):
            sem = es.enter_context(nc.semaphore("s_" + name))
            self.engs[name] = Eng(name, h, sem)
        self.semobj = {e.name: e.sem for e in self.engs.values()}
        self.chans = []

    def chan(self, name):
        sem = self.es.enter_context(self.nc.semaphore("c_" + name))
        c = Chan("c_" + name, sem)
        self.semobj[c.name] = sem
        self.chans.append(c)
        return c

    def sb(self, name, shape, dt):
        return self.es.enter_context(self.nc.sbuf_tensor(name, shape, dt))

    def _deps(self, eng, reads, writes):
        deps = {}

        def add(tok, same_ok):
            if tok is None:
                return
            k, v = tok
            if k == eng.name and not same_ok:
                return
            if deps.get(k, 0) < v:
                deps[k] = v

        for b in reads:
            add(b.w, eng.name != "pe")
        for b in writes:
            add(b.w, False)
            for k, v in b.r.items():
                add((k, v), False)
        return deps

    def _wait(self, eng, deps):
        for k, v in deps.items():
            if eng.waited.get(k, 0) < v:
                eng.h.wait_ge(self.semobj[k], v)
                eng.waited[k] = v

    def _record(self, tok, reads, writes):
        k, v = tok
        for b in writes:
            b.w = tok
            b.r = {}
        for b in reads:
            if b.r.get(k, 0) < v:
                b.r[k] = v

    def op(self, ename, fn, reads=(), writes=(), inc=True):
        eng = self.engs[ename]
        self._wait(eng, self._deps(eng, reads, writes))
        ins = fn(eng.h)
        if inc:
            ins.then_inc(eng.sem, 1)
            eng.cnt += 1
            tok = (eng.name, eng.cnt)
        else:
            tok = (eng.name, eng.cnt + 1)
        self._record(tok, reads, writes)
        return tok

    def dma(self, qname, chan, out, in_, reads=(), writes=()):
        eng = self.engs[qname]
        self._wait(eng, self._deps(eng, reads, writes))
        eng.h.dma_start(out=out, in_=in_).then_inc(chan.sem, 16)
        chan.cnt += 16
        tok = (chan.name, chan.cnt)
        self._record(tok, reads, writes)
        return tok

    def mm(self, out, lhsT, rhs, start, stop, reads=(), writes=(), inc=True, skip=False):
        if skip:
            fn = lambda h: h.matmul(out, lhsT=lhsT, rhs=rhs, start=start, stop=stop, skip_group_check=True)
        else:
            fn = lambda h: h.matmul(out, lhsT=lhsT, rhs=rhs, start=start, stop=stop)
        return self.op("pe", fn, reads, writes, inc)

    def mm_group(self, out, pairs, reads, writes):
        n = len(pairs)
        tok = None
        for i, (lt, rh) in enumerate(pairs):
            tok = self.mm(out, lt, rh, i == 0, i == n - 1,
                          reads if i == 0 else (), writes if i == 0 else (), inc=(i == n - 1))
        return tok

    def wait_all(self, ename, toks):
        eng = self.engs[ename]
        deps = {}
        for k, v in toks:
            if deps.get(k, 0) < v:
                deps[k] = v
        self._wait(eng, deps)

    def barrier(self):
        toks = [(e.name, e.cnt) for e in self.engs.values() if e.cnt > 0]
        toks += [(c.name, c.cnt) for c in self.chans if c.cnt > 0]
        for e in self.engs.values():
            self.wait_all(e.name, [t for t in toks if t[0] != e.name])


def mkap(view, off, dims):
    base = view.ap
    return bass.AP(view.tensor, view.offset + off, [list(base[0])] + [list(d) for d in dims])


class Prog:
    def __init__(self, n_layers=L, n_seq=SEQ_PER_CORE, phases=("ffn1", "diff", "fox", "gla", "ffn2")):
        self.n_layers = n_layers
        self.n_seq = n_seq
        self.phases = phases

    def build(self):
        nc = bass.Bass("TRN2", target_bir_lowering=False)
        self.nc = nc
        nl, ns = self.n_layers, self.n_seq
        dr = {}

        def din(name, shape):
            dr[name] = nc.dram_tensor(name, list(shape), F32, kind="ExternalInput").ap()

        din("xT", (ns, D, S))
        din("ffn1_w13", (L, D, 2 * DFF))
        din("ffn1_w2", (L, DFF, D))
        din("ffn2_w13", (L, D, 2 * DFF))
        din("ffn2_w2", (L, DFF, D))
        din("w_in_r", (L, D, NCOL))
        din("w_out", (L, D, D))
        din("aw2", (L, 16, 512))
        din("gn", (128, 3 * L * NK))
        din("spl", (L, 128, NSP))
        din("cstf", (128, NCF))
        din("cstb", (128, NCB))
        din("qapad", (2, 128, 4 * TT))
        self.dr = dr
        self.outT = nc.dram_tensor("outT", [ns, D, S], F32, kind="ExternalOutput").ap()

        with ExitStack() as es:
            kb = KB(nc, es)
            self.kb = kb
            self.alloc(kb)
            self.setup()
            for s in range(ns):
                self.load_x(s)
                for l in range(nl):
                    self.layer_setup(l)
                    if "ffn1" in self.phases:
                        self.ffn(l, 0, dr["ffn1_w13"], dr["ffn1_w2"])
                    mix = [p for p in ("diff", "fox", "gla") if p in self.phases]
                    if mix:
                        kb.barrier()
                        self.norm_to_HT(l, 1)
                        if "diff" in mix:
                            self.diff_phase(l)
                            kb.barrier()
                        if "fox" in mix:
                            self.fox_phase(l)
                            kb.barrier()
                        if "gla" in mix:
                            self.gla_phase(l)
                            kb.barrier()
                    if "ffn2" in self.phases:
                        self.ffn(l, 2, dr["ffn2_w13"], dr["ffn2_w2"])
                self.store_x(s)
            kb.wait_all("sp", [(self.ch_out.name, self.ch_out.cnt)])
        return nc

    def alloc(self, kb):
        nc = self.nc
        self.XT = kb.sb("XT", [128, NK, S], F32)
        self.XTb = [[Buf(f"xt{k}_{t}") for t in range(NT)] for k in range(NK)]
        self.HT = kb.sb("HT", [128, NK, S], BF16)
        self.HTb = [[Buf(f"ht{k}_{t}") for t in range(NT)] for k in range(NK)]
        WP = kb.sb("WP", [128, 18432], BF16)
        MPB = kb.sb("MPB", [128, 23040], BF16)
        MPF = kb.sb("MPF", [128, 3648], F32)
        self.WP, self.MPB, self.MPF = WP, MPB, MPF

        def v3(pool, off, a, b):
            return pool[:, off:off + a * b].rearrange("p (a b) -> p a b", b=b)

        def v2(pool, off, n):
            return pool[:, off:off + n]

        self.WA = [v3(WP, i * 6144, NK, 2 * GS) for i in range(2)]
        self.WAb = [Buf(f"wa{i}") for i in range(2)]
        self.WB = [v3(WP, 12288 + i * 3072, 3, D) for i in range(2)]
        self.WBb = [Buf(f"wb{i}") for i in range(2)]
        self.ch_wa = [kb.chan(f"wa{i}") for i in range(2)]
        self.ch_wb = [kb.chan(f"wb{i}") for i in range(2)]
        self.SACT = [v2(MPB, i * 512, 512) for i in range(2)]
        self.SACTb = [Buf(f"sact{i}") for i in range(2)]
        self.ACTT = [v3(MPB, 1024 + i * 1536, 3, TT) for i in range(2)]
        self.ACTTb = [[Buf(f"actT{i}_{c}") for c in range(3)] for i in range(2)]
        self.Wd1 = v3(WP, 0, NK, 768)
        self.WOd = v3(WP, 6144, 2, D)
        self.Wf1 = v3(WP, 8192, NK, 768)
        self.Wf2 = v3(WP, 14336, NK, 12)
        self.WOf = v3(WP, 14432, 2, D)
        self.Wg1 = v3(WP, 0, NK, 528)
        self.Wg2 = v3(WP, 4224, NK, 512)
        self.Wg3 = v3(WP, 8320, NK, 512)
        self.WOg = v3(WP, 12416, 4, D)
        self.Wb = {n: Buf("w_" + n) for n in ("d1", "od", "f1", "of", "f2", "g1", "g2", "g3", "og")}
        self.ch_wm = {n: kb.chan("wm_" + n) for n in self.Wb}
        self.ch_qp = kb.chan("qapad")
        self.KA = v3(MPB, 0, 4, S)
        self.KAb = [[Buf(f"ka{h}_{t}") for t in range(NT)] for h in range(4)]
        self.KApad = Buf("kapad")
        self.V1 = MPB[:, 8192:16384].rearrange("p (a b c) -> p a b c", a=16, b=4)
        self.V1b = [Buf(f"v1_{t}") for t in range(NT)]
        self.V1ones = Buf("v1ones")
        self.QA = v3(MPB, 16384, 4, TT)
        self.QAb = [Buf(f"qa{h}") for h in range(4)]
        self.QA1 = v3(MPB, 20992, 4, TT)
        self.QApad = Buf("qapad")
        self.PT = [v2(MPB, 18432 + i * 512, 512) for i in range(3)]
        self.PTb = [Buf(f"pt{i}") for i in range(3)]
        self.OTT = v3(MPB, 19968, 2, TT)
        self.OTTb = [Buf(f"ott{h}") for h in range(4)]
        self.FH, self.FM, self.FL, self.FS = [v2(MPB, 20992 + i * 512, 512) for i in range(4)]
        self.FHb, self.FMb, self.FLb, self.FSb = [Buf(n) for n in ("fh", "fm", "fl", "fs")]
        self.R1, self.T1, self.T2, self.AA = [v2(MPF, i * 512, 512) for i in range(4)]
        self.R1b, self.T1b, self.T2b, self.AAb = [Buf(n) for n in ("r1", "t1", "t2", "aa")]
        self.FE, self.FCS, self.FR1 = [v2(MPF, 512 + i * 512, 512) for i in range(3)]
        self.FLN, self.FR2 = self.FE, self.FR1
        self.FEb, self.FCSb, self.FR1b = [Buf(n) for n in ("fe", "fcs", "fr1")]
        self.FLNb, self.FR2b = self.FEb, self.FR1b
        self.FBIAS = v3(MPF, 2048, 16, 4)
        self.FBIASb = [Buf(f"fbias{t}") for t in range(NT)]
        self.FCAR = v2(MPF, 2112, 1)
        self.FCARb = Buf("fcar")
        self.gQT = [v3(MPB, 0, 4, TT), v3(MPB, 14848, 4, TT)]
        self.gKT = [v3(MPB, 2048, 4, TT), v3(MPB, 16896, 4, TT)]
        self.KDT = v2(MPB, 4096, 512)
        self.KDTb = Buf("kdt")
        self.gKDEC = [MPB[:, o:o + 2048].rearrange("p (a e b c) -> p a e b c", a=4, e=2, b=4) for o in (4608, 18944)]
        self.gVG = [v3(MPB, 6656, 4, TT), v3(MPB, 20992, 4, TT)]
        self.gSR = [v3(MPB, 8704, 4, TT)] * 2
        self.gQTb = [[Buf(f"qt{p}{h}") for h in range(4)] for p in range(2)]
        self.gKTb = [[Buf(f"kt{p}{h}") for h in range(4)] for p in range(2)]
        self.gKDECb = [Buf(f"kdec{p}") for p in range(2)]
        self.gVGb = [[Buf(f"vg{p}{i}") for i in range(4)] for p in range(2)]
        srb = [Buf(f"sr{h}") for h in range(4)]
        self.gSRb = [srb, srb]
        self.gpad = Buf("gpad")
        self.AT = v3(MPB, 10752, 8, 64)
        self.ATb = [Buf(f"at{i}") for i in range(8)]
        self.STB = MPB[:, 11264:12288].rearrange("p (s a b) -> p s a b", s=2, a=4)
        self.STBb = [[Buf(f"stb{s_}{h}") for h in range(4)] for s_ in range(2)]
        self.GAT = v2(MPB, 12288, 512)
        self.GATb = Buf("gat")
        self.OTG = v3(MPB, 12800, 4, TT)
        self.OTGb = [Buf(f"otg{h}") for h in range(4)]
        self.LSP, self.BL, self.EB, self.ENB, self.DD, self.UU = [v2(MPF, i * 512, 512) for i in range(6)]
        self.LSPb, self.BLb, self.EBb, self.ENBb, self.DDb, self.UUb = [Buf(n) for n in ("lsp", "bl", "eb", "enb", "dd", "uu")]
        self.ST = v3(MPF, 3072, 4, 128)
        self.STb = [Buf(f"st{h}") for h in range(4)]
        self.gEBL = [v3(MPF, 3584, 4, 8), v3(MPF, 3616, 4, 8)]
        self.gEBLb = [[Buf(f"ebl{p}{h}") for h in range(4)] for p in range(2)]
        self.SQ = [kb.sb(f"sq{i}", [128, TT], BF16) for i in range(2)]
        self.SQb = [Buf(f"sq{i}") for i in range(2)]
        self.LNT = kb.sb("lnt", [128, TT], F32)
        self.LNTb = Buf("lnt")
        self.RSTD = kb.sb("rstd", [128, TT], F32)
        self.RSTDb = Buf("rstd")
        self.GN = kb.sb("GN", [128, 3 * L * NK], F32)
        self.SPL = kb.sb("SPL", [128, NSP], F32)
        self.SPLb = Buf("spl")
        self.DER = kb.sb("DER", [128, NDE], F32)
        self.DERb = Buf("der")
        self.LTMP = kb.sb("LTMP", [128, 40], F32)
        self.LTMPb = Buf("ltmp")
        self.AW2 = kb.sb("AW2", [128, 4, 128], BF16)
        self.AW2b = Buf("aw2")
        self.CF = kb.sb("CF", [128, NCF], F32)
        self.CB = kb.sb("CB", [128, NCB], BF16)
        self.ONES = kb.sb("ONES", [128, 128], BF16)
        self.MASK = kb.sb("MASK", [128, TT], BF16)
        self.cstb = Buf("cst")
        self.IDENT = self.CB[:, CB_ID:CB_ID + 128]
        self.BD32 = self.CB[:, CB_BD:CB_BD + 128]
        self.BD64 = self.CB[:, CB_BD64:CB_BD64 + 128]
        self.DIAGB = self.CB[:, CB_DIAG:CB_DIAG + 640].rearrange("p (a b) -> p a b", b=128)
        self.SEL = self.CB[:, CB_SEL:CB_SEL + 4]
        self.DBIAS = self.CF[:, CF_DBIAS:CF_DBIAS + 256].rearrange("p (h t k) -> p h t k", h=4, t=4)
        self.TRI = self.CF[:, CF_TRI:CF_TRI + 64]
        self.PS = [kb.es.enter_context(nc.psum_tensor(f"ps{i}", [128, TT], F32)) for i in range(8)]
        self.PSb = [Buf(f"ps{i}") for i in range(8)]
        self.PSsub = {1: [Buf(f"ps1_{i}") for i in range(4)],
                      2: [Buf(f"ps2_{i}") for i in range(8)],
                      3: [Buf(f"ps3_{i}") for i in range(8)]}
        self.ch_x = kb.chan("x")
        self.ch_out = kb.chan("out")
        self.ch_c = kb.chan("cst")
        self.ch_l = kb.chan("lay")
        self.ch_l2 = kb.chan("lay2")
        self.sq_i = 0
        self.gu_i = 0
        self.y_i = 0
        self.w_i = 0
        self.m_i = 0
        self.s_i = 0
        self.pt_i = 0
        self.cur_layer = -1

    def setup(self):
        kb = self.kb
        kb.dma("sp", self.ch_c, self.GN[:], self.dr["gn"][:, :], writes=[self.cstb])
        kb.dma("sp", self.ch_c, self.CF[:], self.dr["cstf"][:, :], writes=[self.cstb])
        kb.dma("pool", self.ch_c, self.CB[:], self.dr["cstb"][:, :], writes=[self.cstb])
        kb.op("pool", lambda h: h.memset(self.ONES[:], 1.0), writes=[self.cstb])
        kb.op("pool", lambda h: h.memset(self.MASK[:], 1.0), writes=[self.cstb])
        kb.op("pool", lambda h: h.memset(self.AW2[:, :, :], 0.0), writes=[self.AW2b])
        kb.op("pool", lambda h: h.memset(self.MASK[:].rearrange("p (c t) -> p c t", t=64)[:, :, 0:1], 0.0), writes=[self.cstb])

    def layer_setup(self, l):
        if self.cur_layer == l and self.n_layers == 1:
            return
        self.cur_layer = l
        kb = self.kb
        lam_init = 0.8 - 0.6 * math.exp(-0.3 * l)
        kb.dma("sp", self.ch_l, self.SPL[:], self.dr["spl"][l], writes=[self.SPLb])
        kb.dma("pool", self.ch_l2, self.AW2[0:16, :, :], self.dr["aw2"][l].rearrange("p (a b) -> p a b", b=128), writes=[self.AW2b])
        sp, de = self.SPL, self.DER
        rd, wr = [self.SPLb], [self.DERb]

        def ts(col_out, col_in, n, mul):
            kb.op("dve", lambda h: h.tensor_scalar(out=de[:, col_out:col_out + n], in0=sp[:, col_in:col_in + n],
                                                   scalar1=mul, scalar2=None, op0=ALU.mult), rd, wr)

        ts(DE_GQD, SP_GQD, 1, 32 ** -0.5)
        ts(DE_GDO, SP_GDO, 1, 1.0 - lam_init)
        ts(DE_GFQ, SP_GFQ, 1, 64 ** -0.5)
        ts(DE_NFB, SP_FB, 1, -1.0)
        ts(DE_NGAB, SP_GAB, 4, -1.0)
        for (r0, col_out, col_in, mul) in ((0, DE_GD, SP_GQD, 32 ** -0.5), (64, DE_GD, SP_GKD, 1.0),
                                           (0, DE_GF, SP_GFQ, 64 ** -0.5), (64, DE_GF, SP_GFK, 1.0)):
            kb.op("dve", lambda h, r0=r0, col_out=col_out, col_in=col_in, mul=mul: h.tensor_scalar(
                out=de[r0:r0 + 64, col_out:col_out + 1], in0=sp[r0:r0 + 64, col_in:col_in + 1],
                scalar1=mul, scalar2=None, op0=ALU.mult), rd, wr)
        lt = self.LTMP
        kb.op("dve", lambda h: h.tensor_tensor(out=lt[:, 0:32], in0=sp[:, SP_LAM:SP_LAM + 32],
                                               in1=sp[:, SP_LAM + 32:SP_LAM + 64], op=ALU.mult), rd, [self.LTMPb])
        kb.op("dve", lambda h: h.reduce_sum(out=lt[:, 32:33], in_=lt[:, 0:32], axis=AX.X), [self.LTMPb], [self.LTMPb])
        kb.op("dve", lambda h: h.tensor_tensor(out=lt[:, 0:32], in0=sp[:, SP_LAM + 64:SP_LAM + 96],
                                               in1=sp[:, SP_LAM + 96:SP_LAM + 128], op=ALU.mult), rd, [self.LTMPb])
        kb.op("dve", lambda h: h.reduce_sum(out=lt[:, 33:34], in_=lt[:, 0:32], axis=AX.X), [self.LTMPb], [self.LTMPb])
        kb.op("act", lambda h: h.activation(out=lt[:, 34:36], in_=lt[:, 32:34], func=AF.Exp), [self.LTMPb], [self.LTMPb])
        kb.op("dve", lambda h: h.scalar_tensor_tensor(out=lt[:, 36:37], in0=lt[:, 34:35], scalar=-1.0, in1=lt[:, 35:36],
                                                      op0=ALU.mult, op1=ALU.add), [self.LTMPb], [self.LTMPb])
        kb.op("dve", lambda h: h.tensor_scalar(out=de[:, DE_NLAM:DE_NLAM + 1], in0=lt[:, 36:37], scalar1=-lam_init,
                                               scalar2=None, op0=ALU.add), [self.LTMPb], wr)

    def load_x(self, s):
        kb = self.kb
        for k in range(NK):
            kb.dma("sp", self.ch_x, self.XT[:, k, :], self.dr["xT"][s, k * 128:(k + 1) * 128, :],
                   writes=[b for b in self.XTb[k]])
        tok = (self.ch_x.name, self.ch_x.cnt)
        for row in self.XTb:
            for b in row:
                b.w = tok

    def store_x(self, s):
        kb = self.kb
        for k in range(NK):
            kb.dma("sp", self.ch_out, self.outT[s, k * 128:(k + 1) * 128, :], self.XT[:, k, :],
                   reads=[b for b in self.XTb[k]])
        tok = (self.ch_out.name, self.ch_out.cnt)
        for row in self.XTb:
            for b in row:
                b.r[tok[0]] = tok[1]

    def misc_bank(self, banks=(0, 1)):
        j = banks[self.m_i % len(banks)]
        self.m_i += 1
        bufs = [self.PSb[j]] + self.PSsub.get(j, [])
        return self.PS[j], bufs

    def rstd_from(self, ps_ap, n, scale, psbufs, which=0):
        kb = self.kb
        R, Rb = (self.RSTD, self.RSTDb) if which == 0 else (self.LNT, self.LNTb)
        kb.op("act", lambda h: h.activation(out=R[0:n, :], in_=ps_ap, func=AF.Ln,
                                            bias=self.CF[0:n, CF_EPS:CF_EPS + 1], scale=scale),
              reads=list(psbufs) + [self.cstb], writes=[Rb])
        kb.op("act", lambda h: h.activation(out=R[0:n, :], in_=R[0:n, :], func=AF.Exp, scale=-0.5),
              reads=[Rb], writes=[Rb])
        return R, Rb

    def norm_to_HT(self, l, which):
        kb = self.kb
        gbase = (which * L + l) * NK
        psn, psnb = self.PS[6], self.PSb[6]
        for t in range(NT):
            ts = slice(t * TT, (t + 1) * TT)
            for k in range(NK):
                i = self.sq_i % 2
                self.sq_i += 1
                kb.op("act", lambda h, k=k, i=i: h.activation(out=self.SQ[i][:], in_=self.XT[:, k, ts], func=AF.Square),
                      reads=[self.XTb[k][t]], writes=[self.SQb[i]])
                kb.mm(psn[:], self.ONES[:], self.SQ[i][:], k == 0, k == NK - 1,
                      reads=[self.SQb[i], self.cstb], writes=[psnb] if k == 0 else [])
            psnb.w = ("pe", kb.engs["pe"].cnt)
            self.rstd_from(psn[:], 128, 1.0 / D, [psnb])
            for k in range(NK):
                kb.op("dve", lambda h, k=k: h.scalar_tensor_tensor(
                    out=self.HT[:, k, ts], in0=self.XT[:, k, ts], scalar=self.GN[:, gbase + k:gbase + k + 1],
                    in1=self.RSTD[:], op0=ALU.mult, op1=ALU.mult),
                    reads=[self.XTb[k][t], self.RSTDb, self.cstb], writes=[self.HTb[k][t]])

    def ffn_load(self, l, gi, w13, w2):
        kb = self.kb
        slot = self.w_i % 2
        self.w_i += 1
        chunks = FFN_GROUPS[gi]
        fo = chunks[0][0]
        width = sum(c[1] for c in chunks)
        wa, wb = self.WA[slot], self.WB[slot]
        src = w13[l].rearrange("(k p) n -> p k n", p=128)
        kb.dma("pool", self.ch_wa[slot], wa[:, :, 0:width], src[:, :, fo:fo + width], writes=[self.WAb[slot]])
        kb.dma("pool", self.ch_wa[slot], wa[:, :, GS:GS + width], src[:, :, DFF + fo:DFF + fo + width],
               writes=[self.WAb[slot]])
        if width >= 128:
            nch = width // 128
            src2 = w2[l, fo:fo + width, :].rearrange("(c p) n -> p c n", p=128)
            kb.dma("pool", self.ch_wb[slot], wb[:, 0:nch, :], src2, writes=[self.WBb[slot]])
        else:
            kb.dma("pool", self.ch_wb[slot], wb[0:width, 0, :], w2[l, fo:fo + width, :], writes=[self.WBb[slot]])
        return slot

    def ffn_p1(self, slot, gi, t, aslot):
        kb = self.kb
        ts = slice(t * TT, (t + 1) * TT)
        wa = self.WA[slot]
        for ci, (fo, fs) in enumerate(FFN_GROUPS[gi]):
            j = self.gu_i % 2
            self.gu_i += 1
            pg, pgb = self.PS[2 * j], self.PSb[2 * j]
            pu, pub = self.PS[2 * j + 1], self.PSb[2 * j + 1]
            hreads = [self.HTb[k][t] for k in range(NK)] + [self.WAb[slot]]
            kb.mm_group(pg[0:fs, :], [(wa[:, k, ci * 128:ci * 128 + fs], self.HT[:, k, ts]) for k in range(NK)],
                        hreads, [pgb])
            kb.mm_group(pu[0:fs, :], [(wa[:, k, GS + ci * 128:GS + ci * 128 + fs], self.HT[:, k, ts]) for k in range(NK)],
                        hreads, [pub])
            kb.op("act", lambda h, j=j, fs=fs, pg=pg: h.activation(out=self.SACT[j][0:fs, :], in_=pg[0:fs, :], func=AF.Silu),
                  reads=[pgb], writes=[self.SACTb[j]])
            kb.op("dve", lambda h, j=j, fs=fs, pu=pu, ci=ci: h.tensor_tensor(
                out=self.ACTT[aslot][0:fs, ci, :], in0=self.SACT[j][0:fs, :], in1=pu[0:fs, :], op=ALU.mult),
                reads=[self.SACTb[j], pub], writes=[self.ACTTb[aslot][ci]])

    def ffn_p2(self, slot, gi, t, aslot):
        kb = self.kb
        ts = slice(t * TT, (t + 1) * TT)
        wb = self.WB[slot]
        chunks = FFN_GROUPS[gi]
        for dc in range(NK):
            j = 4 + (self.y_i % 2)
            self.y_i += 1
            py, pyb = self.PS[j], self.PSb[j]
            kb.mm_group(py[:], [(wb[0:fs, ci, dc * 128:(dc + 1) * 128], self.ACTT[aslot][0:fs, ci, :])
                                for ci, (fo, fs) in enumerate(chunks)],
                        [self.WBb[slot]] + [self.ACTTb[aslot][ci] for ci in range(len(chunks))], [pyb])
            kb.op("dve", lambda h, dc=dc, py=py: h.scalar_tensor_tensor(
                out=self.XT[:, dc, ts], in0=py[:], scalar=0.5, in1=self.XT[:, dc, ts], op0=ALU.mult, op1=ALU.add),
                reads=[pyb, self.XTb[dc][t]], writes=[self.XTb[dc][t]])

    def ffn(self, l, which, w13, w2):
        self.norm_to_HT(l, which)
        ng = len(FFN_GROUPS)
        items = [(gi, t) for gi in range(ng) for t in range(NT)]
        slots = {}
        slots[0] = self.ffn_load(l, 0, w13, w2)
        slots[1] = self.ffn_load(l, 1, w13, w2)
        prev = None
        for idx, (gi, t) in enumerate(items):
            aslot = idx % 2
            self.ffn_p1(slots[gi], gi, t, aslot)
            if prev is not None:
                pgi, pt, pas = prev
                self.ffn_p2(slots[pgi], pgi, pt, pas)
                if pt == NT - 1 and pgi + 2 < ng:
                    slots[pgi + 2] = self.ffn_load(l, pgi + 2, w13, w2)
            prev = (gi, t, aslot)
        pgi, pt, pas = prev
        self.ffn_p2(slots[pgi], pgi, pt, pas)

    def wload(self, l, name, view):
        off, n = GW[name]
        src = self.dr["w_in_r"][l].rearrange("(k p) n -> p k n", p=128)[:, :, off:off + n]
        self.kb.dma("pool", self.ch_wm[name], view[:, :, 0:n], src, writes=[self.Wb[name]])

    def woload(self, l, name, view, r0, nch):
        src = self.dr["w_out"][l, r0:r0 + nch * 128, :].rearrange("(c p) n -> p c n", p=128)
        self.kb.dma("pool", self.ch_wm[name], view[:, 0:nch, :], src, writes=[self.Wb[name]])

    def inproj_fm(self, wview, wbuf, c0, m, t, banks=(0, 1)):
        ts = slice(t * TT, (t + 1) * TT)
        ps, pb = self.misc_bank(banks)
        self.kb.mm_group(ps[0:m, :], [(wview[:, k, c0:c0 + m], self.HT[:, k, ts]) for k in range(NK)],
                         [self.HTb[k][t] for k in range(NK)] + [wbuf], pb)
        return ps, pb

    def v_tokmajor(self, wview, wbuf, c0, n, t, dst_fn, dst_bufs, banks=(0, 1)):
        for tc in range(4):
            tok = slice(t * TT + tc * 128, t * TT + (tc + 1) * 128)
            ps, pb = self.misc_bank(banks)
            self.kb.mm_group(ps[:, 0:n], [(self.HT[:, k, tok], wview[:, k, c0:c0 + n]) for k in range(NK)],
                             [self.HTb[k][t] for k in range(NK)] + [wbuf], pb)
            dst_fn(tc, ps, pb)

    def wout_partial(self, wo, wob, ot, otbufs, nch, t, banks=(0, 1)):
        kb = self.kb
        ts = slice(t * TT, (t + 1) * TT)
        for dc in range(NK):
            ps, pb = self.misc_bank(banks)
            kb.mm_group(ps[:], [(wo[:, j, dc * 128:(dc + 1) * 128], ot[:, j, :]) for j in range(nch)],
                        [wob] + list(otbufs), pb)
            kb.op("dve", lambda h, dc=dc, ps=ps: h.tensor_tensor(out=self.XT[:, dc, ts], in0=ps[:], in1=self.XT[:, dc, ts],
                                                                 op=ALU.add),
                  reads=pb + [self.XTb[dc][t]], writes=[self.XTb[dc][t]])

    def v1_ap(self, kbk, h):
        return self.V1[:, kbk, h, :]

    class _BG:
        def __init__(self, gen):
            self.gen = gen
            self.cond = None
            self.done = gen is None

        def step(self):
            if self.done:
                return
            if self.cond is not None:
                if not self.cond():
                    return
                self.cond = None
            try:
                item = next(self.gen)
                if callable(item):
                    self.cond = item
            except StopIteration:
                self.done = True

        def drain(self):
            while not self.done:
                if self.cond is not None:
                    assert self.cond(), "bg stream blocked at drain"
                    self.cond = None
                self.step()

    def run_streams(self, main, bg, ratio=1):
        b = Prog._BG(bg)
        for _ in main:
            for _i in range(ratio):
                b.step()
        b.drain()

    def attn_core(self, h, t, maps, bias_fn, diag_idx, obanks):
        kb = self.kb
        nkb = 4 * t + 4
        tiles = [(mi, kbk) for kbk in range(nkb) for mi in range(len(maps))]
        pend = []
        LAG = 1
        for item in tiles + [None] * LAG:
            if item is not None:
                mi, kbk = item
                qa = maps[mi]
                qlo = max(t * TT, kbk * 128)
                c0 = qlo - t * TT
                n = TT - c0
                diag = kbk * 128 >= t * TT
                sj = 2 + (self.s_i % 2)
                self.s_i += 1
                pss, pssb = self.PS[sj], [self.PSb[sj]] + self.PSsub[sj]
                kb.mm(pss[:, 0:n], self.KA[:, h, kbk * 128:(kbk + 1) * 128], qa[:, h, c0:TT],
                      True, not diag, reads=[self.KAb[h][kbk // 4], self.KApad, self.QAb[h], self.QApad],
                      writes=pssb, inc=not diag, skip=True)
                if diag:
                    kb.mm(pss[:, 0:128], self.IDENT, self.DIAGB[:, diag_idx, :], False, True,
                          reads=[self.cstb], writes=[], inc=True, skip=True)
                    for b in pssb:
                        b.w = ("pe", kb.engs["pe"].cnt)
                pi = self.pt_i % 3
                self.pt_i += 1
                bias_ap, bias_bufs = bias_fn(kbk)
                kb.op("act", lambda hh, pi=pi, n=n, pss=pss, bias_ap=bias_ap: hh.activation(
                    out=self.PT[pi][:, 0:n], in_=pss[:, 0:n], func=AF.Exp, bias=bias_ap, scale=1.0),
                    reads=pssb + bias_bufs, writes=[self.PTb[pi]])
                pend.append((mi, kbk, pi, c0, n))
            if len(pend) > LAG or (item is None and pend):
                pmi, pkb, ppi, pc0, pn = pend.pop(0)
                ob = obanks[pmi]
                kb.mm(self.PS[ob][:, pc0:TT], self.v1_ap(pkb, h), self.PT[ppi][:, 0:pn], pkb == 0, pkb == nkb - 1,
                      reads=[self.V1b[pkb // 4], self.V1ones, self.PTb[ppi]],
                      writes=[self.PSb[ob]] if pkb == 0 else [], inc=True, skip=True)
                if pkb == nkb - 1:
                    self.PSb[ob].w = ("pe", kb.engs["pe"].cnt)
            yield

    def chain_qk(self, wv, wbuf, c0, m, t, ss_lhsT, nscale, outs):
        kb = self.kb
        ts = slice(t * TT, (t + 1) * TT)
        ps, pb = self.PS[0], [self.PSb[0]]
        ps2, pb2 = self.PS[1], [self.PSb[1]] + self.PSsub[1]
        kb.mm_group(ps[0:m, :], [(wv[:, k, c0:c0 + m], self.HT[:, k, ts]) for k in range(NK)],
                    [self.HTb[k][t] for k in range(NK)] + [wbuf], pb)
        yield
        kb.op("act", lambda hh: hh.activation(out=self.SQ[0][0:m, :], in_=ps[0:m, :], func=AF.Square),
              reads=pb, writes=[self.SQb[0]])
        yield
        kb.mm(ps2[0:m, :], ss_lhsT, self.SQ[0][0:m, :], True, True, reads=[self.SQb[0], self.cstb], writes=pb2)
        yield
        R, Rb = self.rstd_from(ps2[0:m, :], m, nscale, pb2, 1)
        yield
        for (dst, dbufs, r0, nr, gcol, gb) in outs:
            kb.op("dve", lambda hh, dst=dst, r0=r0, nr=nr, gcol=gcol: hh.scalar_tensor_tensor(
                out=dst, in0=ps[r0:r0 + nr, :], scalar=gcol, in1=R[r0:r0 + nr, :], op0=ALU.mult, op1=ALU.mult),
                reads=pb + [Rb, gb], writes=dbufs)
        yield

    def chain_v(self, wv, wbuf, c0, t):
        kb = self.kb
        for tc in range(4):
            tok = slice(t * TT + tc * 128, t * TT + (tc + 1) * 128)
            j = tc % 2
            ps, pb = self.PS[j], [self.PSb[j]] + self.PSsub.get(j, [])
            kb.mm_group(ps[:, 0:256], [(self.HT[:, k, tok], wv[:, k, c0:c0 + 256]) for k in range(NK)],
                        [self.HTb[k][t] for k in range(NK)] + [wbuf], pb)
            yield
            kb.op("dve", lambda hh, ps=ps, tc=tc: hh.tensor_copy(
                out=self.V1[:, 4 * t + tc, :, 0:64], in_=ps[:, 0:256].rearrange("p (a b) -> p a b", b=64)),
                reads=pb, writes=[self.V1b[t]])
            yield

    def wout_gen(self, wo, wob, ot, otbufs, nch, t, banks):
        kb = self.kb
        ts = slice(t * TT, (t + 1) * TT)
        for dc in range(NK):
            j = banks[dc % len(banks)]
            ps, pb = self.PS[j], [self.PSb[j]] + self.PSsub.get(j, [])
            kb.mm_group(ps[:], [(wo[:, jj, dc * 128:(dc + 1) * 128], ot[:, jj, :]) for jj in range(nch)],
                        [wob] + list(otbufs), pb)
            kb.op("dve", lambda hh, dc=dc, ps=ps: hh.tensor_tensor(out=self.XT[:, dc, ts], in0=ps[:], in1=self.XT[:, dc, ts],
                                                                   op=ALU.add),
                  reads=pb + [self.XTb[dc][t]], writes=[self.XTb[dc][t]])
            yield

    def diff_bg(self, t):
        ts = slice(t * TT, (t + 1) * TT)
        yield from self.chain_v(self.Wd1, self.Wb["d1"], 512, t)
        for h in range(4):
            if t > 0:
                yield (lambda h=h, t=t: (t - 1, h) in self.attn_done)
            g = lambda r0: self.DER[r0:r0 + 32, DE_GD:DE_GD + 1]
            outs = [(self.QA[0:32, h, :], [self.QAb[h]], 0, 32, g(0), self.DERb),
                    (self.QA1[64:96, h, :], [self.QAb[h]], 32, 32, g(32), self.DERb),
                    (self.KA[0:32, h, ts], [self.KAb[h][t]], 64, 32, g(64), self.DERb),
                    (self.KA[64:96, h, ts], [self.KAb[h][t]], 96, 32, g(96), self.DERb)]
            yield from self.chain_qk(self.Wd1, self.Wb["d1"], h * 128, 128, t, self.BD32, 1.0 / 32, outs)

    def diff_post(self, h, t):
        kb = self.kb
        obanks = (4, 5) if h % 2 == 0 else (6, 7)
        o0, o1 = self.PS[obanks[0]], self.PS[obanks[1]]
        ob0, ob1 = self.PSb[obanks[0]], self.PSb[obanks[1]]
        kb.op("dve", lambda hh: hh.reciprocal(out=self.R1[0:64, :], in_=o0[64:128, :]), [ob0], [self.R1b])
        kb.op("dve", lambda hh: hh.tensor_tensor(out=self.T1[0:64, :], in0=o0[0:64, :], in1=self.R1[0:64, :], op=ALU.mult),
              [ob0, self.R1b], [self.T1b])
        yield
        kb.op("dve", lambda hh: hh.reciprocal(out=self.R1[0:64, :], in_=o1[64:128, :]), [ob1, self.T1b], [self.R1b])
        kb.op("dve", lambda hh: hh.tensor_tensor(out=self.T2[0:64, :], in0=o1[0:64, :], in1=self.R1[0:64, :], op=ALU.mult),
              [ob1, self.R1b], [self.T2b])
        yield
        kb.op("dve", lambda hh: hh.scalar_tensor_tensor(out=self.AA[0:64, :], in0=self.T2[0:64, :],
                                                        scalar=self.DER[0:64, DE_NLAM:DE_NLAM + 1],
                                                        in1=self.T1[0:64, :], op0=ALU.mult, op1=ALU.add),
              [self.T1b, self.T2b, self.DERb], [self.AAb])
        yield
        kb.op("act", lambda hh: hh.activation(out=self.SQ[1][0:64, :], in_=self.AA[0:64, :], func=AF.Square),
              [self.AAb], [self.SQb[1]])
        yield
        kb.mm(o0[0:64, :], self.ONES[0:64, 0:64], self.SQ[1][0:64, :], True, True,
              reads=[self.SQb[1], self.cstb], writes=[ob0])
        yield
        R, Rb = self.rstd_from(o0[0:64, :], 64, 1.0 / 64, [ob0], 0)
        yield
        hb = (h % 2) * 64
        kb.op("dve", lambda hh: hh.scalar_tensor_tensor(
            out=self.OTT[hb:hb + 64, h // 2, :], in0=self.AA[0:64, :], scalar=self.DER[0:64, DE_GDO:DE_GDO + 1],
            in1=R[0:64, :], op0=ALU.mult, op1=ALU.mult),
            [self.AAb, Rb, self.DERb], [self.OTTb[h]])
        yield

    def diff_main(self, t):
        for h in range(4):
            obanks = (4, 5) if h % 2 == 0 else (6, 7)
            yield from self.attn_core(h, t, [self.QA, self.QA1],
                                      lambda kbk, h=h, t=t: (self.DBIAS[:, h, t, kbk:kbk + 1], [self.cstb]), h, obanks)
            self.attn_done.add((t, h))
            if h >= 1:
                yield from self.diff_post(h - 1, t)
        yield from self.diff_post(3, t)
        yield from self.wout_gen(self.WOd, self.Wb["od"], self.OTT, self.OTTb, 2, t, (4, 5))

    def diff_phase(self, l):
        kb = self.kb
        self.wload(l, "d1", self.Wd1)
        self.woload(l, "od", self.WOd, 0, 2)
        if "fox" in self.phases:
            self.fox_wload(l)
            self.fox_prefetched = True
        kb.op("pool", lambda h: h.memset(self.KA[32:64, :, :], 0.0), writes=[self.KApad])
        kb.op("pool", lambda h: h.memset(self.KA[96:128, :, :], 0.0), writes=[self.KApad])
        kb.op("pool", lambda h: h.memset(self.KA[32:34, :, :], 1.0), writes=[self.KApad])
        kb.op("pool", lambda h: h.memset(self.KA[96:98, :, :], 1.0), writes=[self.KApad])
        kb.op("pool", lambda h: h.memset(self.V1[:, :, :, 64:128], 1.0), writes=[self.V1ones])
        kb.dma("pool", self.ch_qp, self.QA[:, :, :], self.dr["qapad"][0].rearrange("p (a b) -> p a b", b=TT),
               writes=[self.QApad] + self.QAb)
        kb.dma("pool", self.ch_qp, self.QA1[:, :, :], self.dr["qapad"][1].rearrange("p (a b) -> p a b", b=TT),
               writes=[self.QApad] + self.QAb)
        self.attn_done = set()
        self.run_streams(iter(()), self.diff_bg(0))
        for t in range(NT):
            self.run_streams(self.diff_main(t), self.diff_bg(t + 1) if t + 1 < NT else None)

    def fox_fgate(self, t):
        kb = self.kb
        one_col = self.CF[0:12, CF_ONE:CF_ONE + 1]
        ps, pb = self.PS[0], [self.PSb[0]]
        ts = slice(t * TT, (t + 1) * TT)
        kb.mm_group(ps[0:12, :], [(self.Wf2[:, k, 0:12], self.HT[:, k, ts]) for k in range(NK)],
                    [self.HTb[k][t] for k in range(NK)] + [self.Wb["f2"]], pb)
        yield
        kb.op("act", lambda hh: hh.activation(out=self.FE[0:12, :], in_=ps[0:12, :], func=AF.Exp,
                                              bias=self.DER[0:12, DE_NFB:DE_NFB + 1], scale=-1.0),
              reads=pb + [self.DERb], writes=[self.FEb])
        yield
        kb.op("act", lambda hh: hh.activation(out=self.FLN[0:12, :], in_=self.FE[0:12, :], func=AF.Ln, bias=one_col, scale=1.0),
              reads=[self.FEb, self.cstb], writes=[self.FLNb])
        yield
        init = 0.0 if t == 0 else self.FCAR[0:12, 0:1]
        kb.op("dve", lambda hh: hh.tensor_tensor_scan(
            out=self.FCS[0:12, :], data0=one_col.to_broadcast([12, TT]), data1=self.FLN[0:12, :], initial=init,
            op0=ALU.mult, op1=ALU.add), reads=[self.FLNb, self.FCARb, self.cstb], writes=[self.FCSb])
        kb.op("dve", lambda hh: hh.tensor_copy(out=self.FCAR[0:12, 0:1], in_=self.FCS[0:12, TT - 1:TT]),
              reads=[self.FCSb], writes=[self.FCARb])
        yield
        kb.op("dve", lambda hh: hh.tensor_scalar(out=self.FH[0:12, :], in0=self.FCS[0:12, :], scalar1=-1.0, scalar2=None, op0=ALU.mult),
              [self.FCSb], [self.FHb])
        kb.op("dve", lambda hh: hh.scalar_tensor_tensor(out=self.FR1[0:12, :], in0=self.FCS[0:12, :], scalar=-1.0,
                                                        in1=self.FH[0:12, :], op0=ALU.mult, op1=ALU.subtract),
              [self.FCSb, self.FHb], [self.FR1b])
        yield
        kb.op("dve", lambda hh: hh.tensor_copy(out=self.FM[0:12, :], in_=self.FR1[0:12, :]), [self.FR1b], [self.FMb])
        kb.op("dve", lambda hh: hh.tensor_tensor(out=self.FR2[0:12, :], in0=self.FR1[0:12, :], in1=self.FM[0:12, :], op=ALU.subtract),
              [self.FR1b, self.FMb], [self.FR2b])
        yield
        kb.op("dve", lambda hh: hh.tensor_copy(out=self.FL[0:12, :], in_=self.FR2[0:12, :]), [self.FR2b], [self.FLb])
        mj = lambda j: self.CF[0:12, CF_MJ + j:CF_MJ + j + 1]
        kb.op("dve", lambda hh: hh.tensor_scalar(out=self.FS[0:12, :], in0=self.FH[0:12, :], scalar1=mj(0), scalar2=None, op0=ALU.mult),
              [self.FHb, self.cstb], [self.FSb])
        yield
        kb.op("dve", lambda hh: hh.scalar_tensor_tensor(out=self.FS[0:12, :], in0=self.FM[0:12, :], scalar=mj(1),
                                                        in1=self.FS[0:12, :], op0=ALU.mult, op1=ALU.add),
              [self.FMb, self.FSb], [self.FSb])
        kb.op("dve", lambda hh: hh.scalar_tensor_tensor(out=self.FS[0:12, :], in0=self.FL[0:12, :], scalar=mj(2),
                                                        in1=self.FS[0:12, :], op0=ALU.mult, op1=ALU.add),
              [self.FLb, self.FSb], [self.FSb])
        yield
        ps2, pb2 = self.PS[1], [self.PSb[1]] + self.PSsub[1]
        for tc in range(4):
            kb.mm(ps2[:, tc * 4:(tc + 1) * 4], self.FS[0:12, tc * 128:(tc + 1) * 128], self.SEL[0:12, 0:4], True, True,
                  reads=[self.FSb, self.cstb], writes=pb2 if tc == 0 else [], inc=(tc == 3), skip=True)
        for b in pb2:
            b.w = ("pe", kb.engs["pe"].cnt)
        yield
        kb.op("dve", lambda hh: hh.tensor_copy(
            out=self.FBIAS[:, 4 * t:4 * t + 4, :], in_=ps2[:, 0:16].rearrange("p (a b) -> p a b", b=4)),
            reads=pb2, writes=[self.FBIASb[t]])
        yield
        if t > 0:
            yield (lambda t=t: (t - 1, 3) in self.attn_done)
        for h in range(4):
            kb.op("dve", lambda hh, h=h: hh.tensor_scalar(out=self.QA[64:76, h, :], in0=self.FS[0:12, :],
                                                          scalar1=self.CF[0:12, CF_HM + h:CF_HM + h + 1], scalar2=None, op0=ALU.mult),
                  [self.FSb, self.cstb], [self.QAb[h]])
        yield

    def fox_wload(self, l):
        self.wload(l, "f1", self.Wf1)
        self.wload(l, "f2", self.Wf2)
        self.woload(l, "of", self.WOf, 768, 2)

    def fox_bg(self, t):
        ts = slice(t * TT, (t + 1) * TT)
        yield from self.chain_v(self.Wf1, self.Wb["f1"], 512, t)
        for h in range(4):
            if t > 0:
                yield (lambda h=h, t=t: (t - 1, h) in self.attn_done)
            outs = [(self.QA[0:64, h, :], [self.QAb[h]], 0, 64, self.DER[0:64, DE_GF:DE_GF + 1], self.DERb),
                    (self.KA[0:64, h, ts], [self.KAb[h][t]], 64, 64, self.DER[64:128, DE_GF:DE_GF + 1], self.DERb)]
            yield from self.chain_qk(self.Wf1, self.Wb["f1"], h * 128, 128, t, self.BD64, 1.0 / 64, outs)
        yield from self.fox_fgate(t)

    def fox_post(self, h, t):
        kb = self.kb
        ob = 4 + h
        o0, ob0 = self.PS[ob], self.PSb[ob]
        kb.op("dve", lambda hh: hh.reciprocal(out=self.R1[0:64, :], in_=o0[64:128, :]), [ob0], [self.R1b])
        yield
        hb = (h % 2) * 64
        kb.op("dve", lambda hh: hh.tensor_tensor(
            out=self.OTT[hb:hb + 64, h // 2, :], in0=o0[0:64, :], in1=self.R1[0:64, :], op=ALU.mult),
            [ob0, self.R1b], [self.OTTb[h]])
        yield

    def fox_main(self, t):
        for h in range(4):
            yield from self.attn_core(h, t, [self.QA],
                                      lambda kbk, h=h: (self.FBIAS[:, kbk, h:h + 1], [self.FBIASb[kbk // 4]]), 4, (4 + h,))
            self.attn_done.add((t, h))
            if h >= 1:
                yield from self.fox_post(h - 1, t)
        yield from self.fox_post(3, t)
        yield from self.wout_gen(self.WOf, self.Wb["of"], self.OTT, self.OTTb, 2, t, (4, 5))

    def fox_phase(self, l):
        kb = self.kb
        if not getattr(self, "fox_prefetched", False):
            self.fox_wload(l)
        self.fox_prefetched = False
        kb.op("pool", lambda h: h.memset(self.KA[64:128, :, :], 0.0), writes=[self.KApad])
        kb.op("pool", lambda h: h.memset(self.KA[64:76, :, :], 1.0), writes=[self.KApad])
        kb.op("pool", lambda h: h.memset(self.QA[64:128, :, :], 0.0), writes=[self.QApad] + self.QAb)
        kb.op("pool", lambda h: h.memset(self.V1[:, :, :, 64:128], 1.0), writes=[self.V1ones])
        self.attn_done = set()
        self.run_streams(iter(()), self.fox_bg(0))
        for t in range(NT):
            self.run_streams(self.fox_main(t), self.fox_bg(t + 1) if t + 1 < NT else None)

    def gla_bg(self, t, par):
        kb = self.kb
        ts = slice(t * TT, (t + 1) * TT)
        QT, KT, KDEC, VG, SR, EBL = self.gQT[par], self.gKT[par], self.gKDEC[par], self.gVG[par], self.gSR[par], self.gEBL[par]
        QTb, KTb, KDECb, VGb, SRb, EBLb = (self.gQTb[par], self.gKTb[par], self.gKDECb[par], self.gVGb[par],
                                           self.gSRb[par], self.gEBLb[par])
        ps0, pb0 = self.PS[0], [self.PSb[0]]
        hreads = [self.HTb[k][t] for k in range(NK)]
        g1, g1b = self.Wg1, self.Wb["g1"]
        kb.mm_group(ps0[0:16, :], [(g1[:, k, 512:528], self.HT[:, k, ts]) for k in range(NK)], hreads + [g1b], pb0)
        yield
        kb.op("act", lambda hh: hh.activation(out=self.GAT[0:16, :], in_=ps0[0:16, :], func=AF.Copy), reads=pb0, writes=[self.GATb])
        yield
        for h in range(4):
            kb.mm(ps0[:, :], self.AW2[:, h, :], self.GAT[:, :], True, True,
                  reads=[self.AW2b, self.GATb, self.gpad], writes=pb0)
            yield
            kb.op("act", lambda hh, h=h: hh.activation(out=self.LSP[:, :], in_=ps0[:, :], func=AF.Exp,
                                                       bias=self.DER[:, DE_NGAB + h:DE_NGAB + h + 1], scale=-1.0),
                  reads=pb0 + [self.DERb], writes=[self.LSPb])
            kb.op("act", lambda hh: hh.activation(out=self.LSP[:, :], in_=self.LSP[:, :], func=AF.Ln,
                                                  bias=self.CF[:, CF_ONE:CF_ONE + 1], scale=1.0),
                  reads=[self.LSPb, self.cstb], writes=[self.LSPb])
            yield
            kb.op("dve", lambda hh: hh.tensor_tensor_scan(out=self.BL[:, :], data0=self.MASK[:, :], data1=self.LSP[:, :],
                                                          initial=0.0, op0=ALU.mult, op1=ALU.add),
                  reads=[self.LSPb, self.cstb], writes=[self.BLb])
            yield
            kb.op("act", lambda hh: hh.activation(out=self.EB[0:64, :], in_=self.BL[0:64, :], func=AF.Exp, scale=-1.0 / 16),
                  [self.BLb], [self.EBb])
            kb.op("act", lambda hh: hh.activation(out=self.ENB[64:128, :], in_=self.BL[64:128, :], func=AF.Exp, scale=1.0 / 16),
                  [self.BLb], [self.ENBb])
            yield
            bl3 = self.BL[64:128, :].rearrange("p (c t) -> p c t", t=64)
            kb.op("dve", lambda hh, bl3=bl3: hh.tensor_tensor(
                out=self.DD[64:128, :].rearrange("p (c t) -> p c t", t=64), in0=bl3,
                in1=bl3[:, :, 63:64].to_broadcast([64, 8, 64]), op=ALU.subtract), [self.BLb], [self.DDb])
            yield
            kb.op("act", lambda hh: hh.activation(out=self.DD[64:128, :], in_=self.DD[64:128, :], func=AF.Exp, scale=1.0 / 16),
                  [self.DDb], [self.DDb])
            kb.op("dve", lambda hh, h=h: hh.tensor_copy(
                out=EBL[0:64, h, :], in_=self.EB[0:64, :].rearrange("p (c t) -> p c t", t=64)[:, :, 63]),
                [self.EBb], [EBLb[h]])
            yield
            kb.mm_group(ps0[:, :], [(g1[:, k, h * 128:(h + 1) * 128], self.HT[:, k, ts]) for k in range(NK)], hreads + [g1b], pb0)
            yield
            kb.op("dve", lambda hh, h=h: hh.scalar_tensor_tensor(
                out=QT[0:64, h, :], in0=ps0[0:64, :], scalar=0.125, in1=self.EB[0:64, :], op0=ALU.mult, op1=ALU.mult),
                pb0 + [self.EBb], [QTb[h]])
            kb.op("dve", lambda hh, h=h: hh.tensor_tensor(out=KT[0:64, h, :], in0=ps0[64:128, :], in1=self.ENB[64:128, :], op=ALU.mult),
                  pb0 + [self.ENBb], [KTb[h]])
            kb.op("dve", lambda hh: hh.tensor_tensor(out=self.KDT[0:64, :], in0=ps0[64:128, :], in1=self.DD[64:128, :], op=ALU.mult),
                  pb0 + [self.DDb], [self.KDTb])
            yield
            pst_b = ps0[:].bitcast(BF16)[:, 0:512].rearrange("p (a c) -> p a c", a=4)
            for tc in range(4):
                kb.op("pe", lambda hh, tc=tc: hh.transpose(
                    out=pst_b[:, tc, :], in_=self.KDT[:, tc * 128:(tc + 1) * 128], identity=self.IDENT[:, :]),
                    reads=[self.KDTb, self.cstb, self.gpad], writes=pb0 if tc == 0 else [], inc=(tc == 3))
            for b_ in pb0:
                b_.w = ("pe", kb.engs["pe"].cnt)
            yield
            kb.op("act", lambda hh, h=h, pst_b=pst_b: hh.activation(out=KDEC[0:64, :, 0, h, :], in_=pst_b[0:64, :, 0:64], func=AF.Copy),
                  reads=pb0, writes=[KDECb])
            kb.op("act", lambda hh, h=h, pst_b=pst_b: hh.activation(out=KDEC[64:128, :, 1, h, :], in_=pst_b[64:128, :, 0:64], func=AF.Copy),
                  reads=pb0, writes=[KDECb])
            yield
        for tc in range(4):
            tok = slice(t * TT + tc * 128, t * TT + (tc + 1) * 128)
            kb.mm_group(ps0[:, :], [(self.HT[:, k, tok], self.Wg2[:, k, 0:512]) for k in range(NK)], hreads + [self.Wb["g2"]], pb0)
            yield
            kb.op("dve", lambda hh, tc=tc: hh.tensor_copy(out=VG[:, tc, :], in_=ps0[:, :]), reads=pb0, writes=[VGb[tc]])
            yield
        if t > 0:
            yield (lambda t=t: (t - 1) in self.gla_done)
        for h in range(4):
            kb.mm_group(ps0[:, :], [(self.Wg3[:, k, h * 128:(h + 1) * 128], self.HT[:, k, ts]) for k in range(NK)],
                        hreads + [self.Wb["g3"]], pb0)
            yield
            kb.op("act", lambda hh, h=h: hh.activation(out=SR[:, h, :], in_=ps0[:, :], func=AF.Silu), reads=pb0, writes=[SRb[h]])
            yield

    def gla_main(self, t, par):
        kb = self.kb
        QT, KT, KDEC, VG, SR, EBL = self.gQT[par], self.gKT[par], self.gKDEC[par], self.gVG[par], self.gSR[par], self.gEBL[par]
        QTb, KTb, KDECb, VGb, SRb, EBLb = (self.gQTb[par], self.gKTb[par], self.gKDECb[par], self.gVGb[par],
                                           self.gSRb[par], self.gEBLb[par])
        units = [(c, h) for c in range(8) for h in range(4)]

        def stage1(u, c, h):
            base = (c % 2) * 64
            tc = c // 2
            cs = slice(c * 64, (c + 1) * 64)
            bj = 2 + (u % 2)
            pbk = [self.PSb[bj]]
            pbv = [self.PSb[1]]
            psa = self.PS[bj][0:64, 0:64]
            pkv = self.PS[1][0:64, (u % 4) * 128:(u % 4 + 1) * 128]
            kb.mm(psa, KT[:, h, cs], QT[:, h, cs], True, True, reads=[KTb[h], QTb[h], self.gpad], writes=pbk)
            kb.mm(pkv, KDEC[:, tc, c % 2, h, :], VG[:, tc, h * 128:(h + 1) * 128], True, True,
                  reads=[KDECb, VGb[tc], self.gpad], writes=pbv)
            ai = u % 8
            kb.op("dve", lambda hh: hh.tensor_tensor(out=self.AT[base:base + 64, ai, :], in0=psa, in1=self.TRI[0:64, :], op=ALU.mult),
                  reads=pbk + [self.cstb], writes=[self.ATb[ai]])
            gc = 8 * t + c
            kb.op("dve", lambda hh: hh.scalar_tensor_tensor(
                out=self.STB[0:64, (gc + 1) % 2, h, :], in0=self.ST[0:64, h, :], scalar=EBL[0:64, h, c:c + 1], in1=pkv,
                op0=ALU.mult, op1=ALU.add), reads=[self.STb[h], EBLb[h]] + pbv, writes=[self.STBb[(gc + 1) % 2][h]])
            kb.op("dve", lambda hh: hh.scalar_tensor_tensor(
                out=self.ST[0:64, h, :], in0=self.ST[0:64, h, :], scalar=EBL[0:64, h, c:c + 1], in1=pkv,
                op0=ALU.mult, op1=ALU.add), reads=[self.STb[h], EBLb[h]] + pbv, writes=[self.STb[h]])
            return ai

        def stage2(u, c, h, ai):
            base = (c % 2) * 64
            tc = c // 2
            cs = slice(c * 64, (c + 1) * 64)
            po, pob = self.PS[4 + h], self.PSb[4 + h]
            kb.mm(po[:, cs], VG[:, tc, h * 128:(h + 1) * 128], self.AT[:, ai, :], True, False,
                  reads=[VGb[tc], self.ATb[ai], self.gpad], writes=[pob] if c == 0 else [], inc=False, skip=True)
            gc = 8 * t + c
            kb.mm(po[:, cs], self.STB[:, gc % 2, h, :], QT[:, h, cs], False, True,
                  reads=[self.STBb[gc % 2][h], QTb[h]], writes=[], inc=True, skip=True)
            if c == 7:
                pob.w = ("pe", kb.engs["pe"].cnt)

        prev = None
        for u, (c, h) in enumerate(units):
            st = stage1(u, c, h)
            if prev is not None:
                stage2(*prev)
            prev = (u, c, h, st)
            yield
        stage2(*prev)
        yield
        b1 = [self.PSb[2]]
        for h in range(4):
            po, pob = self.PS[4 + h], self.PSb[4 + h]
            i = self.sq_i % 2
            self.sq_i += 1
            kb.op("act", lambda hh, i=i, po=po: hh.activation(out=self.SQ[i][:], in_=po[:], func=AF.Square),
                  reads=[pob], writes=[self.SQb[i]])
            kb.mm(self.PS[2][:], self.ONES[:], self.SQ[i][:], True, True, reads=[self.SQb[i], self.cstb], writes=b1)
            yield
            R, Rb = self.rstd_from(self.PS[2][:], 128, 1.0 / 128, b1, 0)
            kb.op("dve", lambda hh, po=po: hh.scalar_tensor_tensor(
                out=self.UU[:], in0=po[:], scalar=self.SPL[:, SP_GGO:SP_GGO + 1], in1=R[:], op0=ALU.mult, op1=ALU.mult),
                reads=[pob, Rb, self.SPLb], writes=[self.UUb])
            kb.op("dve", lambda hh, h=h: hh.tensor_tensor(out=self.OTG[:, h, :], in0=self.UU[:], in1=SR[:, h, :], op=ALU.mult),
                  reads=[self.UUb, SRb[h]], writes=[self.OTGb[h]])
            yield
        self.gla_done.add(t)
        yield from self.wout_gen(self.WOg, self.Wb["og"], self.OTG, self.OTGb, 4, t, (3, 2))

    def gla_phase(self, l):
        kb = self.kb
        self.wload(l, "g1", self.Wg1)
        self.wload(l, "g2", self.Wg2)
        self.wload(l, "g3", self.Wg3)
        self.woload(l, "og", self.WOg, 256, 4)
        kb.op("pool", lambda h: h.memset(self.ST[0:64, :, :], 0.0), writes=self.STb)
        kb.op("pool", lambda h: h.memset(self.STB[:, :, :, :], 0.0), writes=self.STBb[0] + self.STBb[1] + [self.gpad])
        kb.op("pool", lambda h: h.memset(self.AT[:, :, :], 0.0), writes=self.ATb + [self.gpad])
        kb.op("pool", lambda h: h.memset(self.GAT[:, :], 0.0), writes=[self.GATb, self.gpad])
        kb.op("pool", lambda h: h.memset(self.KDT[64:128, :], 0.0), writes=[self.gpad])
        for p_ in range(2):
            kb.op("pool", lambda h, p_=p_: h.memset(self.gQT[p_][64:128, :, :], 0.0), writes=[self.gpad])
            kb.op("pool", lambda h, p_=p_: h.memset(self.gKT[p_][64:128, :, :], 0.0), writes=[self.gpad])
            kb.op("pool", lambda h, p_=p_: h.memset(self.gKDEC[p_][64:128, :, 0, :, :], 0.0), writes=[self.gpad])
            kb.op("pool", lambda h, p_=p_: h.memset(self.gKDEC[p_][0:64, :, 1, :, :], 0.0), writes=[self.gpad])
        self.gla_done = set()
        self.run_streams(iter(()), self.gla_bg(0, 0))
        for t in range(NT):
            self.run_streams(self.gla_main(t, t % 2), self.gla_bg(t + 1, (t + 1) % 2) if t + 1 < NT else None, ratio=2)


IN_OFF = {"d_q": 0, "d_k": 256, "d_v": 512, "g_q": 768, "g_k": 1024, "g_v": 1280, "g_r": 1792, "g_a": 2304,
          "f_q": 2320, "f_k": 2576, "f_v": 2832, "f_f": 3088}


def host_w_in(w_in):
    w_in = np.asarray(w_in, dtype=np.float32)
    out = np.zeros((L, D, NCOL), np.float32)

    def put(dst, src, n):
        out[:, :, dst:dst + n] = w_in[:, :, src:src + n]

    for h in range(4):
        for m in range(2):
            put(G_D1 + h * 128 + m * 32, IN_OFF["d_q"] + h * 64 + m * 32, 32)
            put(G_D1 + h * 128 + 64 + m * 32, IN_OFF["d_k"] + h * 64 + m * 32, 32)
        put(G_F1 + h * 128, IN_OFF["f_q"] + h * 64, 64)
        put(G_F1 + h * 128 + 64, IN_OFF["f_k"] + h * 64, 64)
    put(G_D1 + 512, IN_OFF["d_v"], 256)
    put(G_F1 + 512, IN_OFF["f_v"], 256)
    for j in range(3):
        put(G_F2 + 4 * j, IN_OFF["f_f"], 4)
    for h in range(4):
        put(G_G1 + h * 128, IN_OFF["g_q"] + h * 64, 64)
        put(G_G1 + h * 128 + 64, IN_OFF["g_k"] + h * 64, 64)
    put(G_G1 + 512, IN_OFF["g_a"], 16)
    put(G_G2, IN_OFF["g_v"], 512)
    put(G_G3, IN_OFF["g_r"], 512)
    return out


def host_gn(ffn1_norm, mix_norm, ffn2_norm):
    arr = np.stack([ffn1_norm, mix_norm, ffn2_norm], axis=0)
    arr = arr.reshape(3, L, NK, 128).transpose(3, 0, 1, 2).reshape(128, 3 * L * NK)
    return np.ascontiguousarray(arr, dtype=np.float32)


def host_spl(diff_q_norm, diff_k_norm, diff_out_norm, fox_q_norm, fox_k_norm, gla_out_norm, fox_f_bias,
             gla_alpha_b, diff_lambda):
    spl = np.zeros((L, 128, NSP), np.float32)
    p = np.arange(128)
    for l in range(L):
        spl[l, :, SP_GQD] = diff_q_norm[l][p % 32]
        spl[l, :, SP_GKD] = diff_k_norm[l][p % 32]
        spl[l, :, SP_GDO] = diff_out_norm[l][p % 64]
        spl[l, :, SP_GFQ] = fox_q_norm[l][p % 64]
        spl[l, :, SP_GFK] = fox_k_norm[l][p % 64]
        spl[l, :, SP_GGO] = gla_out_norm[l][p]
        spl[l, :, SP_FB] = fox_f_bias[l][p % 4]
        for h in range(4):
            spl[l, :, SP_GAB + h] = gla_alpha_b[l][h * 64 + p % 64]
        spl[l, :, SP_LAM:SP_LAM + 128] = np.asarray(diff_lambda[l]).reshape(1, 128)
    return spl


def host_consts():
    slopes = [2.0 ** (-8.0 * (h + 1) / 4) for h in range(4)]
    p = np.arange(128)
    cf = np.zeros((128, NCF), np.float32)
    cf[:, CF_EPS] = EPS
    cf[:, CF_ONE] = 1.0
    for h in range(4):
        cf[:, CF_HM + h] = ((p % 4) == h) & (p < 12)
    for j in range(3):
        cf[:, CF_MJ + j] = ((p // 4) == j) & (p < 12)
    cf[:, CF_MASK:CF_MASK + 64] = 1.0
    cf[:, CF_MASK] = 0.0
    s_ = np.arange(64)
    tri = (s_[:, None] <= s_[None, :]).astype(np.float32)
    cf[0:64, CF_TRI:CF_TRI + 64] = tri
    cf[64:128, CF_TRI:CF_TRI + 64] = tri
    db = np.zeros((128, 4, 4, 16), np.float32)
    for h in range(4):
        for t in range(4):
            for kbk in range(16):
                db[:, h, t, kbk] = slopes[h] * (128 * kbk + p - 512 * t)
    cf[:, CF_DBIAS:CF_DBIAS + 256] = db.reshape(128, 256)
    cb = np.zeros((128, NCB), np.float32)
    for h in range(4):
        cb[:, CB_SEL + h] = -1.0 * (((p % 4) == h) & (p < 12))
    cb[:, CB_ID:CB_ID + 128] = np.eye(128, dtype=np.float32)
    cb[:, CB_BD:CB_BD + 128] = ((p[:, None] // 32) == (p[None, :] // 32)).astype(np.float32)
    cb[:, CB_BD64:CB_BD64 + 128] = ((p[:, None] // 64) == (p[None, :] // 64)).astype(np.float32)
    j = np.arange(128)
    diag = np.zeros((128, 5, 128), np.float32)
    allowed = (p[:, None] // 64) <= (j[None, :] // 64)
    fut = p[:, None] > j[None, :]
    for h in range(4):
        m = np.where(fut, -2.0 * slopes[h] * (p[:, None] - j[None, :]), 0.0)
        diag[:, h, :] = np.where(allowed, m, NEG)
    diag[:, 4, :] = np.where(p[:, None] <= j[None, :], 0.0, NEG)
    cb[:, CB_DIAG:CB_DIAG + 640] = diag.reshape(128, 640)
    qp = np.zeros((2, 128, 4, TT), np.float32)
    jj = np.arange(TT)
    for h in range(4):
        for mi, r in enumerate((32, 96)):
            qp[mi, r, h, :] = -slopes[h] * 128.0 * (jj // 128)
            qp[mi, r + 1, h, :] = -slopes[h] * (jj % 128)
    return cf, cb, qp.reshape(2, 128, 4 * TT)


def kernel(x, ffn1_norm, ffn1_w13, ffn1_w2, mix_norm, w_in, w_out, diff_q_norm,
           diff_k_norm, diff_lambda, diff_out_norm, gla_alpha_w2, gla_alpha_b,
           gla_out_norm, fox_q_norm, fox_k_norm, fox_f_bias, ffn2_norm, ffn2_w13,
           ffn2_w2, _prog=None, _n_cores=N_CORES):
    x = np.asarray(x, dtype=np.float32)
    prog = _prog or Prog()
    nc = prog.build()
    ns = prog.n_seq
    f = lambda a: np.ascontiguousarray(np.asarray(a), dtype=np.float32)
    xT = np.ascontiguousarray(x.transpose(0, 2, 1))
    cf, cb, qp = host_consts()
    shared = {
        "ffn1_w13": f(ffn1_w13), "ffn1_w2": f(ffn1_w2), "ffn2_w13": f(ffn2_w13), "ffn2_w2": f(ffn2_w2),
        "w_in_r": host_w_in(w_in), "w_out": f(w_out),
        "aw2": np.ascontiguousarray(np.repeat(f(gla_alpha_w2).reshape(L, 16, 4, 1, 64), 2, axis=3).reshape(L, 16, 512)),
        "gn": host_gn(f(ffn1_norm), f(mix_norm), f(ffn2_norm)),
        "spl": host_spl(f(diff_q_norm), f(diff_k_norm), f(diff_out_norm), f(fox_q_norm), f(fox_k_norm),
                        f(gla_out_norm), f(fox_f_bias), f(gla_alpha_b), f(diff_lambda)),
        "cstf": cf, "cstb": cb, "qapad": qp,
    }
    in_maps = []
    for c in range(_n_cores):
        m = dict(shared)
        m["xT"] = xT[c * ns:(c + 1) * ns]
        in_maps.append(m)
    res = run_bass_kernel_spmd(nc, in_maps, core_ids=list(range(_n_cores)))
    outT = np.concatenate([r["outT"] for r in res.results], axis=0)
    return np.ascontiguousarray(outT.transpose(0, 2, 1))
```
